# Optimizing a Trainium2 kernel written in Bass

```python
import jax
import jax.numpy as jnp
from jax import lax
import numpy as np

D_MODEL = 2048
BATCH = 8
SEQ = 2048
DEPTH = 2

HEAD_DIM = 64
GRID_W = 64
Q_BLOCK = 128
ROPE_THETA = 10000.0
NEG_INF = -1e30
EPS = 1e-6
N_BRANCH = 4
BRANCH_WIDTH = 512

A_HEADS = 8
A_DILATIONS = ((128, 1), (512, 4), (2048, 16))
N_DIL = 3
MLA_HEADS = 8
MLA_Q_LORA = 512
MLA_KV_LORA = 256
MLA_NOPE = 64
MLA_ROPE = 32
MLA_V = 64
C_Q_HEADS = 8
C_KV_HEADS = 2
D_Q_HEADS = 8
D_KV_HEADS = 2
D_HALF_WINDOW = 128

A_COLS = 3 * N_DIL * A_HEADS * HEAD_DIM
B_COLS = MLA_Q_LORA + MLA_KV_LORA + MLA_ROPE
C_COLS = (C_Q_HEADS + 2 * C_KV_HEADS) * HEAD_DIM
D_COLS = (D_Q_HEADS + 2 * D_KV_HEADS) * HEAD_DIM
GATE_COLS = N_BRANCH * D_MODEL
IN_SPLITS = (A_COLS, A_COLS + B_COLS, A_COLS + B_COLS + C_COLS, A_COLS + B_COLS + C_COLS + D_COLS)
N_IN = A_COLS + B_COLS + C_COLS + D_COLS + GATE_COLS

N_EXPERTS = 16
EXPERT_FF = D_MODEL // 2
CAPACITY_FACTOR = 2

kernel_name = 'hybrid_gated_mixer_ec_moe_encoder'


def rms_norm(x, g):
    xf = x.astype(jnp.float32)
    y = xf * lax.rsqrt(jnp.mean(xf * xf, axis=-1, keepdims=True) + EPS)
    return (y * g.astype(jnp.float32)).astype(x.dtype)


def alibi_slopes(n):
    return jnp.exp2(-8.0 * jnp.arange(1, n + 1, dtype=jnp.float32) / n)


def rope(x, pos):
    dim = x.shape[-1]
    freqs = ROPE_THETA ** (-jnp.arange(0, dim, 2, dtype=jnp.float32) / dim)
    ang = pos.astype(jnp.float32)[:, None] * freqs[None, :]
    cos = jnp.cos(ang)[None, :, None, :]
    sin = jnp.sin(ang)[None, :, None, :]
    xf = x.astype(jnp.float32)
    x1, x2 = xf[..., : dim // 2], xf[..., dim // 2:]
    return jnp.concatenate([x1 * cos - x2 * sin, x1 * sin + x2 * cos], axis=-1).astype(x.dtype)


def axial_rope(x, rows, cols):
    half = x.shape[-1] // 2
    return jnp.concatenate([rope(x[..., :half], rows), rope(x[..., half:], cols)], axis=-1)


def banded_attention(q, k, v, half_window, slopes, sink=None):
    Z, L, H, G, dh = q.shape
    W = half_window
    nb = -(-L // W)
    Lp = nb * W
    pad = Lp - L
    qb = jnp.pad(q, ((0, 0), (0, pad), (0, 0), (0, 0), (0, 0))).reshape(Z, nb, W, H, G, dh)
    kp = jnp.pad(k, ((0, 0), (W, pad + W), (0, 0), (0, 0)))
    vp = jnp.pad(v.astype(jnp.float32), ((0, 0), (W, pad + W), (0, 0), (0, 0)))

    def key_blocks(a):
        return jnp.concatenate([a[:, i * W: i * W + Lp].reshape(Z, nb, W, H, a.shape[-1]) for i in range(3)], axis=2)

    kb, vb = key_blocks(kp), key_blocks(vp)
    qpos = jnp.arange(nb)[:, None] * W + jnp.arange(W)[None, :]
    kpos = jnp.arange(nb)[:, None] * W - W + jnp.arange(3 * W)[None, :]
    rel = kpos[:, None, :] - qpos[:, :, None]
    valid = (jnp.abs(rel) <= W) & (kpos[:, None, :] >= 0) & (kpos[:, None, :] < L)
    dist = jnp.abs(rel).astype(jnp.float32)
    s = jnp.einsum('znqhgd,znkhd->znhgqk', qb, kb).astype(jnp.float32) * (dh ** -0.5)
    s = s - slopes[None, None, :, :, None, None] * dist[None, :, None, None, :, :]
    s = jnp.where(valid[None, :, None, None, :, :], s, NEG_INF)
    m = jnp.max(s, axis=-1)
    if sink is not None:
        sk = sink.astype(jnp.float32)[None, None, :, :, None]
        m = jnp.maximum(m, sk)
    p = jnp.exp(s - m[..., None])
    den = jnp.sum(p, axis=-1)
    if sink is not None:
        den = den + jnp.exp(sk - m)
    o = jnp.einsum('znhgqk,znkhe->znqhge', p, vb) / jnp.transpose(den, (0, 1, 4, 2, 3))[..., None]
    lse = jnp.transpose(m + jnp.log(den), (0, 1, 4, 2, 3))
    o = o.reshape(Z, Lp, H, G, v.shape[-1])[:, :L]
    lse = lse.reshape(Z, Lp, H, G)[:, :L]
    return o.astype(q.dtype), lse


def dense_attention(q, k, v, scale):
    B, S, H, G, dq = q.shape
    nq = S // Q_BLOCK
    qb = jnp.moveaxis(q.reshape(B, nq, Q_BLOCK, H, G, dq), 1, 0)
    vf = v.astype(jnp.float32)

    def block(qi):
        s = jnp.einsum('bqhgd,bkhd->bhgqk', qi, k).astype(jnp.float32) * scale
        p = jax.nn.softmax(s, axis=-1)
        return jnp.einsum('bhgqk,bkhe->bqhge', p, vf).astype(q.dtype)

    out = lax.map(block, qb)
    return jnp.moveaxis(out, 0, 1).reshape(B, S, H, G, v.shape[-1])


def dilated_attention(q, k, v):
    B, S = q.shape[:2]
    slopes = alibi_slopes(A_HEADS)
    outs, lses = [], []
    for g, (window, dil) in enumerate(A_DILATIONS):
        half = (window // 2) // dil
        Ld = S // dil

        def fold(a):
            return jnp.transpose(a[:, :, g].reshape(B, Ld, dil, A_HEADS, HEAD_DIM), (0, 2, 1, 3, 4)).reshape(B * dil, Ld, A_HEADS, HEAD_DIM)

        o, lse = banded_attention(fold(q)[:, :, :, None, :], fold(k), fold(v), half, (slopes * dil)[:, None])
        o = jnp.transpose(o[:, :, :, 0].reshape(B, dil, Ld, A_HEADS, HEAD_DIM), (0, 2, 1, 3, 4)).reshape(B, S, A_HEADS, HEAD_DIM)
        lse = jnp.transpose(lse[:, :, :, 0].reshape(B, dil, Ld, A_HEADS), (0, 2, 1, 3)).reshape(B, S, A_HEADS)
        outs.append(o)
        lses.append(lse)
    w = jax.nn.softmax(jnp.stack(lses), axis=0)
    o = jnp.einsum('nbsh,nbshd->bshd', w, jnp.stack(outs).astype(jnp.float32))
    return o.reshape(B, S, A_HEADS * HEAD_DIM).astype(q.dtype)


def token_mixer(xn, w_in, b_gate, g_mla_q, g_mla_kv, w_mla_uq, w_mla_ukv, g_c_q, g_c_k, sink_d, w_branch, w_out):
    B, S, D = xn.shape
    rows_count = S // GRID_W
    pos = jnp.arange(S)
    rows = jnp.repeat(jnp.arange(rows_count), GRID_W)
    cols = jnp.tile(jnp.arange(GRID_W), rows_count)

    proj = xn @ w_in
    pa, pb, pc, pd, pg = jnp.split(proj, IN_SPLITS, axis=-1)

    pa = pa.reshape(B, S, 3, N_DIL, A_HEADS, HEAD_DIM)
    o_a = dilated_attention(pa[:, :, 0], pa[:, :, 1], pa[:, :, 2])

    c_q, c_kv, k_pe = jnp.split(pb, (MLA_Q_LORA, MLA_Q_LORA + MLA_KV_LORA), axis=-1)
    q_b = (rms_norm(c_q, g_mla_q) @ w_mla_uq).reshape(B, S, MLA_HEADS, MLA_NOPE + MLA_ROPE)
    q_b = jnp.concatenate([q_b[..., :MLA_NOPE], rope(q_b[..., MLA_NOPE:], pos)], axis=-1)
    kv_b = (rms_norm(c_kv, g_mla_kv) @ w_mla_ukv).reshape(B, S, MLA_HEADS, MLA_NOPE + MLA_V)
    k_pe = jnp.broadcast_to(rope(k_pe[:, :, None, :], pos), (B, S, MLA_HEADS, MLA_ROPE))
    k_b = jnp.concatenate([kv_b[..., :MLA_NOPE], k_pe], axis=-1)
    v_b = kv_b[..., MLA_NOPE:]
    o_b = dense_attention(q_b[:, :, :, None, :], k_b, v_b, (MLA_NOPE + MLA_ROPE) ** -0.5).reshape(B, S, MLA_HEADS * MLA_V)

    pc = pc.reshape(B, S, C_Q_HEADS + 2 * C_KV_HEADS, HEAD_DIM)
    q_c = axial_rope(rms_norm(pc[:, :, :C_Q_HEADS], g_c_q), rows, cols)
    k_c = axial_rope(rms_norm(pc[:, :, C_Q_HEADS:C_Q_HEADS + C_KV_HEADS], g_c_k), rows, cols)
    v_c = pc[:, :, C_Q_HEADS + C_KV_HEADS:]
    q_c = q_c.reshape(B, S, C_KV_HEADS, C_Q_HEADS // C_KV_HEADS, HEAD_DIM)
    o_c = dense_attention(q_c, k_c, v_c, HEAD_DIM ** -0.5).reshape(B, S, C_Q_HEADS * HEAD_DIM)

    pd = pd.reshape(B, S, D_Q_HEADS + 2 * D_KV_HEADS, HEAD_DIM)
    grp = D_Q_HEADS // D_KV_HEADS
    q_d = pd[:, :, :D_Q_HEADS].reshape(B, S, D_KV_HEADS, grp, HEAD_DIM)
    k_d = pd[:, :, D_Q_HEADS:D_Q_HEADS + D_KV_HEADS]
    v_d = pd[:, :, D_Q_HEADS + D_KV_HEADS:]
    o_d, _ = banded_attention(q_d, k_d, v_d, D_HALF_WINDOW, alibi_slopes(D_Q_HEADS).reshape(D_KV_HEADS, grp), sink_d.reshape(D_KV_HEADS, grp))
    o_d = o_d.reshape(B, S, D_Q_HEADS * HEAD_DIM)

    mixed = jnp.zeros((B, S, D), xn.dtype)
    for i, o in enumerate((o_a, o_b, o_c, o_d)):
        gate = jax.nn.sigmoid(pg[..., i * D:(i + 1) * D] + b_gate[i])
        mixed = mixed + gate * (o @ w_branch[i])
    return mixed @ w_out


def expert_choice_ffn(xn, w_router, w_gate, w_up, w_down):
    B, S, D = xn.shape
    cap = CAPACITY_FACTOR * S // N_EXPERTS
    probs = jax.nn.softmax((xn @ w_router).astype(jnp.float32), axis=-1)
    gate, idx = lax.top_k(jnp.transpose(probs, (0, 2, 1)), cap)
    xe = jax.vmap(lambda xb, ib: xb[ib])(xn, idx)
    h = jax.nn.silu(jnp.einsum('becd,edf->becf', xe, w_gate)) * jnp.einsum('becd,edf->becf', xe, w_up)
    ye = jnp.einsum('becf,efd->becd', h, w_down) * gate[..., None].astype(xn.dtype)
    return jax.vmap(lambda yb, ib: jax.ops.segment_sum(yb.reshape(-1, D), ib.reshape(-1), num_segments=S))(ye, idx)


def setup_inputs(seed: int = 0) -> dict:
    key = jax.random.key(seed)
    ks = jax.random.split(key, 20)
    f32 = jnp.float32

    def nrm(k, shape, scale):
        return jax.random.normal(k, shape, f32) * scale

    return {
        'x': nrm(ks[0], (BATCH, SEQ, D_MODEL), 1.0),
        'w_in': nrm(ks[1], (DEPTH, D_MODEL, N_IN), D_MODEL ** -0.5),
        'b_gate': nrm(ks[2], (DEPTH, N_BRANCH, D_MODEL), 0.02),
        'g_attn_norm': 1.0 + nrm(ks[3], (DEPTH, D_MODEL), 0.02),
        'g_ffn_norm': 1.0 + nrm(ks[4], (DEPTH, D_MODEL), 0.02),
        'g_mla_q': 1.0 + nrm(ks[5], (DEPTH, MLA_Q_LORA), 0.02),
        'g_mla_kv': 1.0 + nrm(ks[6], (DEPTH, MLA_KV_LORA), 0.02),
        'w_mla_uq': nrm(ks[7], (DEPTH, MLA_Q_LORA, MLA_HEADS * (MLA_NOPE + MLA_ROPE)), MLA_Q_LORA ** -0.5),
        'w_mla_ukv': nrm(ks[8], (DEPTH, MLA_KV_LORA, MLA_HEADS * (MLA_NOPE + MLA_V)), MLA_KV_LORA ** -0.5),
        'g_c_q': 1.0 + nrm(ks[9], (DEPTH, HEAD_DIM), 0.02),
        'g_c_k': 1.0 + nrm(ks[10], (DEPTH, HEAD_DIM), 0.02),
        'sink_d': nrm(ks[11], (DEPTH, D_Q_HEADS), 0.5),
        'w_branch': nrm(ks[12], (DEPTH, N_BRANCH, BRANCH_WIDTH, D_MODEL), BRANCH_WIDTH ** -0.5),
        'w_out': nrm(ks[13], (DEPTH, D_MODEL, D_MODEL), D_MODEL ** -0.5),
        'w_router': nrm(ks[14], (DEPTH, D_MODEL, N_EXPERTS), D_MODEL ** -0.5),
        'w_exp_gate': nrm(ks[15], (DEPTH, N_EXPERTS, D_MODEL, EXPERT_FF), D_MODEL ** -0.5),
        'w_exp_up': nrm(ks[16], (DEPTH, N_EXPERTS, D_MODEL, EXPERT_FF), D_MODEL ** -0.5),
        'w_exp_down': nrm(ks[17], (DEPTH, N_EXPERTS, EXPERT_FF, D_MODEL), EXPERT_FF ** -0.5),
        'g_final': 1.0 + nrm(ks[18], (D_MODEL,), 0.02),
    }


def reference(x, w_in, b_gate, g_attn_norm, g_ffn_norm, g_mla_q, g_mla_kv, w_mla_uq, w_mla_ukv, g_c_q, g_c_k, sink_d, w_branch, w_out, w_router, w_exp_gate, w_exp_up, w_exp_down, g_final):
    h = x
    for l in range(DEPTH):
        h = h + token_mixer(rms_norm(h, g_attn_norm[l]), w_in[l], b_gate[l], g_mla_q[l], g_mla_kv[l], w_mla_uq[l], w_mla_ukv[l], g_c_q[l], g_c_k[l], sink_d[l], w_branch[l], w_out[l])
        h = h + expert_choice_ffn(rms_norm(h, g_ffn_norm[l]), w_router[l], w_exp_gate[l], w_exp_up[l], w_exp_down[l])
    return rms_norm(h, g_final)
```

```python
import numpy as np
from contextlib import ExitStack
import concourse.bass as bass
import concourse.mybir as mybir
from concourse.bass_utils import run_bass_kernel_spmd

F32 = mybir.dt.float32
BF16 = mybir.dt.bfloat16
AF = mybir.ActivationFunctionType
ALU = mybir.AluOpType
AX = mybir.AxisListType

D_MODEL = 2048
S = 2048
DEPTH = 2
N_IN = 15136
G0 = 6944
EPS = 1e-6
NE = 16
EFF = 1024
CAP = 256
A_DIL = (1, 4, 16)
WEIGHT_NAMES = ["w_in", "g_attn_norm", "sink_d", "g_mla_q", "g_mla_kv", "w_mla_uq", "w_mla_ukv", "g_c_q", "g_c_k", "b_gate", "w_branch", "w_out", "g_ffn_norm", "g_final", "w_router", "w_exp_gate", "w_exp_up", "w_exp_down"]


class Sem:
    def __init__(self, handle, name):
        self.h = handle
        self.name = name
        self.count = 0
        self.dma = name.startswith("d_")


class Buf:
    __slots__ = ("name", "w", "r")

    def __init__(self, name):
        self.name = name
        self.w = None
        self.r = {}


class Q:
    def __init__(self, name, eng, sem):
        self.name = name
        self.eng = eng
        self.sem = sem
        self.waited = {}


class K:
    def __init__(self):
        self.nc = bass.Bass("TRN2", target_bir_lowering=False)
        self.es = ExitStack()
        nc = self.nc
        self.sems = []
        self.q = {}
        for name, eng in (("pe", nc.tensor), ("act", nc.scalar), ("dve", nc.vector),
                          ("pool", nc.gpsimd), ("sp", nc.sync)):
            self.q[name] = Q(name, eng, self.sem("q_" + name))
        self.ninst = 0
        self.uid = 0
        self.sem_pool = {}

    def sem(self, name):
        s = Sem(self.es.enter_context(self.nc.semaphore(name)), name)
        self.sems.append(s)
        return s

    def dsem(self, name):
        if name not in self.sem_pool:
            self.sem_pool[name] = self.sem("d_" + name)
        return self.sem_pool[name]

    def dram(self, name, shape, dt, kind="Internal"):
        return self.nc.dram_tensor(name, list(shape), dt, kind=kind)

    def _deps(self, q, reads, writes):
        deps = {}

        def add(s, v, raw):
            if s is q.sem:
                if not raw or q.name == "pe" or q.name == "sp":
                    return
            if deps.get(s, 0) < v:
                deps[s] = v

        for b in reads:
            if b.w is not None:
                add(b.w[0], b.w[1], True)
        for b in writes:
            if b.w is not None:
                add(b.w[0], b.w[1], False)
            for s, v in b.r.items():
                add(s, v, False)
        for s, v in deps.items():
            if s.dma:
                v = s.count
            if q.waited.get(s, 0) >= v:
                continue
            assert v <= s.count, f"wait on unsignalled token {s.name} {v}>{s.count} from {q.name}"
            q.eng.wait_ge(s.h, v)
            q.waited[s] = v

    def op(self, E, emit, reads=(), writes=(), signal=True):
        q = self.q[E]
        self._deps(q, reads, writes)
        ins = emit(q.eng)
        self.ninst += 1
        if signal:
            q.sem.count += 1
            ins.then_inc(q.sem.h, 1)
            v = q.sem.count
        else:
            v = q.sem.count + 1
        for b in reads:
            b.r[q.sem] = v
        for b in writes:
            b.w = (q.sem, v)
            b.r = {}
        return ins

    def dma(self, E, out, in_, sem, reads=(), writes=(), **kw):
        q = self.q[E]
        self._deps(q, reads, writes)
        ins = q.eng.dma_start(out=out, in_=in_, **kw)
        self.ninst += 1
        sem.count += 16
        ins.then_inc(sem.h, 16)
        for b in reads:
            b.r[sem] = sem.count
        for b in writes:
            b.w = (sem, sem.count)
            b.r = {}
        return ins

    def barrier(self):
        for q in self.q.values():
            for s in self.sems:
                if s is q.sem or s.count == 0:
                    continue
                if q.waited.get(s, 0) >= s.count:
                    continue
                q.eng.wait_ge(s.h, s.count)
                q.waited[s] = s.count

    def mm(self, out, lhsT, rhs, start, stop, reads, writes, signal):
        return self.op("pe", lambda e: e.matmul(out, lhsT=lhsT, rhs=rhs, start=start, stop=stop,
                                                skip_group_check=True),
                       reads=reads, writes=writes, signal=signal)

    def tr(self, out, in_, ident, reads, writes, signal):
        return self.op("pe", lambda e: e.transpose(out, in_, ident), reads=reads, writes=writes, signal=signal)


class Scope:
    def __init__(self, k):
        self.k = k
        self.es = ExitStack()

    def __enter__(self):
        return self

    def __exit__(self, *a):
        self.k.barrier()
        self.es.close()
        return False

    def sbuf(self, name, shape, dt):
        self.k.uid += 1
        return self.es.enter_context(self.k.nc.sbuf_tensor(f"{name}_{self.k.uid}", list(shape), dt))

    def psum(self, name, shape, dt=F32):
        self.k.uid += 1
        return self.es.enter_context(self.k.nc.psum_tensor(f"{name}_{self.k.uid}", list(shape), dt))

    def slots(self, name, n, shape, dt):
        return Slots(self, name, n, shape, dt)


class Slots:
    def __init__(self, sc, name, n, shape, dt, psum=False):
        self.n = n
        if psum:
            self.t = [sc.psum(f"{name}{i}", shape, dt) for i in range(n)]
        else:
            self.t = [sc.sbuf(f"{name}{i}", shape, dt) for i in range(n)]
        self.b = [Buf(f"{name}{i}") for i in range(n)]
        self.s = [sc.k.dsem(f"{name}{i}") for i in range(n)] if not psum else [None] * n
        self.i = 0

    def next(self):
        i = self.i % self.n
        self.i += 1
        return self.t[i], self.b[i], self.s[i]


def fold_view(ap2d, dil, n0, n):
    Ld = S // dil
    if dil == 1:
        return ap2d[:, n0:n0 + n]
    v = ap2d.rearrange("p (j r) -> p r j", r=dil)
    if n <= Ld:
        r, j0 = divmod(n0, Ld)
        assert j0 + n <= Ld
        return v[:, r, j0:j0 + n]
    assert n % Ld == 0 and n0 % Ld == 0
    return v[:, n0 // Ld:(n0 + n) // Ld, :]


def like_fold(ap2d, dil, n):
    Ld = S // dil
    if dil == 1 or n <= Ld:
        return ap2d
    return ap2d.rearrange("p (a j) -> p a j", j=Ld)


class Ctx:
    pass


def stage_norm(k, C, src, g_row, tok_major_dst=None, featT_dst=None, xnT_sb=None, xnT_bufs=None,
               xn_sb=None, xn_bufs=None, router=None):
    with Scope(k) as sc:
        hs = sc.slots("nh", 3, [128, D_MODEL], F32)
        gbc = sc.sbuf("gbc", [128, D_MODEL], F32)
        gb = Buf("gbc")
        k.dma("sp", gbc[:], g_row.partition_broadcast(128), k.dsem("misc"), writes=[gb])
        junk = sc.sbuf("junk", [128, D_MODEL], BF16)
        jb = Buf("junk")
        ss = sc.sbuf("ss", [128, 16], F32)
        rs = sc.sbuf("rs", [128, 16], F32)
        ssb = [Buf(f"ss{i}") for i in range(16)]
        rsb = [Buf(f"rs{i}") for i in range(16)]
        if xn_sb is None:
            xns = sc.slots("xnt", 2, [128, D_MODEL], BF16)
        pst = Slots(sc, "npt", 2, [128, 1024], BF16, psum=True)
        if router is not None:
            wr_sb, wr_b, lg_sb, lg_b = router
            psr = Slots(sc, "npr", 2, [128, 16], F32, psum=True)
            xts = sc.slots("nxT", 2, [128, 16, 128], BF16)
        ident, identb = C.ident_bf, C.ident_bf_b
        srcv = src.rearrange("(i p) d -> i p d", p=128)
        for i in range(16):
            ht, hb, hsem = hs.next()
            k.dma("sp", ht[:], srcv[i], hsem, writes=[hb])
            k.op("act", lambda e: e.activation(out=junk[:], in_=ht[:], func=AF.Square, accum_out=ss[:, i:i + 1]),
                 reads=[hb], writes=[jb, ssb[i]])
            k.op("dve", lambda e: e.tensor_scalar(out=rs[:, i:i + 1], in0=ss[:, i:i + 1], scalar1=1.0 / D_MODEL,
                                                  scalar2=EPS, op0=ALU.mult, op1=ALU.add),
                 reads=[ssb[i]], writes=[rsb[i]])
            k.op("act", lambda e: e.activation(out=rs[:, i:i + 1], in_=rs[:, i:i + 1], func=AF.Sqrt),
                 reads=[rsb[i]], writes=[rsb[i]])
            k.op("dve", lambda e: e.reciprocal(out=rs[:, i:i + 1], in_=rs[:, i:i + 1]),
                 reads=[rsb[i]], writes=[rsb[i]])
            if xn_sb is None:
                xt, xb, _ = xns.next()
                xt_ap = xt[:]
            else:
                xt_ap = xn_sb[:, i, :]
                xb = xn_bufs[i]
            k.op("dve", lambda e: e.scalar_tensor_tensor(out=xt_ap, in0=ht[:], scalar=rs[:, i:i + 1], in1=gbc[:],
                                                         op0=ALU.mult, op1=ALU.mult),
                 reads=[hb, rsb[i], gb], writes=[xb])
            if xnT_sb is None and router is None:
                continue
            if router is not None:
                xT, xTb, _ = xts.next()
            for half in range(2):
                pt, pb, _ = pst.next()
                for j in range(8):
                    c = half * 8 + j
                    k.tr(pt[:, j * 128:(j + 1) * 128], xt_ap[:, c * 128:(c + 1) * 128], ident[:],
                         reads=[xb, identb], writes=[pb], signal=(j == 7))
                src_ap = pt[:].rearrange("p (c t) -> p c t", t=128)
                if xnT_sb is not None:
                    dst = xnT_sb[:, half * 8:(half + 1) * 8, i * 128:(i + 1) * 128]
                    wb_ = [xnT_bufs[i]]
                else:
                    dst = xT[:, half * 8:(half + 1) * 8, :]
                    wb_ = [xTb]
                if half == 0:
                    k.op("act", lambda e: e.copy(out=dst, in_=src_ap), reads=[pb], writes=wb_)
                else:
                    k.op("dve", lambda e: e.tensor_copy(out=dst, in_=src_ap), reads=[pb], writes=wb_)
            if router is not None:
                pr, prb, _ = psr.next()
                for c in range(16):
                    k.mm(pr[:], lhsT=xT[:, c, :], rhs=wr_sb[:, c, :], start=(c == 0), stop=(c == 15),
                         reads=[xTb, wr_b], writes=[prb], signal=(c == 15))
                k.op("act", lambda e: e.copy(out=lg_sb[:, i, :], in_=pr[:]), reads=[prb], writes=[lg_b])
        if featT_dst is not None:
            k.dma("sp", featT_dst.rearrange("(c p) s -> p c s", p=128), xnT_sb[:], k.dsem("misc"),
                  reads=xnT_bufs, writes=[C.scr_b["xnT"]])
        if xnT_sb is not None or xn_sb is not None:
            pass


def stage_proj(k, C, l, xnT, xnTb):
    w_in = C.w_in[l]
    jobs = []
    for qk in range(2):
        for g in range(3):
            for half in range(2):
                c0 = (qk * 3 + g) * 512 + half * 256
                dst = (C.A_qT if qk == 0 else C.A_kT)[g * 512 + half * 256: g * 512 + half * 256 + 256, :]
                jobs.append(("fm", c0, 256, A_DIL[g], dst, BF16))
    for g in range(3):
        for half in range(2):
            c0 = (6 + g) * 512 + half * 256
            jobs.append(("tm", c0, 256, A_DIL[g], C.A_v[g][:, half * 256:(half + 1) * 256], BF16))
    for c0, n in ((0, 256), (256, 256), (512, 256), (768, 32)):
        jobs.append(("tm", 4608 + c0, n, 1, C.B_tm[:, c0:c0 + n], F32))
    for c0, n in ((0, 256), (256, 256), (512, 128)):
        jobs.append(("tm", 5408 + c0, n, 1, C.C_tm[:, c0:c0 + n], F32))
    jobs.append(("tm", 6048, 128, 1, C.C_v[:, :], BF16))
    jobs.append(("fm", 6176, 256, 1, C.D_qT[0:256, :], BF16))
    jobs.append(("fm", 6432, 256, 1, C.D_qT[256:512, :], BF16))
    jobs.append(("fm", 6688, 128, 1, C.D_kT[:, :], BF16))
    jobs.append(("tm", 6816, 128, 1, C.D_v[:, :], BF16))

    with Scope(k) as sc:
        ws = sc.slots("pw", 3, [128, 16, 256], BF16)
        stf = sc.slots("pof", 2, [128, S], BF16)
        stt32 = sc.slots("pot32", 2, [128, 16, 256], F32)
        stt16 = sc.slots("pot16", 2, [128, 16, 256], BF16)
        ps = Slots(sc, "pps", 4, [128, 512], F32, psum=True)
        wv = w_in.rearrange("(c p) n -> p c n", p=128)
        loaded = {}

        def load(j):
            if j >= len(jobs) or j in loaded:
                return
            _, c0, n, _, _, _ = jobs[j]
            wt, wb, wsem = ws.next()
            k.dma("pool", wt[:, :, 0:n], wv[:, :, c0:c0 + n], wsem, writes=[wb])
            loaded[j] = (wt, wb)

        load(0)
        load(1)
        ev = 0
        for j, (mode, c0, n, dil, dst, dt) in enumerate(jobs):
            load(j + 2)
            wt, wb = loaded[j]
            if mode == "fm":
                for sub in range(n // 128):
                    ot, ob, osem = stf.next()
                    for tg in range(4):
                        p, pb, _ = ps.next()
                        for c in range(16):
                            rhs = fold_view(xnT[:, c, :], dil, tg * 512, 512)
                            k.mm(like_fold(p[:], dil, 512), lhsT=wt[:, c, sub * 128:(sub + 1) * 128], rhs=rhs,
                                 start=(c == 0), stop=(c == 15), reads=[wb] + xnTb, writes=[pb], signal=(c == 15))
                        o_ap = ot[:, tg * 512:(tg + 1) * 512]
                        if ev % 2 == 0:
                            k.op("act", lambda e: e.copy(out=o_ap, in_=p[:]), reads=[pb], writes=[ob])
                        else:
                            k.op("dve", lambda e: e.tensor_copy(out=o_ap, in_=p[:]), reads=[pb], writes=[ob])
                        ev += 1
                    k.dma("sp", dst[sub * 128:(sub + 1) * 128, :], ot[:], osem, reads=[ob], writes=[C.scr_b["proj"]])
            else:
                ot, ob, osem = (stt32 if dt == F32 else stt16).next()
                for i in range(16):
                    p, pb, _ = ps.next()
                    for c in range(16):
                        lhsT = fold_view(xnT[:, c, :], dil, i * 128, 128)
                        k.mm(p[:, 0:n], lhsT=lhsT, rhs=wt[:, c, 0:n], start=(c == 0), stop=(c == 15),
                             reads=[wb] + xnTb, writes=[pb], signal=(c == 15))
                    o_ap = ot[:, i, 0:n]
                    if ev % 2 == 0:
                        k.op("act", lambda e: e.copy(out=o_ap, in_=p[:, 0:n]), reads=[pb], writes=[ob])
                    else:
                        k.op("dve", lambda e: e.tensor_copy(out=o_ap, in_=p[:, 0:n]), reads=[pb], writes=[ob])
                    ev += 1
                k.dma("sp", dst.rearrange("(i p) n -> p i n", p=128), ot[:, :, 0:n], osem, reads=[ob],
                      writes=[C.scr_b["proj"]])


def attn_units(C):
    units = []
    for h in range(8):
        for g in range(3):
            r0 = g * 512 + h * 64
            units.append(dict(q=C.A_qT[r0:r0 + 64, :], k=C.A_kT[r0:r0 + 64, :], v=C.A_v[g][:, h * 64:(h + 1) * 64],
                              dk=64, kind="band", dil=A_DIL[g], bidx=g * 8 + h, scale=0.125, first=(g == 0),
                              last=(g == 2), branch=0, h=h, sink=False))
    for h in range(8):
        units.append(dict(q=C.B_qT[h], k=C.B_kT[h], v=C.B_v[:, h * 64:(h + 1) * 64], dk=96, kind="dense",
                          scale=96 ** -0.5, first=True, last=True, branch=1, h=h, sink=False))
    for h in range(8):
        kv = h // 4
        units.append(dict(q=C.C_qT[h], k=C.C_kT[kv], v=C.C_v[:, kv * 64:(kv + 1) * 64], dk=64, kind="dense",
                          scale=0.125, first=True, last=True, branch=2, h=h, sink=False))
    for h in range(8):
        kv = h // 4
        units.append(dict(q=C.D_qT[h * 64:(h + 1) * 64, :], k=C.D_kT[kv * 64:(kv + 1) * 64, :],
                          v=C.D_v[:, kv * 64:(kv + 1) * 64], dk=64, kind="band", dil=1, bidx=24 + h, scale=0.125,
                          first=True, last=True, branch=3, h=h, sink=True))
    return units


def stage_attn(k, C, l, units):
    LAG = 2
    with Scope(k) as sc:
        bias_sb = sc.sbuf("bias_sb", [128, 32, 384], BF16)
        biasb = Buf("bias")
        k.dma("pool", bias_sb[:], C.c_bias.rearrange("t p n -> p t n"), k.dsem("misc"), writes=[biasb])
        sinkexp = sc.sbuf("sinkexp", [65, 8], F32)
        sinkb = Buf("sink")
        k.dma("sp", sinkexp[64:65, :], C.sink_d[l:l + 1, :], k.dsem("misc"), writes=[sinkb])
        k.op("act", lambda e: e.activation(out=sinkexp[64:65, :], in_=sinkexp[64:65, :], func=AF.Exp),
             reads=[sinkb], writes=[sinkb])
        qs = sc.slots("aq", 2, [96, S], BF16)
        ks = sc.slots("ak", 2, [96, S], BF16)
        vs = sc.slots("av", 2, [128, 16, 65], BF16)
        for i in range(2):
            k.op("dve", lambda e: e.memset(vs.t[i][:, :, 64:65], 1.0), writes=[vs.b[i]])
        pts = sc.slots("apt", 4, [128, 512], BF16)
        accs = sc.slots("aacc", 2, [65, S], F32)
        rden = sc.sbuf("rden", [65, S], F32)
        rdenb = Buf("rden")
        ots = sc.slots("aot", 2, [64, S], BF16)
        ps_s = Slots(sc, "aps", 3, [128, 512], F32, psum=True)
        ps_a = Slots(sc, "apa", 2, [128, 512], F32, psum=True)
        ps_b = Slots(sc, "apb", 2, [64, 512], F32, psum=True)
        ident, identb = C.ident_bf, C.ident_bf_b
        loaded = {}

        def load(ui):
            if ui >= len(units) or ui in loaded:
                return
            u = units[ui]
            qt, qb, qsem = qs.next()
            kt, kb, ksem = ks.next()
            vt, vb, vsem = vs.next()
            dk = u["dk"]
            k.dma("sp", qt[0:dk, :], u["q"], qsem, reads=[C.scr_b["proj"], C.scr_b["qkv"]], writes=[qb])
            k.dma("sp", kt[0:dk, :], u["k"], ksem, reads=[C.scr_b["proj"], C.scr_b["qkv"]], writes=[kb])
            k.dma("sp", vt[:, :, 0:64], u["v"].rearrange("(i p) d -> p i d", p=128), vsem,
                  reads=[C.scr_b["proj"], C.scr_b["qkv"]], writes=[vb])
            loaded[ui] = (qt, qb, kt, kb, vt, vb)

        load(0)
        acc_cur = None
        for ui, u in enumerate(units):
            load(ui + 1)
            qt, qb, kt, kb, vt, vb = loaded.pop(ui)
            dk = u["dk"]
            scale = u["scale"]
            if u["first"]:
                acc_sb, acc_b, _ = accs.next()
            first_grp = u["first"]
            steps = []
            if u["kind"] == "dense":
                for qg in range(4):
                    for kc in range(16):
                        steps.append(("d", qg, kc))
            else:
                dil = u["dil"]
                Ld = S // dil
                nb = Ld // 128
                for z in range(dil):
                    for c in range(nb):
                        steps.append(("b", z, c))
            state = {}
            accst = dict(cb=0, n0=0, acc=None, accb=None)

            def emit_scores(i):
                st = steps[i]
                s, sb, _ = ps_s.next()
                pt, ptb, _ = pts.next()
                if st[0] == "d":
                    _, qg, kc = st
                    k.mm(s[:, :], lhsT=kt[0:dk, kc * 128:(kc + 1) * 128], rhs=qt[0:dk, qg * 512:(qg + 1) * 512],
                         start=True, stop=True, reads=[kb, qb], writes=[sb], signal=True)
                    k.op("act", lambda e: e.activation(out=pt[:, :], in_=s[:, :], func=AF.Exp, scale=scale),
                         reads=[sb], writes=[ptb])
                    state[i] = (pt, ptb, None)
                else:
                    _, z, c = st
                    chunks = [t for t in range(3) if 0 <= c - 1 + t < nb]
                    t0, t1 = chunks[0], chunks[-1] + 1
                    k.mm(s[:, t0 * 128:t1 * 128], lhsT=ident[:], rhs=bias_sb[:, u["bidx"], t0 * 128:t1 * 128],
                         start=True, stop=False, reads=[identb, biasb], writes=[sb], signal=False)
                    for t in chunks:
                        kblk = c - 1 + t
                        k.mm(s[:, t * 128:(t + 1) * 128],
                             lhsT=kt[0:64, z * Ld + kblk * 128: z * Ld + kblk * 128 + 128],
                             rhs=qt[0:64, z * Ld + c * 128: z * Ld + c * 128 + 128],
                             start=False, stop=(t == chunks[-1]), reads=[kb, qb], writes=[sb],
                             signal=(t == chunks[-1]))
                    k.op("act", lambda e: e.activation(out=pt[:, t0 * 128:t1 * 128], in_=s[:, t0 * 128:t1 * 128],
                                                       func=AF.Exp, scale=scale),
                         reads=[sb], writes=[ptb])
                    state[i] = (pt, ptb, chunks)

            def flush(n0, n, dil_):
                acc, accb = accst["acc"], accst["accb"]
                dst = fold_view(acc_sb[0:65, :], dil_, n0, n)
                src = like_fold(acc[0:65, 0:n], dil_, n)
                if first_grp:
                    k.op("act", lambda e: e.copy(out=dst, in_=src), reads=[accb], writes=[acc_b])
                else:
                    k.op("dve", lambda e: e.tensor_tensor(out=dst, in0=src, in1=dst, op=ALU.add),
                         reads=[accb, acc_b], writes=[acc_b])

            def emit_pv(i):
                st = steps[i]
                pt, ptb, chunks = state.pop(i)
                if st[0] == "d":
                    _, qg, kc = st
                    if kc == 0:
                        accst["acc"], accst["accb"], _ = ps_a.next()
                    acc, accb = accst["acc"], accst["accb"]
                    k.mm(acc[0:65, :], lhsT=vt[:, kc, :], rhs=pt[:, :], start=(kc == 0), stop=(kc == 15),
                         reads=[vb, ptb], writes=[accb], signal=(kc == 15))
                    if kc == 15:
                        flush(qg * 512, 512, 1)
                else:
                    _, z, c = st
                    if accst["cb"] == 0:
                        accst["acc"], accst["accb"], _ = ps_a.next()
                        accst["n0"] = z * Ld + c * 128
                    acc, accb = accst["acc"], accst["accb"]
                    cb = accst["cb"]
                    for t in chunks:
                        kblk = c - 1 + t
                        k.mm(acc[0:65, cb * 128:(cb + 1) * 128], lhsT=vt[:, (z * Ld + kblk * 128) // 128, :],
                             rhs=pt[:, t * 128:(t + 1) * 128], start=(t == chunks[0]), stop=(t == chunks[-1]),
                             reads=[vb, ptb], writes=[accb], signal=(t == chunks[-1]))
                    accst["cb"] += 1
                    if accst["cb"] == 4 or i == len(steps) - 1:
                        flush(accst["n0"], accst["cb"] * 128, dil)
                        accst["cb"] = 0

            for i in range(len(steps) + LAG):
                if i < len(steps):
                    emit_scores(i)
                if i - LAG >= 0:
                    emit_pv(i - LAG)

            if u["last"]:
                h = u["h"]
                if u["sink"]:
                    k.op("dve", lambda e: e.tensor_scalar(out=acc_sb[64:65, :], in0=acc_sb[64:65, :],
                                                          scalar1=sinkexp[64:65, h:h + 1], scalar2=None, op0=ALU.add),
                         reads=[acc_b, sinkb], writes=[acc_b])
                k.op("dve", lambda e: e.reciprocal(out=rden[64:65, :], in_=acc_sb[64:65, :]),
                     reads=[acc_b], writes=[rdenb])
                ot, ob, osem = ots.next()
                for qg in range(4):
                    bc, bcb, _ = ps_b.next()
                    k.mm(bc[0:64, :], lhsT=C.ones_f[64:65, 0:64], rhs=rden[64:65, qg * 512:(qg + 1) * 512],
                         start=True, stop=True, reads=[rdenb, C.ones_f_b], writes=[bcb], signal=True)
                    k.op("dve", lambda e: e.tensor_tensor(out=ot[0:64, qg * 512:(qg + 1) * 512],
                                                          in0=acc_sb[0:64, qg * 512:(qg + 1) * 512], in1=bc[0:64, :],
                                                          op=ALU.mult),
                         reads=[acc_b, bcb], writes=[ob])
                r0 = u["branch"] * 512 + h * 64
                k.dma("sp", C.oT_d[r0:r0 + 64, :], ot[0:64, :], osem, reads=[ob], writes=[C.scr_b["oT"]])


def _rstd(k, ss_ap, rs_ap, n, inv, ssb, rsb):
    k.op("dve", lambda e: e.tensor_scalar(out=rs_ap, in0=ss_ap, scalar1=inv, scalar2=EPS, op0=ALU.mult, op1=ALU.add),
         reads=[ssb], writes=[rsb])
    k.op("act", lambda e: e.activation(out=rs_ap, in_=rs_ap, func=AF.Sqrt), reads=[rsb], writes=[rsb])
    k.op("dve", lambda e: e.reciprocal(out=rs_ap, in_=rs_ap), reads=[rsb], writes=[rsb])


def _rope(k, x1, x2, cos, sin, o1, o2, shape, tmp, rb, wb, tb):
    t1, t2 = tmp
    k.op("dve", lambda e: e.tensor_tensor(out=t1, in0=x1, in1=cos, op=ALU.mult), reads=rb, writes=[tb])
    k.op("dve", lambda e: e.tensor_tensor(out=t2, in0=x2, in1=sin, op=ALU.mult), reads=rb, writes=[tb])
    k.op("dve", lambda e: e.tensor_tensor(out=o1, in0=t1, in1=t2, op=ALU.subtract), reads=[tb], writes=wb)
    k.op("dve", lambda e: e.tensor_tensor(out=t1, in0=x1, in1=sin, op=ALU.mult), reads=rb + [tb], writes=[tb])
    k.op("dve", lambda e: e.tensor_tensor(out=t2, in0=x2, in1=cos, op=ALU.mult), reads=rb, writes=[tb])
    k.op("dve", lambda e: e.tensor_tensor(out=o2, in0=t1, in1=t2, op=ALU.add), reads=[tb], writes=wb)


def stage_prepB(k, C, l):
    with Scope(k) as sc:
        ms = k.dsem("misc")
        rope = sc.sbuf("ropeB", [128, 16, 32], F32)
        ropeb = Buf("ropeB")
        k.dma("sp", rope[:], C.c_ropeB.rearrange("(i p) n -> p i n", p=128), ms, writes=[ropeb])
        gq = sc.sbuf("gq", [128, 512], F32)
        gkv = sc.sbuf("gkv", [128, 256], F32)
        gb = Buf("gB")
        k.dma("sp", gq[:], C.g_mla_q[l].partition_broadcast(128), ms, writes=[gb])
        k.dma("sp", gkv[:], C.g_mla_kv[l].partition_broadcast(128), ms, writes=[gb])
        wuq = sc.sbuf("wuq", [128, 4, 768], BF16)
        wukv = sc.sbuf("wukv", [128, 2, 1024], BF16)
        wb = Buf("wB")
        k.dma("pool", wuq[:], C.w_mla_uq[l].rearrange("(c p) n -> p c n", p=128), ms, writes=[wb])
        k.dma("pool", wukv[:], C.w_mla_ukv[l].rearrange("(c p) n -> p c n", p=128), ms, writes=[wb])
        bts = sc.slots("bt", 2, [128, 800], F32)
        junk = sc.sbuf("bjunk", [128, 512], BF16)
        jb = Buf("bjunk")
        ss = sc.sbuf("bss", [128, 32], F32)
        rs = sc.sbuf("brs", [128, 32], F32)
        cn = sc.slots("bcn", 2, [128, 768], BF16)
        cT = sc.slots("bcT", 2, [128, 6, 128], BF16)
        qtm = sc.slots("bqtm", 2, [128, 8, 96], BF16)
        ktm = sc.slots("bktm", 2, [128, 8, 96], BF16)
        tmp1 = sc.sbuf("btmp1", [128, 8, 16], F32)
        tmp2 = sc.sbuf("btmp2", [128, 8, 16], F32)
        tb = Buf("btmp")
        kr = sc.sbuf("bkr", [128, 32], F32)
        krb = Buf("bkr")
        vtm = sc.sbuf("bvtm", [128, 16, 512], BF16)
        vtmb = Buf("bvtm")
        qTs = sc.sbuf("bqTs", [96, 8, S], BF16)
        kTs = sc.sbuf("bkTs", [96, 8, S], BF16)
        qTb = Buf("bqTs")
        kTb = Buf("bkTs")
        pt_in = Slots(sc, "bpi", 1, [128, 1024], BF16, psum=True)
        psq = Slots(sc, "bpq", 1, [128, 512], F32, psum=True)
        psqr = Slots(sc, "bpqr", 1, [128, 256], F32, psum=True)
        pskv = Slots(sc, "bpkv", 1, [128, 512], F32, psum=True)
        psv = Slots(sc, "bpv", 1, [128, 512], F32, psum=True)
        pt_q = Slots(sc, "bptq", 1, [96, 1024], BF16, psum=True)
        pt_k = Slots(sc, "bptk", 1, [96, 1024], BF16, psum=True)
        ident, identb = C.ident_bf, C.ident_bf_b
        for i in range(16):
            bt, bb, bsem = bts.next()
            k.dma("sp", bt[:], C.B_tm[i * 128:(i + 1) * 128, :], bsem, reads=[C.scr_b["proj"]], writes=[bb])
            ssb, rsb = Buf("bss"), Buf("brs")
            k.op("act", lambda e: e.activation(out=junk[:, 0:512], in_=bt[:, 0:512], func=AF.Square,
                                               accum_out=ss[:, 2 * i:2 * i + 1]), reads=[bb], writes=[jb, ssb])
            k.op("act", lambda e: e.activation(out=junk[:, 0:256], in_=bt[:, 512:768], func=AF.Square,
                                               accum_out=ss[:, 2 * i + 1:2 * i + 2]), reads=[bb], writes=[jb, ssb])
            k.op("dve", lambda e: e.tensor_scalar(out=ss[:, 2 * i:2 * i + 1], in0=ss[:, 2 * i:2 * i + 1], scalar1=0.5,
                                                  scalar2=None, op0=ALU.mult), reads=[ssb], writes=[ssb])
            _rstd(k, ss[:, 2 * i:2 * i + 2], rs[:, 2 * i:2 * i + 2], 2, 1.0 / 256, ssb, rsb)
            cnt, cnb, _ = cn.next()
            k.op("dve", lambda e: e.scalar_tensor_tensor(out=cnt[:, 0:512], in0=bt[:, 0:512], scalar=rs[:, 2 * i:2 * i + 1],
                                                         in1=gq[:], op0=ALU.mult, op1=ALU.mult),
                 reads=[bb, rsb, gb], writes=[cnb])
            k.op("dve", lambda e: e.scalar_tensor_tensor(out=cnt[:, 512:768], in0=bt[:, 512:768],
                                                         scalar=rs[:, 2 * i + 1:2 * i + 2], in1=gkv[:], op0=ALU.mult,
                                                         op1=ALU.mult),
                 reads=[bb, rsb, gb], writes=[cnb])
            pin, pinb, _ = pt_in.next()
            for c in range(6):
                k.tr(pin[:, c * 128:(c + 1) * 128], cnt[:, c * 128:(c + 1) * 128], ident[:], reads=[cnb, identb],
                     writes=[pinb], signal=(c == 5))
            ct, ctb, _ = cT.next()
            k.op("act", lambda e: e.copy(out=ct[:], in_=pin[:, 0:768].rearrange("p (c t) -> p c t", t=128)),
                 reads=[pinb], writes=[ctb])
            pq, pqb, _ = psq.next()
            pqr, pqrb, _ = psqr.next()
            wuqv = wuq[:].rearrange("p c (h d) -> p c h d", d=96)
            for kc in range(4):
                k.mm(pq[:, :].rearrange("p (h d) -> p h d", d=64), lhsT=ct[:, kc, :], rhs=wuqv[:, kc, :, 0:64],
                     start=(kc == 0), stop=(kc == 3), reads=[ctb, wb], writes=[pqb], signal=(kc == 3))
            for kc in range(4):
                k.mm(pqr[:, :].rearrange("p (h d) -> p h d", d=32), lhsT=ct[:, kc, :], rhs=wuqv[:, kc, :, 64:96],
                     start=(kc == 0), stop=(kc == 3), reads=[ctb, wb], writes=[pqrb], signal=(kc == 3))
            pkv, pkvb, _ = pskv.next()
            pv, pvb, _ = psv.next()
            wukvv = wukv[:].rearrange("p c (h d) -> p c h d", d=128)
            for kc in range(2):
                k.mm(pkv[:, :].rearrange("p (h d) -> p h d", d=64), lhsT=ct[:, 4 + kc, :], rhs=wukvv[:, kc, :, 0:64],
                     start=(kc == 0), stop=(kc == 1), reads=[ctb, wb], writes=[pkvb], signal=(kc == 1))
            for kc in range(2):
                k.mm(pv[:, :].rearrange("p (h d) -> p h d", d=64), lhsT=ct[:, 4 + kc, :], rhs=wukvv[:, kc, :, 64:128],
                     start=(kc == 0), stop=(kc == 1), reads=[ctb, wb], writes=[pvb], signal=(kc == 1))
            qt, qtb, _ = qtm.next()
            kt, ktb, _ = ktm.next()
            pqv = pq[:, :].rearrange("p (h d) -> p h d", d=64)
            pqrv = pqr[:, :].rearrange("p (h d) -> p h d", d=32)
            pkvv = pkv[:, :].rearrange("p (h d) -> p h d", d=64)
            pvv = pv[:, :].rearrange("p (h d) -> p h d", d=64)
            k.op("act", lambda e: e.copy(out=qt[:, :, 0:64], in_=pqv[:, :, 0:64]), reads=[pqb], writes=[qtb])
            cosb = rope[:, i, 0:16].unsqueeze(1).to_broadcast([128, 8, 16])
            sinb = rope[:, i, 16:32].unsqueeze(1).to_broadcast([128, 8, 16])
            _rope(k, pqrv[:, :, 0:16], pqrv[:, :, 16:32], cosb, sinb, qt[:, :, 64:80], qt[:, :, 80:96], None,
                  (tmp1[:], tmp2[:]), [pqrb, ropeb], [qtb], tb)
            k.op("act", lambda e: e.copy(out=kt[:, :, 0:64], in_=pkvv[:, :, 0:64]), reads=[pkvb], writes=[ktb])
            k.op("act", lambda e: e.copy(out=vtm[:, i, :], in_=pv[:, :]), reads=[pvb], writes=[vtmb])
            _rope(k, bt[:, 768:784], bt[:, 784:800], rope[:, i, 0:16], rope[:, i, 16:32], kr[:, 0:16], kr[:, 16:32], None,
                  (tmp1[:, 0, :], tmp2[:, 0, :]), [bb, ropeb], [krb], tb)
            k.op("dve", lambda e: e.tensor_copy(out=kt[:, :, 64:96], in_=kr[:].unsqueeze(1).to_broadcast([128, 8, 32])),
                 reads=[krb], writes=[ktb])
            for (src, srcb, pts_, dst, dstb, eng) in ((qt, qtb, pt_q, qTs, qTb, "act"), (kt, ktb, pt_k, kTs, kTb, "dve")):
                po, pob, _ = pts_.next()
                for h in range(8):
                    k.tr(po[0:96, h * 128:(h + 1) * 128], src[:, h, :], ident[:], reads=[srcb, identb], writes=[pob],
                         signal=(h == 7))
                o_ap = dst[0:96, :, i * 128:(i + 1) * 128]
                i_ap = po[0:96, :].rearrange("p (h t) -> p h t", t=128)
                if eng == "act":
                    k.op("act", lambda e: e.copy(out=o_ap, in_=i_ap), reads=[pob], writes=[dstb])
                else:
                    k.op("dve", lambda e: e.tensor_copy(out=o_ap, in_=i_ap), reads=[pob], writes=[dstb])
        st = k.dsem("bstore")
        k.dma("sp", C.B_qT.rearrange("h p s -> p h s"), qTs[:], st, reads=[qTb], writes=[C.scr_b["qkv"]])
        k.dma("sp", C.B_kT.rearrange("h p s -> p h s"), kTs[:], st, reads=[kTb], writes=[C.scr_b["qkv"]])
        k.dma("sp", C.B_v.rearrange("(i p) n -> p i n", p=128), vtm[:], st, reads=[vtmb], writes=[C.scr_b["qkv"]])


def stage_prepC(k, C, l):
    with Scope(k) as sc:
        ms = k.dsem("misc")
        rope = sc.sbuf("ropeC", [128, 16, 64], F32)
        ropeb = Buf("ropeC")
        k.dma("sp", rope[:], C.c_ropeC.rearrange("(i p) n -> p i n", p=128), ms, writes=[ropeb])
        gc = sc.sbuf("gc", [128, 10, 64], F32)
        gb = Buf("gC")
        k.dma("sp", gc[:, 0:8, :], C.g_c_q[l:l + 1, :].partition_broadcast(128).to_broadcast([128, 8, 64]), ms, writes=[gb])
        k.dma("sp", gc[:, 8:10, :], C.g_c_k[l:l + 1, :].partition_broadcast(128).to_broadcast([128, 2, 64]), ms, writes=[gb])
        cts = sc.slots("ct", 2, [128, 640], F32)
        sq = sc.sbuf("csq", [128, 640], F32)
        sqb = Buf("csq")
        xn = sc.sbuf("cxn", [128, 640], F32)
        xnb = Buf("cxn")
        ss = sc.sbuf("css", [128, 160], F32)
        rs = sc.sbuf("crs", [128, 160], F32)
        tmp1 = sc.sbuf("ctmp1", [128, 10, 2, 16], F32)
        tmp2 = sc.sbuf("ctmp2", [128, 10, 2, 16], F32)
        tb = Buf("ctmp")
        qtm = sc.slots("cqtm", 2, [128, 10, 64], BF16)
        qTs = sc.sbuf("cqTs", [64, 10, S], BF16)
        qTb = Buf("cqTs")
        pt_q = Slots(sc, "cptq", 2, [64, 1024], BF16, psum=True)
        pt_k = Slots(sc, "cptk", 2, [64, 256], BF16, psum=True)
        ident, identb = C.ident_bf, C.ident_bf_b
        for i in range(16):
            ct, cb, csem = cts.next()
            k.dma("sp", ct[:], C.C_tm[i * 128:(i + 1) * 128, :], csem, reads=[C.scr_b["proj"]], writes=[cb])
            ssb, rsb = Buf("css"), Buf("crs")
            k.op("dve", lambda e: e.tensor_tensor(out=sq[:], in0=ct[:], in1=ct[:], op=ALU.mult), reads=[cb], writes=[sqb])
            k.op("dve", lambda e: e.tensor_reduce(out=ss[:, 10 * i:10 * i + 10], in_=sq[:].rearrange("p (h d) -> p h d", d=64),
                                                  axis=AX.X, op=ALU.add), reads=[sqb], writes=[ssb])
            _rstd(k, ss[:, 10 * i:10 * i + 10], rs[:, 10 * i:10 * i + 10], 10, 1.0 / 64, ssb, rsb)
            ctv = ct[:].rearrange("p (h d) -> p h d", d=64)
            xnv = xn[:].rearrange("p (h d) -> p h d", d=64)
            k.op("dve", lambda e: e.tensor_tensor(out=xnv, in0=ctv,
                                                  in1=rs[:, 10 * i:10 * i + 10].unsqueeze(2).to_broadcast([128, 10, 64]),
                                                  op=ALU.mult), reads=[cb, rsb], writes=[xnb])
            k.op("dve", lambda e: e.tensor_tensor(out=xnv, in0=xnv, in1=gc[:], op=ALU.mult), reads=[xnb, gb], writes=[xnb])
            qt, qtb, _ = qtm.next()
            x5 = xn[:].rearrange("p (h a t f) -> p h a t f", a=2, t=2, f=16)
            o5 = qt[:].rearrange("p h (a t f) -> p h a t f", a=2, t=2, f=16)
            r4 = rope[:, i, :].rearrange("p (a t f) -> p a t f", a=2, t=2, f=16)
            cosb = r4[:, :, 0, :].unsqueeze(1).to_broadcast([128, 10, 2, 16])
            sinb = r4[:, :, 1, :].unsqueeze(1).to_broadcast([128, 10, 2, 16])
            _rope(k, x5[:, :, :, 0, :], x5[:, :, :, 1, :], cosb, sinb, o5[:, :, :, 0, :], o5[:, :, :, 1, :], None,
                  (tmp1[:], tmp2[:]), [xnb, ropeb], [qtb], tb)
            po, pob, _ = pt_q.next()
            for h in range(8):
                k.tr(po[0:64, h * 128:(h + 1) * 128], qt[:, h, :], ident[:], reads=[qtb, identb], writes=[pob],
                     signal=(h == 7))
            k.op("act", lambda e: e.copy(out=qTs[0:64, 0:8, i * 128:(i + 1) * 128],
                                         in_=po[0:64, :].rearrange("p (h t) -> p h t", t=128)), reads=[pob], writes=[qTb])
            pk, pkb, _ = pt_k.next()
            for h in range(2):
                k.tr(pk[0:64, h * 128:(h + 1) * 128], qt[:, 8 + h, :], ident[:], reads=[qtb, identb], writes=[pkb],
                     signal=(h == 1))
            k.op("act", lambda e: e.copy(out=qTs[0:64, 8:10, i * 128:(i + 1) * 128],
                                         in_=pk[0:64, :].rearrange("p (h t) -> p h t", t=128)), reads=[pkb], writes=[qTb])
        st = k.dsem("bstore")
        k.dma("sp", C.C_qT.rearrange("h p s -> p h s"), qTs[:, 0:8, :], st, reads=[qTb], writes=[C.scr_b["qkv"]])
        k.dma("sp", C.C_kT.rearrange("h p s -> p h s"), qTs[:, 8:10, :], st, reads=[qTb], writes=[C.scr_b["qkv"]])


def stage_merge(k, C, l):
    with Scope(k) as sc:
        ms = k.dsem("misc")
        xnT = sc.sbuf("mxnT", [128, 16, S], BF16)
        xb = Buf("mxnT")
        k.dma("sp", xnT[:], C.xnT_d.rearrange("(c p) s -> p c s", p=128), k.dsem("mld0"), reads=[C.scr_b["xnT"]], writes=[xb])
        oT = sc.sbuf("moT", [128, 16, S], BF16)
        ob = Buf("moT")
        k.dma("sp", oT[:], C.oT_d.rearrange("(c p) s -> p c s", p=128), k.dsem("mld1"), reads=[C.scr_b["oT"]], writes=[ob])
        bg = sc.sbuf("mbg", [128, 4, 16], F32)
        bgb = Buf("mbg")
        k.dma("sp", bg[:], C.b_gate[l].rearrange("i (c p) -> p i c", p=128), ms, writes=[bgb], allow_slow_non_contiguous=True)
        wgs = sc.slots("mwg", 2, [128, 16, 4, 128], BF16)
        wbs = sc.slots("mwb", 2, [128, 4, 4, 128], BF16)
        sgs = sc.slots("msg", 4, [128, 512], F32)
        tmp = sc.slots("mtmp", 2, [128, 512], F32)
        macc = sc.slots("macc", 2, [128, 512], F32)
        sts = sc.slots("mst", 2, [128, S], BF16)
        pg = Slots(sc, "mpg", 3, [128, 512], F32, psum=True)
        pb_ = Slots(sc, "mpb", 3, [128, 512], F32, psum=True)
        wgv = C.w_in[l][:, G0:].rearrange("(c p) (i n) -> p c i n", p=128, i=4)
        wbv = C.w_branch[l].rearrange("i (kc p) n -> p i kc n", p=128)
        loaded = {}

        def load(cc):
            if cc >= 16 or cc in loaded:
                return
            wg, wgb, wgsem = wgs.next()
            wb, wbb, wbsem = wbs.next()
            for i in range(4):
                k.dma("pool", wg[:, :, i, :], wgv[:, :, i, cc * 128:(cc + 1) * 128], wgsem, writes=[wgb])
            for i in range(4):
                k.dma("pool", wb[:, i, :, :], wbv[:, i, :, cc * 128:(cc + 1) * 128], wbsem, writes=[wbb])
            loaded[cc] = (wg, wgb, wb, wbb)

        load(0)
        for cc in range(16):
            load(cc + 1)
            wg, wgb, wb, wbb = loaded.pop(cc)
            st, stb, stsem = sts.next()
            for tg in range(4):
                tsl = slice(tg * 512, (tg + 1) * 512)
                sg_l = []
                for i in range(4):
                    p, pb, _ = pg.next()
                    for c in range(16):
                        k.mm(p[:, :], lhsT=wg[:, c, i, :], rhs=xnT[:, c, tsl], start=(c == 0), stop=(c == 15),
                             reads=[wgb, xb], writes=[pb], signal=(c == 15))
                    sg, sgb, _ = sgs.next()
                    k.op("act", lambda e: e.activation(out=sg[:, :], in_=p[:, :], func=AF.Sigmoid, bias=bg[:, i, cc:cc + 1]),
                         reads=[pb, bgb], writes=[sgb])
                    sg_l.append((sg, sgb))
                m, mb, _ = macc.next()
                for i in range(4):
                    p, pb, _ = pb_.next()
                    for kc in range(4):
                        k.mm(p[:, :], lhsT=wb[:, i, kc, :], rhs=oT[:, i * 4 + kc, tsl], start=(kc == 0), stop=(kc == 3),
                             reads=[wbb, ob], writes=[pb], signal=(kc == 3))
                    sg, sgb = sg_l[i]
                    if i == 0:
                        k.op("dve", lambda e: e.tensor_tensor(out=m[:, :], in0=p[:, :], in1=sg[:, :], op=ALU.mult),
                             reads=[pb, sgb], writes=[mb])
                    else:
                        t, tb, _ = tmp.next()
                        k.op("dve", lambda e: e.tensor_tensor(out=t[:, :], in0=p[:, :], in1=sg[:, :], op=ALU.mult),
                             reads=[pb, sgb], writes=[tb])
                        o_ap = m[:, :] if i < 3 else st[:, tsl]
                        k.op("pool", lambda e: e.tensor_tensor(out=o_ap, in0=m[:, :], in1=t[:, :], op=ALU.add),
                             reads=[mb, tb], writes=[mb] if i < 3 else [stb])
            k.dma("sp", C.mixT_d[cc * 128:(cc + 1) * 128, :], st[:, :], stsem, reads=[stb], writes=[C.scr_b["mixT"]])


def stage_out(k, C, l, h_src):
    with Scope(k) as sc:
        mixT = sc.sbuf("omixT", [128, 16, S], BF16)
        mb = Buf("omixT")
        k.dma("sp", mixT[:], C.mixT_d.rearrange("(c p) s -> p c s", p=128), k.dsem("mld0"), reads=[C.scr_b["mixT"]], writes=[mb])
        ws = sc.slots("ow", 3, [128, 16, 256], BF16)
        hs = sc.slots("oh", 2, [128, 16, 256], F32)
        ps = Slots(sc, "ops", 4, [128, 256], F32, psum=True)
        wv = C.w_out[l].rearrange("(c p) n -> p c n", p=128)
        hv = h_src.rearrange("(i p) n -> p i n", p=128)
        ho = C.h_d.rearrange("(i p) n -> p i n", p=128)
        loaded = {}

        def load(cb):
            if cb >= 8 or cb in loaded:
                return
            w, wb, wsem = ws.next()
            k.dma("pool", w[:], wv[:, :, cb * 256:(cb + 1) * 256], wsem, writes=[wb])
            h, hb, hsem = hs.next()
            k.dma("sp", h[:], hv[:, :, cb * 256:(cb + 1) * 256], hsem, reads=[C.scr_b["h"]], writes=[hb])
            loaded[cb] = (w, wb, h, hb, hsem)

        load(0)
        for cb in range(8):
            load(cb + 1)
            w, wb, h, hb, hsem = loaded.pop(cb)
            for i in range(16):
                p, pb, _ = ps.next()
                for c in range(16):
                    k.mm(p[:, :], lhsT=mixT[:, c, i * 128:(i + 1) * 128], rhs=w[:, c, :], start=(c == 0), stop=(c == 15),
                         reads=[mb, wb], writes=[pb], signal=(c == 15))
                k.op("dve", lambda e: e.tensor_tensor(out=h[:, i, :], in0=p[:, :], in1=h[:, i, :], op=ALU.add),
                     reads=[pb, hb], writes=[hb])
            k.dma("sp", ho[:, :, cb * 256:(cb + 1) * 256], h[:], hsem, reads=[hb], writes=[C.scr_b["h"]])


def moe_routing(k, C, lg, lgb, probs, pb_, mask, mkb, rank, rkb, gm, gmb, upto=99):
    ms = k.dsem("misc")
    with Scope(k) as sc:
        mx = sc.sbuf("emx", [128, 16], F32)
        mxb = Buf("emx")
        identf = sc.sbuf("eidf", [128, 128], F32)
        idfb = Buf("eidf")
        k.dma("sp", identf[:], C.c_ident, ms, writes=[idfb])
        ustr = sc.sbuf("eustr", [128, 128], BF16)
        onesb = sc.sbuf("eones", [128, 128], BF16)
        ub = Buf("eustr")
        k.dma("pool", ustr[:], C.c_ustr, ms, writes=[ub])
        k.dma("pool", onesb[:], C.c_ones, ms, writes=[ub])
        import os
        PRE = os.environ.get("PRE", "")
        k.op("dve", lambda e: e.tensor_reduce(out=mx[:], in_=lg[:], axis=AX.X, op=ALU.max), reads=[lgb], writes=[mxb])
        k.op("dve", lambda e: e.tensor_tensor(out=lg[:], in0=lg[:], in1=mx[:].unsqueeze(2).to_broadcast([128, 16, 16]),
                                              op=ALU.subtract), reads=[lgb, mxb], writes=[lgb])
        if "noexp" not in PRE:
            k.op("act", lambda e: e.activation(out=lg[:], in_=lg[:], func=AF.Exp), reads=[lgb], writes=[lgb])
        k.op("dve", lambda e: e.tensor_reduce(out=mx[:], in_=lg[:], axis=AX.X, op=ALU.add), reads=[lgb], writes=[mxb])
        k.op("dve", lambda e: e.reciprocal(out=mx[:], in_=mx[:]), reads=[mxb], writes=[mxb])
        k.op("dve", lambda e: e.tensor_tensor(out=probs[:], in0=lg[:], in1=mx[:].unsqueeze(2).to_broadcast([128, 16, 16]),
                                              op=ALU.mult), reads=[lgb, mxb], writes=[pb_])
        if upto < 1:
            return
        pT = sc.sbuf("epT", [128, S], F32)[0:16, :]
        work = sc.sbuf("ework", [128, S], F32)[0:16, :]
        mT = sc.sbuf("emT", [128, S], F32)[0:16, :]
        pTb, wkb, mTb = Buf("epT"), Buf("ework"), Buf("emT")
        m8 = sc.sbuf("em8", [128, 8], F32)[0:16, :]
        m8b = Buf("em8")
        ppT = Slots(sc, "eppT", 4, [128, 512], F32, psum=True)
        import os
        NB = int(os.environ.get("NB", "4"))
        for bnk in range(NB):
            p, pb2, _ = ppT.next()
            for j in range(4):
                i = bnk * 4 + j
                if "mm" not in os.environ.get("SKIP", ""):
                    k.mm(p[0:16, j * 128:(j + 1) * 128], lhsT=probs[:, i, :], rhs=identf[:], start=True, stop=True,
                         reads=[pb_, idfb], writes=[pb2], signal=(j == 3))
            if "act" not in os.environ.get("SKIP", ""):
                k.op("act", lambda e: e.copy(out=pT[:, bnk * 512:(bnk + 1) * 512], in_=p[0:16, :]), reads=[pb2], writes=[pTb])
            if "dve" not in os.environ.get("SKIP", ""):
                k.op("dve", lambda e: e.tensor_copy(out=work[:, bnk * 512:(bnk + 1) * 512],
                                                    in_=pT[:, bnk * 512:(bnk + 1) * 512]), reads=[pTb], writes=[wkb])
        if upto < 2:
            return
        for r in range(CAP // 8):
            k.op("dve", lambda e: e.max(out=m8[:], in_=work[:]), reads=[wkb], writes=[m8b])
            if r < CAP // 8 - 1:
                k.op("dve", lambda e: e.match_replace(out=work[:], in_to_replace=m8[:], in_values=work[:], imm_value=-1.0),
                     reads=[wkb, m8b], writes=[wkb])
        k.op("dve", lambda e: e.tensor_scalar(out=mT[:], in0=pT[:], scalar1=m8[:, 7:8], scalar2=None, op0=ALU.is_ge),
             reads=[pTb, m8b], writes=[mTb])
        if upto < 3:
            return
        pm = Slots(sc, "epm", 1, [128, 256], F32, psum=True)
        p, pb2, _ = pm.next()
        for i in range(16):
            k.mm(p[:, i * 16:(i + 1) * 16], lhsT=mT[:, i * 128:(i + 1) * 128], rhs=identf[0:16, 0:16], start=True,
                 stop=True, reads=[mTb, idfb], writes=[pb2], signal=(i == 15))
        k.op("act", lambda e: e.copy(out=mask[:].rearrange("p i e -> p (i e)"), in_=p[:, :]), reads=[pb2], writes=[mkb])
        maskbf = sc.sbuf("emaskbf", [128, 16, 16], BF16)
        mbfb = Buf("emaskbf")
        k.op("dve", lambda e: e.tensor_copy(out=maskbf[:], in_=mask[:]), reads=[mkb], writes=[mbfb])
        k.op("dve", lambda e: e.tensor_tensor(out=gm[:], in0=probs[:], in1=mask[:], op=ALU.mult), reads=[pb_, mkb],
             writes=[gmb])
        if upto < 4:
            return
        pr = Slots(sc, "epr", 1, [128, 256], F32, psum=True)
        p, pb2, _ = pr.next()
        for i in range(16):
            for j in range(i + 1):
                k.mm(p[:, i * 16:(i + 1) * 16], lhsT=(ustr[:] if j == i else onesb[:]), rhs=maskbf[:, j, :],
                     start=(j == 0), stop=(j == i), reads=[ub, mbfb], writes=[pb2], signal=(i == 15 and j == i))
        k.op("act", lambda e: e.copy(out=rank[:].rearrange("p i e -> p (i e)"), in_=p[:, :]), reads=[pb2], writes=[rkb])


def stage_moe(k, C, l, dbg=None):
    with Scope(k) as osc:
        ms = k.dsem("misc")
        xn = osc.sbuf("exn", [128, 16, D_MODEL], BF16)
        xnb = [Buf(f"exn{i}") for i in range(16)]
        lg = osc.sbuf("elg", [128, 16, 16], F32)
        lgb = Buf("elg")
        wr = osc.sbuf("ewr", [128, 16, 16], BF16)
        wrb = Buf("ewr")
        k.dma("pool", wr[:], C.w_router[l].rearrange("(c p) e -> p c e", p=128), ms, writes=[wrb])
        stage_norm(k, C, C.h_d, C.g_ffn[l], xn_sb=xn, xn_bufs=xnb, router=(wr, wrb, lg, lgb))
        probs = osc.sbuf("eprobs", [128, 16, 16], F32)
        mask = osc.sbuf("emask", [128, 16, 16], F32)
        rank = osc.sbuf("erank", [128, 16, 16], F32)
        gm = osc.sbuf("egm", [128, 16, 16], F32)
        pb_, mkb, rkb, gmb = Buf("eprobs"), Buf("emask"), Buf("erank"), Buf("egm")
        iota = osc.sbuf("eiota", [128, 256], F32)
        iob = Buf("eiota")
        k.dma("sp", iota[:], C.c_iota, ms, writes=[iob])
        moe_routing(k, C, lg, lgb, probs, pb_, mask, mkb, rank, rkb, gm, gmb)
        if dbg:
            st = k.dsem("bstore")
            k.dma("sp", C.dbg_probs.rearrange("(i p) e -> p i e", p=128), probs[:], st, reads=[pb_], writes=[C.scr_b["moe"]])
            k.dma("sp", C.dbg_mask.rearrange("(i p) e -> p i e", p=128), mask[:], st, reads=[mkb], writes=[C.scr_b["moe"]])
            k.dma("sp", C.dbg_rank.rearrange("(i p) e -> p i e", p=128), rank[:], st, reads=[rkb], writes=[C.scr_b["moe"]])
        if dbg and 6 in dbg:
            return
        with Scope(k) as sc:
            sels = sc.slots("esel", 2, [128, 16, 256], BF16)
            selg = sc.slots("eselg", 1, [128, 16, 256], BF16)
            sgT = sc.slots("esgT", 1, [128, 2, S], BF16)
            xeTs = sc.slots("exeT", 1, [128, 16, 256], BF16)
            hTs = sc.slots("ehT", 1, [128, 8, 256], BF16)
            sgt = sc.slots("esg", 2, [128, 256], F32)
            yes = sc.slots("eye", 1, [128, 2, D_MODEL], BF16)
            w1s = sc.slots("ew1", 4, [128, 16, 256], BF16)
            wds = sc.slots("ewd", 2, [128, 8, 512], BF16)
            pgt = Slots(sc, "epg", 2, [128, 512], F32, psum=True)
            ptr = Slots(sc, "eptr", 1, [128, 1024], BF16, psum=True)
            pgu = Slots(sc, "epgu", 2, [128, 512], F32, psum=True)
            py = Slots(sc, "epy", 2, [128, 512], F32, psum=True)
            ident, identb = C.ident_bf, C.ident_bf_b
            w1jobs = [(e, fb, gu) for e in range(NE) for fb in range(4) for gu in range(2)]
            wdjobs = [(e, cb) for e in range(NE) for cb in range(4)]
            w1l, wdl = {}, {}

            def load_w1(j):
                if j >= len(w1jobs) or j in w1l:
                    return
                e, fb, gu = w1jobs[j]
                src = (C.w_exp_gate if gu == 0 else C.w_exp_up)[l, e].rearrange("(c p) f -> p c f", p=128)
                w, wb, wsem = w1s.next()
                k.dma("pool", w[:], src[:, :, fb * 256:(fb + 1) * 256], wsem, writes=[wb])
                w1l[j] = (w, wb)

            def load_wd(j):
                if j >= len(wdjobs) or j in wdl:
                    return
                e, cb = wdjobs[j]
                src = C.w_exp_down[l, e].rearrange("(f p) n -> p f n", p=128)
                w, wb, wsem = wds.next()
                k.dma("pool", w[:], src[:, :, cb * 512:(cb + 1) * 512], wsem, writes=[wb])
                wdl[j] = (w, wb)

            load_w1(0)
            load_w1(1)
            load_wd(0)
            for e in range(NE):
                sel, selb, _ = sels.next()
                sg_, sgb_, _ = selg.next()
                for i in range(16):
                    k.op("dve", lambda en: en.tensor_scalar(out=sel[:, i, :], in0=iota[:], scalar1=rank[:, i, e:e + 1],
                                                            scalar2=mask[:, i, e:e + 1], op0=ALU.is_equal, op1=ALU.mult),
                         reads=[iob, rkb, mkb], writes=[selb])
                for i in range(16):
                    k.op("dve", lambda en: en.tensor_scalar(out=sg_[:, i, :], in0=iota[:], scalar1=rank[:, i, e:e + 1],
                                                            scalar2=gm[:, i, e:e + 1], op0=ALU.is_equal, op1=ALU.mult),
                         reads=[iob, rkb, gmb], writes=[sgb_])
                xeT, xeTb, _ = xeTs.next()
                for d2 in range(8):
                    p, pb2, _ = pgt.next()
                    for dd in range(2):
                        dc = d2 * 2 + dd
                        for i in range(16):
                            k.mm(p[:, dd * 256:(dd + 1) * 256], lhsT=xn[:, i, dc * 128:(dc + 1) * 128], rhs=sel[:, i, :],
                                 start=(i == 0), stop=(i == 15), reads=[xnb[i], selb], writes=[pb2],
                                 signal=(i == 15 and dd == 1))
                    k.op("act", lambda en: en.copy(out=xeT[:, d2 * 2:d2 * 2 + 2, :],
                                                   in_=p[:, :].rearrange("p (a c) -> p a c", a=2)),
                         reads=[pb2], writes=[xeTb])
                st_, stb_, stsem = sgT.next()
                for ct in range(2):
                    for half in range(2):
                        p, pb2, _ = ptr.next()
                        for j in range(8):
                            i = half * 8 + j
                            k.tr(p[:, j * 128:(j + 1) * 128], sg_[:, i, ct * 128:(ct + 1) * 128], ident[:],
                                 reads=[sgb_, identb], writes=[pb2], signal=(j == 7))
                        k.op("dve", lambda en: en.tensor_copy(out=st_[:, ct, half * 1024:(half + 1) * 1024], in_=p[:, :]),
                             reads=[pb2], writes=[stb_])
                    k.dma("sp", C.selGT_d[:, :, 2 * e + ct, :].rearrange("i c t -> c i t"),
                          st_[:, ct, :].rearrange("c (i t) -> c i t", t=128), stsem, reads=[stb_], writes=[C.scr_b["moe"]])
                hT, hTb, _ = hTs.next()
                for fb in range(4):
                    j0 = (e * 4 + fb) * 2
                    load_w1(j0 + 2)
                    load_w1(j0 + 3)
                    wg, wgb = w1l.pop(j0)
                    wu, wub = w1l.pop(j0 + 1)
                    for fc in range(2):
                        p, pb2, _ = pgu.next()
                        for (w_, wb_, off) in ((wg, wgb, 0), (wu, wub, 256)):
                            for dc in range(16):
                                k.mm(p[:, off:off + 256], lhsT=w_[:, dc, fc * 128:(fc + 1) * 128], rhs=xeT[:, dc, :],
                                     start=(dc == 0), stop=(dc == 15), reads=[wb_, xeTb], writes=[pb2],
                                     signal=(dc == 15 and off == 256))
                        s_, sb_, _ = sgt.next()
                        k.op("act", lambda en: en.activation(out=s_[:, :], in_=p[:, 0:256], func=AF.Silu), reads=[pb2],
                             writes=[sb_])
                        k.op("dve", lambda en: en.tensor_tensor(out=hT[:, fb * 2 + fc, :], in0=p[:, 256:512], in1=s_[:, :],
                                                                op=ALU.mult), reads=[pb2, sb_], writes=[hTb])
                ye, yeb, yesem = yes.next()
                for cb in range(4):
                    j = e * 4 + cb
                    load_wd(j + 1)
                    wd, wdb = wdl.pop(j)
                    for ct in range(2):
                        p, pb2, _ = py.next()
                        for f in range(8):
                            k.mm(p[:, :], lhsT=hT[:, f, ct * 128:(ct + 1) * 128], rhs=wd[:, f, :], start=(f == 0),
                                 stop=(f == 7), reads=[hTb, wdb], writes=[pb2], signal=(f == 7))
                        if ct == 0:
                            k.op("act", lambda en: en.copy(out=ye[:, ct, cb * 512:(cb + 1) * 512], in_=p[:, :]), reads=[pb2],
                                 writes=[yeb])
                        else:
                            k.op("dve", lambda en: en.tensor_copy(out=ye[:, ct, cb * 512:(cb + 1) * 512], in_=p[:, :]),
                                 reads=[pb2], writes=[yeb])
                k.dma("sp", C.ye_d[:, 2 * e:2 * e + 2, :], ye[:], yesem, reads=[yeb], writes=[C.scr_b["moe"]])
    if dbg and 7 in dbg:
        return
    with Scope(k) as sc:
        yes = sc.slots("sye", 2, [128, 32, 512], BF16)
        sls = sc.slots("ssl", 2, [128, 32, 128], BF16)
        hs = sc.slots("sh", 2, [128, 16, 512], F32)
        ps = Slots(sc, "sps", 3, [128, 512], F32, psum=True)
        hv = C.h_d.rearrange("(i p) n -> p i n", p=128)
        for cb in range(4):
            ye, yeb, yesem = yes.next()
            k.dma("sp", ye[:], C.ye_d[:, :, cb * 512:(cb + 1) * 512], yesem, reads=[C.scr_b["moe"]], writes=[yeb])
            h, hb, hsem = hs.next()
            k.dma("sp", h[:], hv[:, :, cb * 512:(cb + 1) * 512], hsem, reads=[C.scr_b["h"]], writes=[hb])
            for i in range(16):
                sl, slb, slsem = sls.next()
                k.dma("sp", sl[:], C.selGT_d[i], slsem, reads=[C.scr_b["moe"]], writes=[slb])
                p, pb2, _ = ps.next()
                for et in range(32):
                    k.mm(p[:, :], lhsT=sl[:, et, :], rhs=ye[:, et, :], start=(et == 0), stop=(et == 31), reads=[slb, yeb],
                         writes=[pb2], signal=(et == 31))
                k.op("dve", lambda en: en.tensor_tensor(out=h[:, i, :], in0=p[:, :], in1=h[:, i, :], op=ALU.add),
                     reads=[pb2, hb], writes=[hb])
            k.dma("sp", hv[:, :, cb * 512:(cb + 1) * 512], h[:], hsem, reads=[hb], writes=[C.scr_b["h"]])


def stage_final(k, C, yb):
    with Scope(k) as sc:
        hs = sc.slots("fh", 3, [128, D_MODEL], F32)
        os_ = sc.slots("fo", 2, [128, D_MODEL], F32)
        gbc = sc.sbuf("fgbc", [128, D_MODEL], F32)
        gb = Buf("fgbc")
        k.dma("sp", gbc[:], C.g_final.partition_broadcast(128), k.dsem("misc"), writes=[gb])
        junk = sc.sbuf("fjunk", [128, D_MODEL], BF16)
        jb = Buf("fjunk")
        ss = sc.sbuf("fss", [128, 16], F32)
        rs = sc.sbuf("frs", [128, 16], F32)
        for i in range(16):
            ht, hb, hsem = hs.next()
            k.dma("sp", ht[:], C.h_d[i * 128:(i + 1) * 128, :], hsem, reads=[C.scr_b["h"]], writes=[hb])
            ssb, rsb = Buf("fss"), Buf("frs")
            k.op("act", lambda e: e.activation(out=junk[:], in_=ht[:], func=AF.Square, accum_out=ss[:, i:i + 1]),
                 reads=[hb], writes=[jb, ssb])
            _rstd(k, ss[:, i:i + 1], rs[:, i:i + 1], 1, 1.0 / D_MODEL, ssb, rsb)
            ot, ob, osem = os_.next()
            k.op("dve", lambda e: e.scalar_tensor_tensor(out=ot[:], in0=ht[:], scalar=rs[:, i:i + 1], in1=gbc[:],
                                                         op0=ALU.mult, op1=ALU.mult), reads=[hb, rsb, gb], writes=[ob])
            k.dma("sp", C.y[i * 128:(i + 1) * 128, :], ot[:], osem, reads=[ob], writes=[yb])


def alloc_scratch(k, C, dbg):
    kind = "ExternalOutput" if dbg else "Internal"
    C.scr_b = {n: Buf(n) for n in ("h", "xnT", "proj", "qkv", "oT", "mixT", "moe")}
    C.h_d = k.dram("h_d", [S, D_MODEL], F32, kind).ap()
    C.xnT_d = k.dram("xnT_d", [D_MODEL, S], BF16).ap()
    C.A_qT = k.dram("A_qT", [1536, S], BF16, kind).ap()
    C.A_kT = k.dram("A_kT", [1536, S], BF16, kind).ap()
    C.A_v = [k.dram(f"A_v{g}", [S, 512], BF16, kind).ap() for g in range(3)]
    C.B_tm = k.dram("B_tm", [S, 800], F32, kind).ap()
    C.C_tm = k.dram("C_tm", [S, 640], F32, kind).ap()
    C.C_v = k.dram("C_v", [S, 128], BF16, kind).ap()
    C.D_qT = k.dram("D_qT", [512, S], BF16, kind).ap()
    C.D_kT = k.dram("D_kT", [128, S], BF16, kind).ap()
    C.D_v = k.dram("D_v", [S, 128], BF16, kind).ap()
    C.B_qT = k.dram("B_qT", [8, 96, S], BF16, kind).ap()
    C.B_kT = k.dram("B_kT", [8, 96, S], BF16, kind).ap()
    C.B_v = k.dram("B_v", [S, 512], BF16, kind).ap()
    C.C_qT = k.dram("C_qT", [8, 64, S], BF16, kind).ap()
    C.C_kT = k.dram("C_kT", [2, 64, S], BF16, kind).ap()
    C.oT_d = k.dram("oT_d", [2048, S], BF16, kind).ap()
    C.mixT_d = k.dram("mixT_d", [D_MODEL, S], BF16, kind).ap()
    C.selGT_d = k.dram("selGT_d", [16, 128, 32, 128], BF16).ap()
    C.ye_d = k.dram("ye_d", [128, 32, D_MODEL], BF16, kind).ap()
    if dbg:
        C.dbg_probs = k.dram("dbg_probs", [S, 16], F32, kind).ap()
        C.dbg_mask = k.dram("dbg_mask", [S, 16], F32, kind).ap()
        C.dbg_rank = k.dram("dbg_rank", [S, 16], F32, kind).ap()


def build(dbg=None):
    k = K()
    C = Ctx()
    C.x = k.dram("x", [S, D_MODEL], F32, "ExternalInput").ap()
    C.w_in = k.dram("w_in", [DEPTH, D_MODEL, N_IN], F32, "ExternalInput").ap()
    C.g_attn = k.dram("g_attn_norm", [DEPTH, D_MODEL], F32, "ExternalInput").ap()
    C.c_ident = k.dram("c_ident", [128, 128], F32, "ExternalInput").ap()
    C.c_bias = k.dram("c_bias", [32, 128, 384], F32, "ExternalInput").ap()
    C.c_ones = k.dram("c_ones", [128, 128], F32, "ExternalInput").ap()
    C.sink_d = k.dram("sink_d", [DEPTH, 8], F32, "ExternalInput").ap()
    C.g_mla_q = k.dram("g_mla_q", [DEPTH, 512], F32, "ExternalInput").ap()
    C.b_gate = k.dram("b_gate", [DEPTH, 4, D_MODEL], F32, "ExternalInput").ap()
    C.g_ffn = k.dram("g_ffn_norm", [DEPTH, D_MODEL], F32, "ExternalInput").ap()
    C.g_final = k.dram("g_final", [D_MODEL], F32, "ExternalInput").ap()
    C.w_router = k.dram("w_router", [DEPTH, D_MODEL, NE], F32, "ExternalInput").ap()
    C.w_exp_gate = k.dram("w_exp_gate", [DEPTH, NE, D_MODEL, EFF], F32, "ExternalInput").ap()
    C.w_exp_up = k.dram("w_exp_up", [DEPTH, NE, D_MODEL, EFF], F32, "ExternalInput").ap()
    C.w_exp_down = k.dram("w_exp_down", [DEPTH, NE, EFF, D_MODEL], F32, "ExternalInput").ap()
    C.c_iota = k.dram("c_iota", [128, 256], F32, "ExternalInput").ap()
    C.c_ustr = k.dram("c_ustr", [128, 128], F32, "ExternalInput").ap()
    C.w_branch = k.dram("w_branch", [DEPTH, 4, 512, D_MODEL], F32, "ExternalInput").ap()
    C.w_out = k.dram("w_out", [DEPTH, D_MODEL, D_MODEL], F32, "ExternalInput").ap()
    C.g_mla_kv = k.dram("g_mla_kv", [DEPTH, 256], F32, "ExternalInput").ap()
    C.w_mla_uq = k.dram("w_mla_uq", [DEPTH, 512, 768], F32, "ExternalInput").ap()
    C.w_mla_ukv = k.dram("w_mla_ukv", [DEPTH, 256, 1024], F32, "ExternalInput").ap()
    C.g_c_q = k.dram("g_c_q", [DEPTH, 64], F32, "ExternalInput").ap()
    C.g_c_k = k.dram("g_c_k", [DEPTH, 64], F32, "ExternalInput").ap()
    C.c_ropeB = k.dram("c_ropeB", [S, 32], F32, "ExternalInput").ap()
    C.c_ropeC = k.dram("c_ropeC", [S, 64], F32, "ExternalInput").ap()
    C.y = k.dram("y", [S, D_MODEL], F32, "ExternalOutput").ap()
    alloc_scratch(k, C, dbg)
    yb = Buf("y")

    with Scope(k) as top:
        C.ident_bf = top.sbuf("ident_bf", [128, 128], BF16)
        C.ident_bf_b = Buf("ident_bf")
        k.dma("pool", C.ident_bf[:], C.c_ident, k.dsem("misc"), writes=[C.ident_bf_b])
        C.ones_f = top.sbuf("ones_f", [128, 128], F32)
        C.ones_f_b = Buf("ones_f")
        k.dma("sp", C.ones_f[:], C.c_ones, k.dsem("misc"), writes=[C.ones_f_b])
        for l in range(1 if dbg else DEPTH):
            with Scope(k) as lsc:
                xnT = lsc.sbuf("xnT", [128, 16, S], BF16)
                xnTb = [Buf(f"xnT{i}") for i in range(16)]
                stage_norm(k, C, C.x if l == 0 else C.h_d, C.g_attn[l], xnT_sb=xnT, xnT_bufs=xnTb, featT_dst=C.xnT_d)
                stage_proj(k, C, l, xnT, xnTb)
            if not dbg or 1 in dbg:
                stage_prepB(k, C, l)
            if not dbg or 2 in dbg:
                stage_prepC(k, C, l)
            units = attn_units(C)
            if dbg:
                units = [u for u in units if u["branch"] in dbg]
            stage_attn(k, C, l, units)
            if not dbg or 4 in dbg:
                stage_merge(k, C, l)
                stage_out(k, C, l, C.x if l == 0 else C.h_d)
            if dbg and 4 not in dbg:
                k.dma("sp", C.h_d, C.x, k.dsem("misc"), writes=[C.scr_b["h"]])
            if not dbg or 5 in dbg:
                stage_moe(k, C, l, dbg)
        if not dbg or 8 in dbg:
            stage_final(k, C, yb)
        else:
            k.dma('sp', C.y[0:128, :], C.x[0:128, :], k.dsem('out'), writes=[yb])
        k.barrier()
    return k


def host_consts():
    c = {"c_ident": np.eye(128, dtype=np.float32), "c_ones": np.ones((128, 128), np.float32)}
    c["c_iota"] = np.tile(np.arange(256, dtype=np.float32)[None, :], (128, 1))
    c["c_ustr"] = (np.arange(128)[:, None] < np.arange(128)[None, :]).astype(np.float32)
    bias = np.zeros((32, 128, 384), np.float32)
    kk = np.arange(128)[:, None].astype(np.float64)
    qq = np.arange(128)[None, :].astype(np.float64)
    dists = [qq - kk + 128, np.abs(qq - kk), kk + 128 - qq]
    for idx in range(32):
        if idx < 24:
            g, h = divmod(idx, 8)
            slope = 2.0 ** (-(h + 1)) * A_DIL[g]
            W = 64
        else:
            h = idx - 24
            slope = 2.0 ** (-(h + 1))
            W = 128
        for t in range(3):
            d = dists[t]
            bias[idx, :, t * 128:(t + 1) * 128] = np.where(d <= W, -slope * d * 8.0, -240000.0)
    c["c_bias"] = bias
    freqs = 10000.0 ** (-np.arange(0, 32, 2, dtype=np.float32) / 32)
    pos = np.arange(S, dtype=np.float32)
    ang = pos[:, None] * freqs[None, :]
    c["c_ropeB"] = np.concatenate([np.cos(ang), np.sin(ang)], axis=1).astype(np.float32)
    rows = np.repeat(np.arange(S // 64), 64).astype(np.float32)
    cols = np.tile(np.arange(64), S // 64).astype(np.float32)
    ar = rows[:, None] * freqs[None, :]
    ac = cols[:, None] * freqs[None, :]
    c["c_ropeC"] = np.concatenate([np.cos(ar), np.sin(ar), np.cos(ac), np.sin(ac)], axis=1).astype(np.float32)
    return c


def kernel(**inputs):
    k = build()
    consts = host_consts()
    in_maps = []
    for b in range(8):
        m = {"x": np.ascontiguousarray(inputs["x"][b])}
        for n in WEIGHT_NAMES:
            m[n] = np.ascontiguousarray(inputs[n])
        m.update(consts)
        in_maps.append(m)
    res = run_bass_kernel_spmd(k.nc, in_maps, core_ids=list(range(8)))
    return np.stack([r["y"] for r in res.results], axis=0)
```

```python
import numpy as np
from contextlib import ExitStack
import concourse.bass as bass
import concourse.mybir as mybir
from concourse.bass_utils import run_bass_kernel_spmd

F32 = mybir.dt.float32
BF16 = mybir.dt.bfloat16
AF = mybir.ActivationFunctionType
ALU = mybir.AluOpType
AX = mybir.AxisListType

D_MODEL = 2048
S = 2048
DEPTH = 2
N_IN = 15136
G0 = 6944
EPS = 1e-6
NE = 16
EFF = 1024
CAP = 256
A_DIL = (1, 4, 16)
WEIGHT_NAMES = ["w_in", "g_attn_norm", "sink_d", "g_mla_q", "g_mla_kv", "w_mla_uq", "w_mla_ukv", "g_c_q", "g_c_k", "b_gate", "w_branch", "w_out", "g_ffn_norm", "g_final", "w_router", "w_exp_gate", "w_exp_up", "w_exp_down"]


class Sem:
    def __init__(self, handle, name):
        self.h = handle
        self.name = name
        self.count = 0
        self.dma = name.startswith("d_")


class Buf:
    __slots__ = ("name", "w", "r")

    def __init__(self, name):
        self.name = name
        self.w = None
        self.r = {}


class Q:
    def __init__(self, name, eng, sem):
        self.name = name
        self.eng = eng
        self.sem = sem
        self.waited = {}


class K:
    def __init__(self):
        self.nc = bass.Bass("TRN2", target_bir_lowering=False)
        self.es = ExitStack()
        nc = self.nc
        self.sems = []
        self.q = {}
        for name, eng in (("pe", nc.tensor), ("act", nc.scalar), ("dve", nc.vector),
                          ("pool", nc.gpsimd), ("sp", nc.sync)):
            self.q[name] = Q(name, eng, self.sem("q_" + name))
        self.ninst = 0
        self.uid = 0
        self.slot_sem_idx = 0
        self.sem_pool = {}

    def sem(self, name):
        s = Sem(self.es.enter_context(self.nc.semaphore(name)), name)
        self.sems.append(s)
        return s

    def dsem(self, name):
        if name not in self.sem_pool:
            self.sem_pool[name] = self.sem("d_" + name)
        return self.sem_pool[name]

    def dram(self, name, shape, dt, kind="Internal"):
        return self.nc.dram_tensor(name, list(shape), dt, kind=kind)

    def _deps(self, q, reads, writes):
        deps = {}

        def add(s, v, raw):
            if s is q.sem:
                if not raw or q.name == "pe" or q.name == "sp":
                    return
            if deps.get(s, 0) < v:
                deps[s] = v

        for b in reads:
            if b.w is not None:
                add(b.w[0], b.w[1], True)
        for b in writes:
            if b.w is not None:
                add(b.w[0], b.w[1], False)
            for s, v in b.r.items():
                add(s, v, False)
        for s, v in deps.items():
            if s.dma:
                v = s.count
            if q.waited.get(s, 0) >= v:
                continue
            assert v <= s.count, f"wait on unsignalled token {s.name} {v}>{s.count} from {q.name}"
            q.eng.wait_ge(s.h, v)
            q.waited[s] = v

    def op(self, E, emit, reads=(), writes=(), signal=True):
        q = self.q[E]
        self._deps(q, reads, writes)
        ins = emit(q.eng)
        self.ninst += 1
        if signal:
            q.sem.count += 1
            ins.then_inc(q.sem.h, 1)
            v = q.sem.count
        else:
            v = q.sem.count + 1
        for b in reads:
            b.r[q.sem] = v
        for b in writes:
            b.w = (q.sem, v)
            b.r = {}
        return ins

    def dma(self, E, out, in_, sem, reads=(), writes=(), **kw):
        q = self.q[E]
        self._deps(q, reads, writes)
        ins = q.eng.dma_start(out=out, in_=in_, **kw)
        self.ninst += 1
        sem.count += 16
        ins.then_inc(sem.h, 16)
        for b in reads:
            b.r[sem] = sem.count
        for b in writes:
            b.w = (sem, sem.count)
            b.r = {}
        return ins

    def barrier(self):
        for q in self.q.values():
            for s in self.sems:
                if s is q.sem or s.count == 0:
                    continue
                if q.waited.get(s, 0) >= s.count:
                    continue
                q.eng.wait_ge(s.h, s.count)
                q.waited[s] = s.count

    def finish(self):
        self.barrier()
        self.nc.all_engine_barrier()
        self.nc.clear_and_free_semaphores([s.h for s in self.sems])
        self.nc.all_engine_barrier()

    def mm(self, out, lhsT, rhs, start, stop, reads, writes, signal):
        return self.op("pe", lambda e: e.matmul(out, lhsT=lhsT, rhs=rhs, start=start, stop=stop,
                                                skip_group_check=True),
                       reads=reads, writes=writes, signal=signal)

    def tr(self, out, in_, ident, reads, writes, signal):
        return self.op("pe", lambda e: e.transpose(out, in_, ident), reads=reads, writes=writes, signal=signal)


class Scope:
    def __init__(self, k):
        self.k = k
        self.es = ExitStack()

    def __enter__(self):
        self.sem_idx0 = self.k.slot_sem_idx
        return self

    def __exit__(self, *a):
        self.k.barrier()
        self.es.close()
        self.k.slot_sem_idx = self.sem_idx0
        return False

    def sbuf(self, name, shape, dt):
        self.k.uid += 1
        return self.es.enter_context(self.k.nc.sbuf_tensor(f"{name}_{self.k.uid}", list(shape), dt))

    def psum(self, name, shape, dt=F32):
        self.k.uid += 1
        return self.es.enter_context(self.k.nc.psum_tensor(f"{name}_{self.k.uid}", list(shape), dt))

    def slots(self, name, n, shape, dt):
        return Slots(self, name, n, shape, dt)


class Slots:
    def __init__(self, sc, name, n, shape, dt, psum=False):
        self.n = n
        if psum:
            self.t = [sc.psum(f"{name}{i}", shape, dt) for i in range(n)]
        else:
            self.t = [sc.sbuf(f"{name}{i}", shape, dt) for i in range(n)]
        self.b = [Buf(f"{name}{i}") for i in range(n)]
        if psum:
            self.s = [None] * n
        else:
            self.s = []
            for i in range(n):
                self.s.append(sc.k.dsem(f"S{sc.k.slot_sem_idx}"))
                sc.k.slot_sem_idx += 1
        self.i = 0

    def next(self):
        i = self.i % self.n
        self.i += 1
        return self.t[i], self.b[i], self.s[i]


def fold_view(ap2d, dil, n0, n):
    Ld = S // dil
    if dil == 1:
        return ap2d[:, n0:n0 + n]
    v = ap2d.rearrange("p (j r) -> p r j", r=dil)
    if n <= Ld:
        r, j0 = divmod(n0, Ld)
        assert j0 + n <= Ld
        return v[:, r, j0:j0 + n]
    assert n % Ld == 0 and n0 % Ld == 0
    return v[:, n0 // Ld:(n0 + n) // Ld, :]


def like_fold(ap2d, dil, n):
    Ld = S // dil
    if dil == 1 or n <= Ld:
        return ap2d
    return ap2d.rearrange("p (a j) -> p a j", j=Ld)


class Ctx:
    pass


def stage_norm(k, C, src, g_row, tok_major_dst=None, featT_dst=None, xnT_sb=None, xnT_bufs=None,
               xn_sb=None, xn_bufs=None, router=None):
    with Scope(k) as sc:
        hs = sc.slots("nh", 3, [128, D_MODEL], F32)
        gbc = sc.sbuf("gbc", [128, D_MODEL], F32)
        gb = Buf("gbc")
        k.dma("sp", gbc[:], g_row.partition_broadcast(128), k.dsem("misc"), writes=[gb])
        junk = sc.sbuf("junk", [128, D_MODEL], BF16)
        jb = Buf("junk")
        ss = sc.sbuf("ss", [128, 16], F32)
        rs = sc.sbuf("rs", [128, 16], F32)
        ssb = [Buf(f"ss{i}") for i in range(16)]
        rsb = [Buf(f"rs{i}") for i in range(16)]
        if xn_sb is None:
            xns = sc.slots("xnt", 2, [128, D_MODEL], BF16)
        pst = Slots(sc, "npt", 2, [128, 1024], BF16, psum=True)
        if router is not None:
            wr_sb, wr_b, lg_sb, lg_b = router
            psr = Slots(sc, "npr", 2, [128, 16], F32, psum=True)
            xts = sc.slots("nxT", 2, [128, 16, 128], BF16)
        ident, identb = C.ident_bf, C.ident_bf_b
        srcv = src.rearrange("(i p) d -> i p d", p=128)
        for i in range(16):
            ht, hb, hsem = hs.next()
            k.dma("sp", ht[:], srcv[i], hsem, writes=[hb])
            k.op("act", lambda e: e.activation(out=junk[:], in_=ht[:], func=AF.Square, accum_out=ss[:, i:i + 1]),
                 reads=[hb], writes=[jb, ssb[i]])
            k.op("dve", lambda e: e.tensor_scalar(out=rs[:, i:i + 1], in0=ss[:, i:i + 1], scalar1=1.0 / D_MODEL,
                                                  scalar2=EPS, op0=ALU.mult, op1=ALU.add),
                 reads=[ssb[i]], writes=[rsb[i]])
            k.op("act", lambda e: e.activation(out=rs[:, i:i + 1], in_=rs[:, i:i + 1], func=AF.Sqrt),
                 reads=[rsb[i]], writes=[rsb[i]])
            k.op("dve", lambda e: e.reciprocal(out=rs[:, i:i + 1], in_=rs[:, i:i + 1]),
                 reads=[rsb[i]], writes=[rsb[i]])
            if xn_sb is None:
                xt, xb, _ = xns.next()
                xt_ap = xt[:]
            else:
                xt_ap = xn_sb[:, i, :]
                xb = xn_bufs[i]
            k.op("dve", lambda e: e.scalar_tensor_tensor(out=xt_ap, in0=ht[:], scalar=rs[:, i:i + 1], in1=gbc[:],
                                                         op0=ALU.mult, op1=ALU.mult),
                 reads=[hb, rsb[i], gb], writes=[xb])
            if xnT_sb is None and router is None:
                continue
            if router is not None:
                xT, xTb, _ = xts.next()
            for half in range(2):
                pt, pb, _ = pst.next()
                for j in range(8):
                    c = half * 8 + j
                    k.tr(pt[:, j * 128:(j + 1) * 128], xt_ap[:, c * 128:(c + 1) * 128], ident[:],
                         reads=[xb, identb], writes=[pb], signal=(j == 7))
                src_ap = pt[:].rearrange("p (c t) -> p c t", t=128)
                if xnT_sb is not None:
                    dst = xnT_sb[:, half * 8:(half + 1) * 8, i * 128:(i + 1) * 128]
                    wb_ = [xnT_bufs[i]]
                else:
                    dst = xT[:, half * 8:(half + 1) * 8, :]
                    wb_ = [xTb]
                if half == 0:
                    k.op("act", lambda e: e.copy(out=dst, in_=src_ap), reads=[pb], writes=wb_)
                else:
                    k.op("dve", lambda e: e.tensor_copy(out=dst, in_=src_ap), reads=[pb], writes=wb_)
            if router is not None:
                pr, prb, _ = psr.next()
                for c in range(16):
                    k.mm(pr[:], lhsT=xT[:, c, :], rhs=wr_sb[:, c, :], start=(c == 0), stop=(c == 15),
                         reads=[xTb, wr_b], writes=[prb], signal=(c == 15))
                k.op("act", lambda e: e.copy(out=lg_sb[:, i, :], in_=pr[:]), reads=[prb], writes=[lg_b])
        if featT_dst is not None:
            k.dma("sp", featT_dst.rearrange("(c p) s -> p c s", p=128), xnT_sb[:], k.dsem("misc"),
                  reads=xnT_bufs, writes=[C.scr_b["xnT"]])
        if xnT_sb is not None or xn_sb is not None:
            pass


def stage_proj(k, C, l, xnT, xnTb):
    w_in = C.w_in[l]
    jobs = []
    for qk in range(2):
        for g in range(3):
            c0 = (qk * 3 + g) * 512
            dst = (C.A_qT if qk == 0 else C.A_kT)[g * 512:(g + 1) * 512, :]
            jobs.append(("fm", c0, 512, A_DIL[g], dst, BF16))
    for g in range(3):
        jobs.append(("tm", (6 + g) * 512, 512, A_DIL[g], C.A_v[g][:, :], BF16))
    for c0, n in ((0, 512), (512, 288)):
        jobs.append(("tm", 4608 + c0, n, 1, C.B_tm[:, c0:c0 + n], F32))
    for c0, n in ((0, 512), (512, 128)):
        jobs.append(("tm", 5408 + c0, n, 1, C.C_tm[:, c0:c0 + n], F32))
    jobs.append(("tm", 6048, 128, 1, C.C_v[:, :], BF16))
    jobs.append(("fm", 6176, 512, 1, C.D_qT[:, :], BF16))
    jobs.append(("fm", 6688, 128, 1, C.D_kT[:, :], BF16))
    jobs.append(("tm", 6816, 128, 1, C.D_v[:, :], BF16))

    with Scope(k) as sc:
        ws = sc.slots("pw", 3, [128, 16, 512], BF16)
        stf = sc.slots("pof", 2, [128, S], BF16)
        stt32 = sc.slots("pot32", 3, [128, 512], F32)
        stt16 = sc.slots("pot16", 3, [128, 512], BF16)
        ps = Slots(sc, "pps", 4, [128, 512], F32, psum=True)
        wv = w_in.rearrange("(c p) n -> p c n", p=128)
        loaded = {}

        def load(j):
            if j >= len(jobs) or j in loaded:
                return
            _, c0, n, _, _, _ = jobs[j]
            wt, wb, wsem = ws.next()
            k.dma("pool", wt[:, :, 0:n], wv[:, :, c0:c0 + n], wsem, writes=[wb])
            loaded[j] = (wt, wb)

        load(0)
        load(1)
        ev = 0
        for j, (mode, c0, n, dil, dst, dt) in enumerate(jobs):
            load(j + 2)
            wt, wb = loaded[j]
            if mode == "fm":
                for sub in range(n // 128):
                    ot, ob, osem = stf.next()
                    for tg in range(4):
                        p, pb, _ = ps.next()
                        for c in range(16):
                            rhs = fold_view(xnT[:, c, :], dil, tg * 512, 512)
                            k.mm(like_fold(p[:], dil, 512), lhsT=wt[:, c, sub * 128:(sub + 1) * 128], rhs=rhs,
                                 start=(c == 0), stop=(c == 15), reads=[wb] + xnTb, writes=[pb], signal=(c == 15))
                        o_ap = ot[:, tg * 512:(tg + 1) * 512]
                        if ev % 2 == 0:
                            k.op("act", lambda e: e.copy(out=o_ap, in_=p[:]), reads=[pb], writes=[ob])
                        else:
                            k.op("dve", lambda e: e.tensor_copy(out=o_ap, in_=p[:]), reads=[pb], writes=[ob])
                        ev += 1
                    k.dma("sp", dst[sub * 128:(sub + 1) * 128, :], ot[:], osem, reads=[ob], writes=[C.scr_b["proj"]])
            else:
                for i in range(16):
                    ot, ob, osem = (stt32 if dt == F32 else stt16).next()
                    p, pb, _ = ps.next()
                    for c in range(16):
                        lhsT = fold_view(xnT[:, c, :], dil, i * 128, 128)
                        k.mm(p[:, 0:n], lhsT=lhsT, rhs=wt[:, c, 0:n], start=(c == 0), stop=(c == 15),
                             reads=[wb] + xnTb, writes=[pb], signal=(c == 15))
                    o_ap = ot[:, 0:n]
                    if ev % 2 == 0:
                        k.op("act", lambda e: e.copy(out=o_ap, in_=p[:, 0:n]), reads=[pb], writes=[ob])
                    else:
                        k.op("dve", lambda e: e.tensor_copy(out=o_ap, in_=p[:, 0:n]), reads=[pb], writes=[ob])
                    ev += 1
                    k.dma("sp", dst[i * 128:(i + 1) * 128, :], ot[:, 0:n], osem, reads=[ob], writes=[C.scr_b["proj"]])


def attn_units(C):
    units = []
    for h in range(8):
        for g in range(3):
            r0 = g * 512 + h * 64
            units.append(dict(q=C.A_qT[r0:r0 + 64, :], k=C.A_kT[r0:r0 + 64, :], v=("A%d" % g, h),
                              dk=64, kind="band", dil=A_DIL[g], bidx=g * 8 + h, scale=0.125, first=(g == 0),
                              last=(g == 2), branch=0, h=h, sink=False))
    for h in range(8):
        units.append(dict(q=C.B_qT[h], k=C.B_kT[h], v=("B", h), dk=96, kind="dense",
                          scale=96 ** -0.5, first=True, last=True, branch=1, h=h, sink=False))
    for h in range(8):
        kv = h // 4
        units.append(dict(q=C.C_qT[h], k=C.C_kT[kv], v=("C", kv), dk=64, kind="dense",
                          scale=0.125, first=True, last=True, branch=2, h=h, sink=False))
    for h in range(8):
        kv = h // 4
        units.append(dict(q=C.D_qT[h * 64:(h + 1) * 64, :], k=C.D_kT[kv * 64:(kv + 1) * 64, :],
                          v=("D", kv), dk=64, kind="band", dil=1, bidx=24 + h, scale=0.125,
                          first=True, last=True, branch=3, h=h, sink=True))
    return units


def stage_attn(k, C, l, units):
    LAG = 2
    with Scope(k) as sc:
        bias_sb = sc.sbuf("bias_sb", [128, 32, 384], BF16)
        biasb = Buf("bias")
        k.dma("pool", bias_sb[:], C.c_bias.rearrange("t p n -> p t n"), k.dsem("misc"), writes=[biasb])
        sinkexp = sc.sbuf("sinkexp", [65, 8], F32)
        sinkb = Buf("sink")
        k.dma("sp", sinkexp[64:65, :], C.sink_d[l:l + 1, :], k.dsem("misc"), writes=[sinkb])
        k.op("act", lambda e: e.activation(out=sinkexp[64:65, :], in_=sinkexp[64:65, :], func=AF.Exp),
             reads=[sinkb], writes=[sinkb])
        qk_slots = {}
        for dk_ in sorted({u["dk"] for u in units}):
            qs_ = sc.slots(f"aq{dk_}", 2, [128, S], BF16)
            ks_ = sc.slots(f"ak{dk_}", 2, [128, S], BF16)
            for sl_ in (qs_, ks_):
                for i in range(2):
                    k.op("pool", lambda e: e.memset(sl_.t[i][dk_:128, :], 0.0), writes=[sl_.b[i]])
            qk_slots[dk_] = (qs_, ks_)
        vsrc = {"A0": (C.A_v[0], 8, "proj"), "A1": (C.A_v[1], 8, "proj"), "A2": (C.A_v[2], 8, "proj"),
                "B": (C.B_v, 8, "qkv"), "C": (C.C_v, 2, "proj"), "D": (C.D_v, 2, "proj")}
        vall = {}
        vstg = sc.slots("avst", 2, [128, 16, 512], BF16)
        for name in sorted({u["v"][0] for u in units}):
            src, nh, sb = vsrc[name]
            vt_ = sc.sbuf("av" + name, [128, 16, nh, 65], BF16)
            vb_ = Buf("av" + name)
            k.op("pool", lambda e: e.memset(vt_[:], 1.0), writes=[vb_])
            stg, stgb, stgsem = vstg.next()
            k.dma("sp", stg[:, :, 0:nh * 64], src.rearrange("(i p) n -> p i n", p=128), stgsem,
                  reads=[C.scr_b[sb]], writes=[stgb])
            k.op("pool", lambda e: e.tensor_copy(out=vt_[:, :, :, 0:64],
                                                 in_=stg[:, :, 0:nh * 64].rearrange("p i (h d) -> p i h d", d=64)),
                 reads=[stgb], writes=[vb_])
            vall[name] = (vt_, vb_)
        pts = sc.slots("apt", 4, [128, 512], BF16)
        accs = sc.slots("aacc", 2, [65, S], F32)
        rden = sc.sbuf("rden", [128, S], F32)
        rdenb = Buf("rden")
        k.op("pool", lambda e: e.memset(rden[:], 0.0), writes=[rdenb])
        sel64 = sc.sbuf("sel64", [128, 64], F32)
        sel64b = Buf("sel64")
        k.dma("sp", sel64[:], C.c_sel64, k.dsem("misc"), writes=[sel64b])
        ots = sc.slots("aot", 2, [64, S], BF16)
        ps_s = Slots(sc, "aps", 3, [128, 512], F32, psum=True)
        ps_a = Slots(sc, "apa", 2, [128, 512], F32, psum=True)
        ps_b = Slots(sc, "apb", 2, [64, 512], F32, psum=True)
        ident, identb = C.ident_bf, C.ident_bf_b
        loaded = {}

        def load(ui):
            if ui >= len(units) or ui in loaded:
                return
            u = units[ui]
            qs, ks = qk_slots[u["dk"]]
            qt, qb, qsem = qs.next()
            kt, kb, ksem = ks.next()
            vt_, vb = vall[u["v"][0]]
            vt = vt_[:, :, u["v"][1], :]
            dk = u["dk"]
            k.dma("sp", qt[0:dk, :], u["q"], qsem, reads=[C.scr_b["proj"], C.scr_b["qkv"]], writes=[qb])
            k.dma("sp", kt[0:dk, :], u["k"], ksem, reads=[C.scr_b["proj"], C.scr_b["qkv"]], writes=[kb])
            loaded[ui] = (qt, qb, kt, kb, vt, vb)

        load(0)
        pending = []
        for ui, u in enumerate(units):
            load(ui + 1)
            qt, qb, kt, kb, vt, vb = loaded.pop(ui)
            dk = u["dk"]
            scale = u["scale"]
            if u["first"]:
                acc_sb, acc_b, _ = accs.next()
            first_grp = u["first"]
            steps = []
            if u["kind"] == "dense":
                for qg in range(4):
                    for kc in range(16):
                        steps.append(("d", qg, kc))
            else:
                dil = u["dil"]
                Ld = S // dil
                nb = Ld // 128
                for z in range(dil):
                    for c in range(nb):
                        steps.append(("b", z, c))
            state = {}
            accst = dict(cb=0, n0=0, acc=None, accb=None)

            def emit_scores(i):
                st = steps[i]
                s, sb, _ = ps_s.next()
                pt, ptb, _ = pts.next()
                if st[0] == "d":
                    _, qg, kc = st
                    k.mm(s[:, :], lhsT=kt[:, kc * 128:(kc + 1) * 128], rhs=qt[:, qg * 512:(qg + 1) * 512],
                         start=True, stop=True, reads=[kb, qb], writes=[sb], signal=True)
                    k.op("act", lambda e: e.activation(out=pt[:, :], in_=s[:, :], func=AF.Exp, scale=scale),
                         reads=[sb], writes=[ptb])
                    state[i] = (pt, ptb, None)
                else:
                    _, z, c = st
                    chunks = [t for t in range(3) if 0 <= c - 1 + t < nb]
                    t0, t1 = chunks[0], chunks[-1] + 1
                    k.mm(s[:, t0 * 128:t1 * 128], lhsT=ident[:], rhs=bias_sb[:, u["bidx"], t0 * 128:t1 * 128],
                         start=True, stop=False, reads=[identb, biasb], writes=[sb], signal=False)
                    for t in chunks:
                        kblk = c - 1 + t
                        k.mm(s[:, t * 128:(t + 1) * 128],
                             lhsT=kt[:, z * Ld + kblk * 128: z * Ld + kblk * 128 + 128],
                             rhs=qt[:, z * Ld + c * 128: z * Ld + c * 128 + 128],
                             start=False, stop=(t == chunks[-1]), reads=[kb, qb], writes=[sb],
                             signal=(t == chunks[-1]))
                    k.op("act", lambda e: e.activation(out=pt[:, t0 * 128:t1 * 128], in_=s[:, t0 * 128:t1 * 128],
                                                       func=AF.Exp, scale=scale),
                         reads=[sb], writes=[ptb])
                    state[i] = (pt, ptb, chunks)

            def flush(n0, n, dil_):
                acc, accb = accst["acc"], accst["accb"]
                dst = fold_view(acc_sb[0:65, :], dil_, n0, n)
                src = like_fold(acc[0:65, 0:n], dil_, n)
                if first_grp:
                    k.op("act", lambda e: e.copy(out=dst, in_=src), reads=[accb], writes=[acc_b])
                else:
                    k.op("dve", lambda e: e.tensor_tensor(out=dst, in0=src, in1=dst, op=ALU.add),
                         reads=[accb, acc_b], writes=[acc_b])

            def emit_pv(i):
                st = steps[i]
                pt, ptb, chunks = state.pop(i)
                if st[0] == "d":
                    _, qg, kc = st
                    if kc == 0:
                        accst["acc"], accst["accb"], _ = ps_a.next()
                    acc, accb = accst["acc"], accst["accb"]
                    k.mm(acc[0:65, :], lhsT=vt[:, kc, :], rhs=pt[:, :], start=(kc == 0), stop=(kc == 15),
                         reads=[vb, ptb], writes=[accb], signal=(kc == 15))
                    if kc == 15:
                        flush(qg * 512, 512, 1)
                else:
                    _, z, c = st
                    if accst["cb"] == 0:
                        accst["acc"], accst["accb"], _ = ps_a.next()
                        accst["n0"] = z * Ld + c * 128
                    acc, accb = accst["acc"], accst["accb"]
                    cb = accst["cb"]
                    for t in chunks:
                        kblk = c - 1 + t
                        k.mm(acc[0:65, cb * 128:(cb + 1) * 128], lhsT=vt[:, (z * Ld + kblk * 128) // 128, :],
                             rhs=pt[:, t * 128:(t + 1) * 128], start=(t == chunks[0]), stop=(t == chunks[-1]),
                             reads=[vb, ptb], writes=[accb], signal=(t == chunks[-1]))
                    accst["cb"] += 1
                    if accst["cb"] == 4 or i == len(steps) - 1:
                        flush(accst["n0"], accst["cb"] * 128, dil)
                        accst["cb"] = 0

            DEFER = 6
            for i in range(len(steps) + LAG):
                if i < len(steps):
                    emit_scores(i)
                if i - LAG >= 0:
                    emit_pv(i - LAG)
                if i == DEFER and pending:
                    pending.pop()()

            if u["last"]:
                if pending:
                    pending.pop()()
                h = u["h"]
                if u["sink"]:
                    k.op("dve", lambda e: e.tensor_scalar(out=acc_sb[64:65, :], in0=acc_sb[64:65, :],
                                                          scalar1=sinkexp[64:65, h:h + 1], scalar2=None, op0=ALU.add),
                         reads=[acc_b, sinkb], writes=[acc_b])
                if u["kind"] == "band":
                    k.op("act", lambda e: e.activation(out=rden[64:65, :], in_=acc_sb[64:65, :], func=AF.Ln),
                         reads=[acc_b], writes=[rdenb])
                    k.op("act", lambda e: e.activation(out=rden[64:65, :], in_=rden[64:65, :], func=AF.Exp, scale=-1.0),
                         reads=[rdenb], writes=[rdenb])
                else:
                    k.op("dve", lambda e: e.reciprocal(out=rden[64:65, :], in_=acc_sb[64:65, :]),
                         reads=[acc_b], writes=[rdenb])

                def part2(acc_sb=acc_sb, acc_b=acc_b, h=h, branch=u["branch"]):
                    ot, ob, osem = ots.next()
                    for qg in range(4):
                        bc, bcb, _ = ps_b.next()
                        k.mm(bc[0:64, :], lhsT=sel64[:, :], rhs=rden[:, qg * 512:(qg + 1) * 512],
                             start=True, stop=True, reads=[rdenb, sel64b], writes=[bcb], signal=True)
                        k.op("dve", lambda e: e.tensor_tensor(out=ot[0:64, qg * 512:(qg + 1) * 512],
                                                              in0=acc_sb[0:64, qg * 512:(qg + 1) * 512], in1=bc[0:64, :],
                                                              op=ALU.mult),
                             reads=[acc_b, bcb], writes=[ob])
                    r0 = branch * 512 + h * 64
                    k.dma("sp", C.oT_d[r0:r0 + 64, :], ot[0:64, :], osem, reads=[ob], writes=[C.scr_b["oT"]])

                pending.append(part2)
        while pending:
            pending.pop()()


def _rstd(k, ss_ap, rs_ap, n, inv, ssb, rsb):
    k.op("dve", lambda e: e.tensor_scalar(out=rs_ap, in0=ss_ap, scalar1=inv, scalar2=EPS, op0=ALU.mult, op1=ALU.add),
         reads=[ssb], writes=[rsb])
    k.op("act", lambda e: e.activation(out=rs_ap, in_=rs_ap, func=AF.Sqrt), reads=[rsb], writes=[rsb])
    k.op("dve", lambda e: e.reciprocal(out=rs_ap, in_=rs_ap), reads=[rsb], writes=[rsb])


def _rope(k, x1, x2, cos, sin, o1, o2, shape, tmp, rb, wb, tb):
    t1, t2 = tmp
    k.op("dve", lambda e: e.tensor_tensor(out=t1, in0=x1, in1=cos, op=ALU.mult), reads=rb, writes=[tb])
    k.op("dve", lambda e: e.tensor_tensor(out=t2, in0=x2, in1=sin, op=ALU.mult), reads=rb, writes=[tb])
    k.op("dve", lambda e: e.tensor_tensor(out=o1, in0=t1, in1=t2, op=ALU.subtract), reads=[tb], writes=wb)
    k.op("dve", lambda e: e.tensor_tensor(out=t1, in0=x1, in1=sin, op=ALU.mult), reads=rb + [tb], writes=[tb])
    k.op("dve", lambda e: e.tensor_tensor(out=t2, in0=x2, in1=cos, op=ALU.mult), reads=rb, writes=[tb])
    k.op("dve", lambda e: e.tensor_tensor(out=o2, in0=t1, in1=t2, op=ALU.add), reads=[tb], writes=wb)


def stage_prepB(k, C, l):
    with Scope(k) as sc:
        ms = k.dsem("misc")
        rope = sc.sbuf("ropeB", [128, 16, 32], F32)
        ropeb = Buf("ropeB")
        k.dma("sp", rope[:], C.c_ropeB.rearrange("(i p) n -> p i n", p=128), ms, writes=[ropeb])
        gq = sc.sbuf("gq", [128, 512], F32)
        gkv = sc.sbuf("gkv", [128, 256], F32)
        gb = Buf("gB")
        k.dma("sp", gq[:], C.g_mla_q[l].partition_broadcast(128), ms, writes=[gb])
        k.dma("sp", gkv[:], C.g_mla_kv[l].partition_broadcast(128), ms, writes=[gb])
        wuq = sc.sbuf("wuq", [128, 4, 768], BF16)
        wukv = sc.sbuf("wukv", [128, 2, 1024], BF16)
        wb = Buf("wB")
        k.dma("pool", wuq[:], C.w_mla_uq[l].rearrange("(c p) n -> p c n", p=128), ms, writes=[wb])
        k.dma("pool", wukv[:], C.w_mla_ukv[l].rearrange("(c p) n -> p c n", p=128), ms, writes=[wb])
        bts = sc.slots("bt", 2, [128, 800], F32)
        junk = sc.sbuf("bjunk", [128, 512], BF16)
        jb = Buf("bjunk")
        ss = sc.sbuf("bss", [128, 32], F32)
        rs = sc.sbuf("brs", [128, 32], F32)
        cn = sc.slots("bcn", 2, [128, 768], BF16)
        cT = sc.slots("bcT", 2, [128, 6, 128], BF16)
        qtm = sc.slots("bqtm", 2, [128, 8, 96], BF16)
        ktm = sc.slots("bktm", 2, [128, 8, 96], BF16)
        tmp1 = sc.sbuf("btmp1", [128, 8, 16], F32)
        tmp2 = sc.sbuf("btmp2", [128, 8, 16], F32)
        tb = Buf("btmp")
        kr = sc.sbuf("bkr", [128, 32], F32)
        krb = Buf("bkr")
        vtm = sc.sbuf("bvtm", [128, 16, 512], BF16)
        vtmb = Buf("bvtm")
        qTs = sc.sbuf("bqTs", [96, 8, S], BF16)
        kTs = sc.sbuf("bkTs", [96, 8, S], BF16)
        qTb = Buf("bqTs")
        kTb = Buf("bkTs")
        pt_in = Slots(sc, "bpi", 1, [128, 1024], BF16, psum=True)
        psq = Slots(sc, "bpq", 1, [128, 512], F32, psum=True)
        psqr = Slots(sc, "bpqr", 1, [128, 256], F32, psum=True)
        pskv = Slots(sc, "bpkv", 1, [128, 512], F32, psum=True)
        psv = Slots(sc, "bpv", 1, [128, 512], F32, psum=True)
        pt_q = Slots(sc, "bptq", 1, [96, 1024], BF16, psum=True)
        pt_k = Slots(sc, "bptk", 1, [96, 1024], BF16, psum=True)
        ident, identb = C.ident_bf, C.ident_bf_b
        for i in range(16):
            bt, bb, bsem = bts.next()
            k.dma("sp", bt[:], C.B_tm[i * 128:(i + 1) * 128, :], bsem, reads=[C.scr_b["proj"]], writes=[bb])
            ssb, rsb = Buf("bss"), Buf("brs")
            k.op("act", lambda e: e.activation(out=junk[:, 0:512], in_=bt[:, 0:512], func=AF.Square,
                                               accum_out=ss[:, 2 * i:2 * i + 1]), reads=[bb], writes=[jb, ssb])
            k.op("act", lambda e: e.activation(out=junk[:, 0:256], in_=bt[:, 512:768], func=AF.Square,
                                               accum_out=ss[:, 2 * i + 1:2 * i + 2]), reads=[bb], writes=[jb, ssb])
            k.op("dve", lambda e: e.tensor_scalar(out=ss[:, 2 * i:2 * i + 1], in0=ss[:, 2 * i:2 * i + 1], scalar1=0.5,
                                                  scalar2=None, op0=ALU.mult), reads=[ssb], writes=[ssb])
            _rstd(k, ss[:, 2 * i:2 * i + 2], rs[:, 2 * i:2 * i + 2], 2, 1.0 / 256, ssb, rsb)
            cnt, cnb, _ = cn.next()
            k.op("dve", lambda e: e.scalar_tensor_tensor(out=cnt[:, 0:512], in0=bt[:, 0:512], scalar=rs[:, 2 * i:2 * i + 1],
                                                         in1=gq[:], op0=ALU.mult, op1=ALU.mult),
                 reads=[bb, rsb, gb], writes=[cnb])
            k.op("dve", lambda e: e.scalar_tensor_tensor(out=cnt[:, 512:768], in0=bt[:, 512:768],
                                                         scalar=rs[:, 2 * i + 1:2 * i + 2], in1=gkv[:], op0=ALU.mult,
                                                         op1=ALU.mult),
                 reads=[bb, rsb, gb], writes=[cnb])
            pin, pinb, _ = pt_in.next()
            for c in range(6):
                k.tr(pin[:, c * 128:(c + 1) * 128], cnt[:, c * 128:(c + 1) * 128], ident[:], reads=[cnb, identb],
                     writes=[pinb], signal=(c == 5))
            ct, ctb, _ = cT.next()
            k.op("act", lambda e: e.copy(out=ct[:], in_=pin[:, 0:768].rearrange("p (c t) -> p c t", t=128)),
                 reads=[pinb], writes=[ctb])
            pq, pqb, _ = psq.next()
            pqr, pqrb, _ = psqr.next()
            wuqv = wuq[:].rearrange("p c (h d) -> p c h d", d=96)
            for kc in range(4):
                k.mm(pq[:, :].rearrange("p (h d) -> p h d", d=64), lhsT=ct[:, kc, :], rhs=wuqv[:, kc, :, 0:64],
                     start=(kc == 0), stop=(kc == 3), reads=[ctb, wb], writes=[pqb], signal=(kc == 3))
            for kc in range(4):
                k.mm(pqr[:, :].rearrange("p (h d) -> p h d", d=32), lhsT=ct[:, kc, :], rhs=wuqv[:, kc, :, 64:96],
                     start=(kc == 0), stop=(kc == 3), reads=[ctb, wb], writes=[pqrb], signal=(kc == 3))
            pkv, pkvb, _ = pskv.next()
            pv, pvb, _ = psv.next()
            wukvv = wukv[:].rearrange("p c (h d) -> p c h d", d=128)
            for kc in range(2):
                k.mm(pkv[:, :].rearrange("p (h d) -> p h d", d=64), lhsT=ct[:, 4 + kc, :], rhs=wukvv[:, kc, :, 0:64],
                     start=(kc == 0), stop=(kc == 1), reads=[ctb, wb], writes=[pkvb], signal=(kc == 1))
            for kc in range(2):
                k.mm(pv[:, :].rearrange("p (h d) -> p h d", d=64), lhsT=ct[:, 4 + kc, :], rhs=wukvv[:, kc, :, 64:128],
                     start=(kc == 0), stop=(kc == 1), reads=[ctb, wb], writes=[pvb], signal=(kc == 1))
            qt, qtb, _ = qtm.next()
            kt, ktb, _ = ktm.next()
            pqv = pq[:, :].rearrange("p (h d) -> p h d", d=64)
            pqrv = pqr[:, :].rearrange("p (h d) -> p h d", d=32)
            pkvv = pkv[:, :].rearrange("p (h d) -> p h d", d=64)
            pvv = pv[:, :].rearrange("p (h d) -> p h d", d=64)
            k.op("act", lambda e: e.copy(out=qt[:, :, 0:64], in_=pqv[:, :, 0:64]), reads=[pqb], writes=[qtb])
            cosb = rope[:, i, 0:16].unsqueeze(1).to_broadcast([128, 8, 16])
            sinb = rope[:, i, 16:32].unsqueeze(1).to_broadcast([128, 8, 16])
            _rope(k, pqrv[:, :, 0:16], pqrv[:, :, 16:32], cosb, sinb, qt[:, :, 64:80], qt[:, :, 80:96], None,
                  (tmp1[:], tmp2[:]), [pqrb, ropeb], [qtb], tb)
            k.op("act", lambda e: e.copy(out=kt[:, :, 0:64], in_=pkvv[:, :, 0:64]), reads=[pkvb], writes=[ktb])
            k.op("act", lambda e: e.copy(out=vtm[:, i, :], in_=pv[:, :]), reads=[pvb], writes=[vtmb])
            _rope(k, bt[:, 768:784], bt[:, 784:800], rope[:, i, 0:16], rope[:, i, 16:32], kr[:, 0:16], kr[:, 16:32], None,
                  (tmp1[:, 0, :], tmp2[:, 0, :]), [bb, ropeb], [krb], tb)
            k.op("dve", lambda e: e.tensor_copy(out=kt[:, :, 64:96], in_=kr[:].unsqueeze(1).to_broadcast([128, 8, 32])),
                 reads=[krb], writes=[ktb])
            for (src, srcb, pts_, dst, dstb, eng) in ((qt, qtb, pt_q, qTs, qTb, "act"), (kt, ktb, pt_k, kTs, kTb, "dve")):
                po, pob, _ = pts_.next()
                for h in range(8):
                    k.tr(po[0:96, h * 128:(h + 1) * 128], src[:, h, :], ident[:], reads=[srcb, identb], writes=[pob],
                         signal=(h == 7))
                o_ap = dst[0:96, :, i * 128:(i + 1) * 128]
                i_ap = po[0:96, :].rearrange("p (h t) -> p h t", t=128)
                if eng == "act":
                    k.op("act", lambda e: e.copy(out=o_ap, in_=i_ap), reads=[pob], writes=[dstb])
                else:
                    k.op("dve", lambda e: e.tensor_copy(out=o_ap, in_=i_ap), reads=[pob], writes=[dstb])
        st = k.dsem("bstore")
        k.dma("sp", C.B_qT.rearrange("h p s -> p h s"), qTs[:], st, reads=[qTb], writes=[C.scr_b["qkv"]])
        k.dma("sp", C.B_kT.rearrange("h p s -> p h s"), kTs[:], st, reads=[kTb], writes=[C.scr_b["qkv"]])
        k.dma("sp", C.B_v.rearrange("(i p) n -> p i n", p=128), vtm[:], st, reads=[vtmb], writes=[C.scr_b["qkv"]])


def stage_prepC(k, C, l):
    with Scope(k) as sc:
        ms = k.dsem("misc")
        rope = sc.sbuf("ropeC", [128, 16, 64], F32)
        ropeb = Buf("ropeC")
        k.dma("sp", rope[:], C.c_ropeC.rearrange("(i p) n -> p i n", p=128), ms, writes=[ropeb])
        gc = sc.sbuf("gc", [128, 10, 64], F32)
        gb = Buf("gC")
        k.dma("sp", gc[:, 0:8, :], C.g_c_q[l:l + 1, :].partition_broadcast(128).to_broadcast([128, 8, 64]), ms, writes=[gb])
        k.dma("sp", gc[:, 8:10, :], C.g_c_k[l:l + 1, :].partition_broadcast(128).to_broadcast([128, 2, 64]), ms, writes=[gb])
        cts = sc.slots("ct", 2, [128, 640], F32)
        sq = sc.sbuf("csq", [128, 640], F32)
        sqb = Buf("csq")
        xn = sc.sbuf("cxn", [128, 640], F32)
        xnb = Buf("cxn")
        ss = sc.sbuf("css", [128, 160], F32)
        rs = sc.sbuf("crs", [128, 160], F32)
        tmp1 = sc.sbuf("ctmp1", [128, 10, 2, 16], F32)
        tmp2 = sc.sbuf("ctmp2", [128, 10, 2, 16], F32)
        tb = Buf("ctmp")
        qtm = sc.slots("cqtm", 2, [128, 10, 64], BF16)
        qTs = sc.sbuf("cqTs", [64, 10, S], BF16)
        qTb = Buf("cqTs")
        pt_q = Slots(sc, "cptq", 2, [64, 1024], BF16, psum=True)
        pt_k = Slots(sc, "cptk", 2, [64, 256], BF16, psum=True)
        ident, identb = C.ident_bf, C.ident_bf_b
        for i in range(16):
            ct, cb, csem = cts.next()
            k.dma("sp", ct[:], C.C_tm[i * 128:(i + 1) * 128, :], csem, reads=[C.scr_b["proj"]], writes=[cb])
            ssb, rsb = Buf("css"), Buf("crs")
            k.op("dve", lambda e: e.tensor_tensor(out=sq[:], in0=ct[:], in1=ct[:], op=ALU.mult), reads=[cb], writes=[sqb])
            k.op("dve", lambda e: e.tensor_reduce(out=ss[:, 10 * i:10 * i + 10], in_=sq[:].rearrange("p (h d) -> p h d", d=64),
                                                  axis=AX.X, op=ALU.add), reads=[sqb], writes=[ssb])
            _rstd(k, ss[:, 10 * i:10 * i + 10], rs[:, 10 * i:10 * i + 10], 10, 1.0 / 64, ssb, rsb)
            ctv = ct[:].rearrange("p (h d) -> p h d", d=64)
            xnv = xn[:].rearrange("p (h d) -> p h d", d=64)
            k.op("dve", lambda e: e.tensor_tensor(out=xnv, in0=ctv,
                                                  in1=rs[:, 10 * i:10 * i + 10].unsqueeze(2).to_broadcast([128, 10, 64]),
                                                  op=ALU.mult), reads=[cb, rsb], writes=[xnb])
            k.op("dve", lambda e: e.tensor_tensor(out=xnv, in0=xnv, in1=gc[:], op=ALU.mult), reads=[xnb, gb], writes=[xnb])
            qt, qtb, _ = qtm.next()
            x5 = xn[:].rearrange("p (h a t f) -> p h a t f", a=2, t=2, f=16)
            o5 = qt[:].rearrange("p h (a t f) -> p h a t f", a=2, t=2, f=16)
            r4 = rope[:, i, :].rearrange("p (a t f) -> p a t f", a=2, t=2, f=16)
            cosb = r4[:, :, 0, :].unsqueeze(1).to_broadcast([128, 10, 2, 16])
            sinb = r4[:, :, 1, :].unsqueeze(1).to_broadcast([128, 10, 2, 16])
            _rope(k, x5[:, :, :, 0, :], x5[:, :, :, 1, :], cosb, sinb, o5[:, :, :, 0, :], o5[:, :, :, 1, :], None,
                  (tmp1[:], tmp2[:]), [xnb, ropeb], [qtb], tb)
            po, pob, _ = pt_q.next()
            for h in range(8):
                k.tr(po[0:64, h * 128:(h + 1) * 128], qt[:, h, :], ident[:], reads=[qtb, identb], writes=[pob],
                     signal=(h == 7))
            k.op("act", lambda e: e.copy(out=qTs[0:64, 0:8, i * 128:(i + 1) * 128],
                                         in_=po[0:64, :].rearrange("p (h t) -> p h t", t=128)), reads=[pob], writes=[qTb])
            pk, pkb, _ = pt_k.next()
            for h in range(2):
                k.tr(pk[0:64, h * 128:(h + 1) * 128], qt[:, 8 + h, :], ident[:], reads=[qtb, identb], writes=[pkb],
                     signal=(h == 1))
            k.op("act", lambda e: e.copy(out=qTs[0:64, 8:10, i * 128:(i + 1) * 128],
                                         in_=pk[0:64, :].rearrange("p (h t) -> p h t", t=128)), reads=[pkb], writes=[qTb])
        st = k.dsem("bstore")
        k.dma("sp", C.C_qT.rearrange("h p s -> p h s"), qTs[:, 0:8, :], st, reads=[qTb], writes=[C.scr_b["qkv"]])
        k.dma("sp", C.C_kT.rearrange("h p s -> p h s"), qTs[:, 8:10, :], st, reads=[qTb], writes=[C.scr_b["qkv"]])


def stage_merge(k, C, l):
    with Scope(k) as sc:
        ms = k.dsem("misc")
        xnT = sc.sbuf("mxnT", [128, 16, S], BF16)
        xb = Buf("mxnT")
        k.dma("sp", xnT[:], C.xnT_d.rearrange("(c p) s -> p c s", p=128), k.dsem("mld0"), reads=[C.scr_b["xnT"]], writes=[xb])
        oT = sc.sbuf("moT", [128, 16, S], BF16)
        ob = Buf("moT")
        k.dma("sp", oT[:], C.oT_d.rearrange("(c p) s -> p c s", p=128), k.dsem("mld1"), reads=[C.scr_b["oT"]], writes=[ob])
        bg = sc.sbuf("mbg", [128, 4, 16], F32)
        bgb = Buf("mbg")
        k.dma("sp", bg[:], C.b_gate[l].rearrange("i (c p) -> p i c", p=128), ms, writes=[bgb], allow_slow_non_contiguous=True)
        wgs = sc.slots("mwg", 2, [128, 16, 4, 128], BF16)
        wbs = sc.slots("mwb", 2, [128, 4, 4, 128], BF16)
        sgs = sc.slots("msg", 4, [128, 512], F32)
        tmp = sc.slots("mtmp", 2, [128, 512], F32)
        macc = sc.slots("macc", 2, [128, 512], F32)
        sts = sc.slots("mst", 2, [128, S], BF16)
        pg = Slots(sc, "mpg", 3, [128, 512], F32, psum=True)
        pb_ = Slots(sc, "mpb", 3, [128, 512], F32, psum=True)
        wgv = C.w_in[l][:, G0:].rearrange("(c p) (i n) -> p c i n", p=128, i=4)
        wbv = C.w_branch[l].rearrange("i (kc p) n -> p i kc n", p=128)
        loaded = {}

        def load(cc):
            if cc >= 16 or cc in loaded:
                return
            wg, wgb, wgsem = wgs.next()
            wb, wbb, wbsem = wbs.next()
            for i in range(4):
                k.dma("pool", wg[:, :, i, :], wgv[:, :, i, cc * 128:(cc + 1) * 128], wgsem, writes=[wgb])
            for i in range(4):
                k.dma("pool", wb[:, i, :, :], wbv[:, i, :, cc * 128:(cc + 1) * 128], wbsem, writes=[wbb])
            loaded[cc] = (wg, wgb, wb, wbb)

        load(0)
        for cc in range(16):
            load(cc + 1)
            wg, wgb, wb, wbb = loaded.pop(cc)
            st, stb, stsem = sts.next()
            for tg in range(4):
                tsl = slice(tg * 512, (tg + 1) * 512)
                sg_l = []
                for i in range(4):
                    p, pb, _ = pg.next()
                    for c in range(16):
                        k.mm(p[:, :], lhsT=wg[:, c, i, :], rhs=xnT[:, c, tsl], start=(c == 0), stop=(c == 15),
                             reads=[wgb, xb], writes=[pb], signal=(c == 15))
                    sg, sgb, _ = sgs.next()
                    k.op("act", lambda e: e.activation(out=sg[:, :], in_=p[:, :], func=AF.Sigmoid, bias=bg[:, i, cc:cc + 1]),
                         reads=[pb, bgb], writes=[sgb])
                    sg_l.append((sg, sgb))
                m, mb, _ = macc.next()
                for i in range(4):
                    p, pb, _ = pb_.next()
                    for kc in range(4):
                        k.mm(p[:, :], lhsT=wb[:, i, kc, :], rhs=oT[:, i * 4 + kc, tsl], start=(kc == 0), stop=(kc == 3),
                             reads=[wbb, ob], writes=[pb], signal=(kc == 3))
                    sg, sgb = sg_l[i]
                    if i == 0:
                        k.op("dve", lambda e: e.tensor_tensor(out=m[:, :], in0=p[:, :], in1=sg[:, :], op=ALU.mult),
                             reads=[pb, sgb], writes=[mb])
                    else:
                        t, tb, _ = tmp.next()
                        k.op("dve", lambda e: e.tensor_tensor(out=t[:, :], in0=p[:, :], in1=sg[:, :], op=ALU.mult),
                             reads=[pb, sgb], writes=[tb])
                        o_ap = m[:, :] if i < 3 else st[:, tsl]
                        k.op("pool", lambda e: e.tensor_tensor(out=o_ap, in0=m[:, :], in1=t[:, :], op=ALU.add),
                             reads=[mb, tb], writes=[mb] if i < 3 else [stb])
            k.dma("sp", C.mixT_d[cc * 128:(cc + 1) * 128, :], st[:, :], stsem, reads=[stb], writes=[C.scr_b["mixT"]])


def stage_out(k, C, l, h_src):
    with Scope(k) as sc:
        mixT = sc.sbuf("omixT", [128, 16, S], BF16)
        mb = Buf("omixT")
        k.dma("sp", mixT[:], C.mixT_d.rearrange("(c p) s -> p c s", p=128), k.dsem("mld0"), reads=[C.scr_b["mixT"]], writes=[mb])
        ws = sc.slots("ow", 2, [128, 16, 512], BF16)
        hs = sc.slots("oh", 2, [128, 16, 512], F32)
        ps = Slots(sc, "ops", 4, [128, 512], F32, psum=True)
        wv = C.w_out[l].rearrange("(c p) n -> p c n", p=128)
        hv = h_src.rearrange("(i p) n -> p i n", p=128)
        ho = C.h_d.rearrange("(i p) n -> p i n", p=128)
        loaded = {}

        def load(cb):
            if cb >= 4 or cb in loaded:
                return
            w, wb, wsem = ws.next()
            k.dma("pool", w[:], wv[:, :, cb * 512:(cb + 1) * 512], wsem, writes=[wb])
            h, hb, hsem = hs.next()
            k.dma("sp", h[:], hv[:, :, cb * 512:(cb + 1) * 512], hsem, reads=[C.scr_b["h"]], writes=[hb])
            loaded[cb] = (w, wb, h, hb, hsem)

        load(0)
        for cb in range(4):
            load(cb + 1)
            w, wb, h, hb, hsem = loaded.pop(cb)
            for i in range(16):
                p, pb, _ = ps.next()
                for c in range(16):
                    k.mm(p[:, :], lhsT=mixT[:, c, i * 128:(i + 1) * 128], rhs=w[:, c, :], start=(c == 0), stop=(c == 15),
                         reads=[mb, wb], writes=[pb], signal=(c == 15))
                k.op("dve", lambda e: e.tensor_tensor(out=h[:, i, :], in0=p[:, :], in1=h[:, i, :], op=ALU.add),
                     reads=[pb, hb], writes=[hb])
            k.dma("sp", ho[:, :, cb * 512:(cb + 1) * 512], h[:], hsem, reads=[hb], writes=[C.scr_b["h"]])


def moe_routing(k, C, lg, lgb, probs, pb_, mask, mkb, rank, rkb, gm, gmb, upto=99):
    ms = k.dsem("misc")
    with Scope(k) as sc:
        mx = sc.sbuf("emx", [128, 16], F32)
        mxb = Buf("emx")
        identf = sc.sbuf("eidf", [128, 128], F32)
        idfb = Buf("eidf")
        k.dma("sp", identf[:], C.c_ident, ms, writes=[idfb])
        ustr = sc.sbuf("eustr", [128, 128], BF16)
        onesb = sc.sbuf("eones", [128, 128], BF16)
        ub = Buf("eustr")
        k.dma("pool", ustr[:], C.c_ustr, ms, writes=[ub])
        k.dma("pool", onesb[:], C.c_ones, ms, writes=[ub])
        import os
        PRE = os.environ.get("PRE", "")
        k.op("dve", lambda e: e.tensor_reduce(out=mx[:], in_=lg[:], axis=AX.X, op=ALU.max), reads=[lgb], writes=[mxb])
        k.op("dve", lambda e: e.tensor_tensor(out=lg[:], in0=lg[:], in1=mx[:].unsqueeze(2).to_broadcast([128, 16, 16]),
                                              op=ALU.subtract), reads=[lgb, mxb], writes=[lgb])
        if "noexp" not in PRE:
            k.op("act", lambda e: e.activation(out=lg[:], in_=lg[:], func=AF.Exp), reads=[lgb], writes=[lgb])
        k.op("dve", lambda e: e.tensor_reduce(out=mx[:], in_=lg[:], axis=AX.X, op=ALU.add), reads=[lgb], writes=[mxb])
        k.op("dve", lambda e: e.reciprocal(out=mx[:], in_=mx[:]), reads=[mxb], writes=[mxb])
        k.op("dve", lambda e: e.tensor_tensor(out=probs[:], in0=lg[:], in1=mx[:].unsqueeze(2).to_broadcast([128, 16, 16]),
                                              op=ALU.mult), reads=[lgb, mxb], writes=[pb_])
        if upto < 1:
            return
        pT = sc.sbuf("epT", [128, S], F32)[0:16, :]
        work = sc.sbuf("ework", [128, S], F32)[0:16, :]
        mT = sc.sbuf("emT", [128, S], F32)[0:16, :]
        pTb, wkb, mTb = Buf("epT"), Buf("ework"), Buf("emT")
        m8 = sc.sbuf("em8", [128, 8], F32)[0:16, :]
        m8b = Buf("em8")
        ppT = Slots(sc, "eppT", 4, [128, 512], F32, psum=True)
        import os
        NB = int(os.environ.get("NB", "4"))
        for bnk in range(NB):
            p, pb2, _ = ppT.next()
            for j in range(4):
                i = bnk * 4 + j
                if "mm" not in os.environ.get("SKIP", ""):
                    k.mm(p[0:16, j * 128:(j + 1) * 128], lhsT=probs[:, i, :], rhs=identf[:], start=True, stop=True,
                         reads=[pb_, idfb], writes=[pb2], signal=(j == 3))
            if "act" not in os.environ.get("SKIP", ""):
                k.op("act", lambda e: e.copy(out=pT[:, bnk * 512:(bnk + 1) * 512], in_=p[0:16, :]), reads=[pb2], writes=[pTb])
            if "dve" not in os.environ.get("SKIP", ""):
                k.op("dve", lambda e: e.tensor_copy(out=work[:, bnk * 512:(bnk + 1) * 512],
                                                    in_=pT[:, bnk * 512:(bnk + 1) * 512]), reads=[pTb], writes=[wkb])
        if upto < 2:
            return
        for r in range(CAP // 8):
            k.op("dve", lambda e: e.max(out=m8[:], in_=work[:]), reads=[wkb], writes=[m8b])
            if r < CAP // 8 - 1:
                k.op("dve", lambda e: e.match_replace(out=work[:], in_to_replace=m8[:], in_values=work[:], imm_value=-1.0),
                     reads=[wkb, m8b], writes=[wkb])
        k.op("dve", lambda e: e.tensor_scalar(out=mT[:], in0=pT[:], scalar1=m8[:, 7:8], scalar2=None, op0=ALU.is_ge),
             reads=[pTb, m8b], writes=[mTb])
        if upto < 3:
            return
        pm = Slots(sc, "epm", 1, [128, 256], F32, psum=True)
        p, pb2, _ = pm.next()
        for i in range(16):
            k.mm(p[:, i * 16:(i + 1) * 16], lhsT=mT[:, i * 128:(i + 1) * 128], rhs=identf[0:16, 0:16], start=True,
                 stop=True, reads=[mTb, idfb], writes=[pb2], signal=(i == 15))
        k.op("act", lambda e: e.copy(out=mask[:].rearrange("p i e -> p (i e)"), in_=p[:, :]), reads=[pb2], writes=[mkb])
        maskbf = sc.sbuf("emaskbf", [128, 16, 16], BF16)
        mbfb = Buf("emaskbf")
        k.op("dve", lambda e: e.tensor_copy(out=maskbf[:], in_=mask[:]), reads=[mkb], writes=[mbfb])
        k.op("dve", lambda e: e.tensor_tensor(out=gm[:], in0=probs[:], in1=mask[:], op=ALU.mult), reads=[pb_, mkb],
             writes=[gmb])
        if upto < 4:
            return
        pr = Slots(sc, "epr", 1, [128, 256], F32, psum=True)
        p, pb2, _ = pr.next()
        for i in range(16):
            for j in range(i + 1):
                k.mm(p[:, i * 16:(i + 1) * 16], lhsT=(ustr[:] if j == i else onesb[:]), rhs=maskbf[:, j, :],
                     start=(j == 0), stop=(j == i), reads=[ub, mbfb], writes=[pb2], signal=(i == 15 and j == i))
        k.op("act", lambda e: e.copy(out=rank[:].rearrange("p i e -> p (i e)"), in_=p[:, :]), reads=[pb2], writes=[rkb])


def stage_moe(k, C, l, dbg=None):
    with Scope(k) as osc:
        ms = k.dsem("misc")
        xn = osc.sbuf("exn", [128, 16, D_MODEL], BF16)
        xnb = [Buf(f"exn{i}") for i in range(16)]
        lg = osc.sbuf("elg", [128, 16, 16], F32)
        lgb = Buf("elg")
        wr = osc.sbuf("ewr", [128, 16, 16], BF16)
        wrb = Buf("ewr")
        k.dma("pool", wr[:], C.w_router[l].rearrange("(c p) e -> p c e", p=128), ms, writes=[wrb])
        stage_norm(k, C, C.h_d, C.g_ffn[l], xn_sb=xn, xn_bufs=xnb, router=(wr, wrb, lg, lgb))
        probs = osc.sbuf("eprobs", [128, 16, 16], F32)
        mask = osc.sbuf("emask", [128, 16, 16], F32)
        rank = osc.sbuf("erank", [128, 16, 16], F32)
        gm = osc.sbuf("egm", [128, 16, 16], F32)
        pb_, mkb, rkb, gmb = Buf("eprobs"), Buf("emask"), Buf("erank"), Buf("egm")
        iota = osc.sbuf("eiota", [128, 256], F32)
        iob = Buf("eiota")
        k.dma("sp", iota[:], C.c_iota, ms, writes=[iob])
        moe_routing(k, C, lg, lgb, probs, pb_, mask, mkb, rank, rkb, gm, gmb)
        if dbg:
            st = k.dsem("bstore")
            k.dma("sp", C.dbg_probs.rearrange("(i p) e -> p i e", p=128), probs[:], st, reads=[pb_], writes=[C.scr_b["moe"]])
            k.dma("sp", C.dbg_mask.rearrange("(i p) e -> p i e", p=128), mask[:], st, reads=[mkb], writes=[C.scr_b["moe"]])
            k.dma("sp", C.dbg_rank.rearrange("(i p) e -> p i e", p=128), rank[:], st, reads=[rkb], writes=[C.scr_b["moe"]])
        if dbg and 6 in dbg:
            return
        with Scope(k) as sc:
            sels = sc.slots("esel", 2, [128, 16, 256], BF16)
            selg = sc.slots("eselg", 1, [128, 16, 256], BF16)
            sgT = sc.slots("esgT", 1, [128, 2, S], BF16)
            xeTs = sc.slots("exeT", 1, [128, 16, 256], BF16)
            hTs = sc.slots("ehT", 1, [128, 8, 256], BF16)
            sgt = sc.slots("esg", 2, [128, 256], F32)
            yes = sc.slots("eye", 1, [128, 2, D_MODEL], BF16)
            NW = 9
            wring = sc.slots("ewr", NW, [128, 4096], BF16)
            pgt = Slots(sc, "epg", 2, [128, 512], F32, psum=True)
            ptr = Slots(sc, "eptr", 1, [128, 1024], BF16, psum=True)
            pgu = Slots(sc, "epgu", 2, [128, 512], F32, psum=True)
            py = Slots(sc, "epy", 2, [128, 512], F32, psum=True)
            ident, identb = C.ident_bf, C.ident_bf_b
            wjobs = []
            for e in range(NE):
                for fb in range(4):
                    wjobs.append((e, 0, fb))
                    wjobs.append((e, 1, fb))
                for cb in range(4):
                    wjobs.append((e, 2, cb))
            wl = {}
            wnext = [0]

            def ensure(upto):
                while wnext[0] <= upto and wnext[0] < len(wjobs):
                    j = wnext[0]
                    e, kind, idx = wjobs[j]
                    w, wb, wsem = wring.next()
                    if kind < 2:
                        src = (C.w_exp_gate if kind == 0 else C.w_exp_up)[l, e].rearrange("(c p) f -> p c f", p=128)
                        wv_ = w[:].rearrange("p (c f) -> p c f", f=256)
                        k.dma("pool", wv_, src[:, :, idx * 256:(idx + 1) * 256], wsem, writes=[wb])
                    else:
                        src = C.w_exp_down[l, e].rearrange("(f p) n -> p f n", p=128)
                        wv_ = w[:].rearrange("p (f n) -> p f n", n=512)
                        k.dma("pool", wv_, src[:, :, idx * 512:(idx + 1) * 512], wsem, writes=[wb])
                    wl[j] = (wv_, wb)
                    wnext[0] += 1

            ensure(NW - 1)
            for e in range(NE):
                sel, selb, _ = sels.next()
                sg_, sgb_, _ = selg.next()
                for i in range(16):
                    k.op("dve", lambda en: en.tensor_scalar(out=sel[:, i, :], in0=iota[:], scalar1=rank[:, i, e:e + 1],
                                                            scalar2=mask[:, i, e:e + 1], op0=ALU.is_equal, op1=ALU.mult),
                         reads=[iob, rkb, mkb], writes=[selb])
                for i in range(16):
                    k.op("dve", lambda en: en.tensor_scalar(out=sg_[:, i, :], in0=iota[:], scalar1=rank[:, i, e:e + 1],
                                                            scalar2=gm[:, i, e:e + 1], op0=ALU.is_equal, op1=ALU.mult),
                         reads=[iob, rkb, gmb], writes=[sgb_])
                xeT, xeTb, _ = xeTs.next()
                for d2 in range(8):
                    p, pb2, _ = pgt.next()
                    for dd in range(2):
                        dc = d2 * 2 + dd
                        for i in range(16):
                            k.mm(p[:, dd * 256:(dd + 1) * 256], lhsT=xn[:, i, dc * 128:(dc + 1) * 128], rhs=sel[:, i, :],
                                 start=(i == 0), stop=(i == 15), reads=[xnb[i], selb], writes=[pb2],
                                 signal=(i == 15 and dd == 1))
                    k.op("act", lambda en: en.copy(out=xeT[:, d2 * 2:d2 * 2 + 2, :],
                                                   in_=p[:, :].rearrange("p (a c) -> p a c", a=2)),
                         reads=[pb2], writes=[xeTb])
                st_, stb_, stsem = sgT.next()
                for ct in range(2):
                    for half in range(2):
                        p, pb2, _ = ptr.next()
                        for j in range(8):
                            i = half * 8 + j
                            k.tr(p[:, j * 128:(j + 1) * 128], sg_[:, i, ct * 128:(ct + 1) * 128], ident[:],
                                 reads=[sgb_, identb], writes=[pb2], signal=(j == 7))
                        k.op("dve", lambda en: en.tensor_copy(out=st_[:, ct, half * 1024:(half + 1) * 1024], in_=p[:, :]),
                             reads=[pb2], writes=[stb_])
                    k.dma("sp", C.selGT_d[:, :, 2 * e + ct, :].rearrange("i c t -> c i t"),
                          st_[:, ct, :].rearrange("c (i t) -> c i t", t=128), stsem, reads=[stb_], writes=[C.scr_b["moe"]])
                hT, hTb, _ = hTs.next()
                for fb in range(4):
                    j0 = e * 12 + fb * 2
                    ensure(j0 + NW - 1)
                    wg, wgb = wl.pop(j0)
                    wu, wub = wl.pop(j0 + 1)
                    for fc in range(2):
                        p, pb2, _ = pgu.next()
                        for (w_, wb_, off) in ((wg, wgb, 0), (wu, wub, 256)):
                            for dc in range(16):
                                k.mm(p[:, off:off + 256], lhsT=w_[:, dc, fc * 128:(fc + 1) * 128], rhs=xeT[:, dc, :],
                                     start=(dc == 0), stop=(dc == 15), reads=[wb_, xeTb], writes=[pb2],
                                     signal=(dc == 15 and off == 256))
                        s_, sb_, _ = sgt.next()
                        k.op("act", lambda en: en.activation(out=s_[:, :], in_=p[:, 0:256], func=AF.Silu), reads=[pb2],
                             writes=[sb_])
                        k.op("dve", lambda en: en.tensor_tensor(out=hT[:, fb * 2 + fc, :], in0=p[:, 256:512], in1=s_[:, :],
                                                                op=ALU.mult), reads=[pb2, sb_], writes=[hTb])
                ye, yeb, yesem = yes.next()
                for cb in range(4):
                    j = e * 12 + 8 + cb
                    ensure(j + NW - 1)
                    wd, wdb = wl.pop(j)
                    for ct in range(2):
                        p, pb2, _ = py.next()
                        for f in range(8):
                            k.mm(p[:, :], lhsT=hT[:, f, ct * 128:(ct + 1) * 128], rhs=wd[:, f, :], start=(f == 0),
                                 stop=(f == 7), reads=[hTb, wdb], writes=[pb2], signal=(f == 7))
                        if ct == 0:
                            k.op("act", lambda en: en.copy(out=ye[:, ct, cb * 512:(cb + 1) * 512], in_=p[:, :]), reads=[pb2],
                                 writes=[yeb])
                        else:
                            k.op("dve", lambda en: en.tensor_copy(out=ye[:, ct, cb * 512:(cb + 1) * 512], in_=p[:, :]),
                                 reads=[pb2], writes=[yeb])
                k.dma("sp", C.ye_d[:, 2 * e:2 * e + 2, :], ye[:], yesem, reads=[yeb], writes=[C.scr_b["moe"]])
    if dbg and 7 in dbg:
        return
    with Scope(k) as sc:
        yes = sc.slots("sye", 2, [128, 32, 512], BF16)
        sls = sc.slots("ssl", 2, [128, 32, 128], BF16)
        hs = sc.slots("sh", 2, [128, 16, 512], F32)
        ps = Slots(sc, "sps", 3, [128, 512], F32, psum=True)
        hv = C.h_d.rearrange("(i p) n -> p i n", p=128)
        for cb in range(4):
            ye, yeb, yesem = yes.next()
            k.dma("sp", ye[:], C.ye_d[:, :, cb * 512:(cb + 1) * 512], yesem, reads=[C.scr_b["moe"]], writes=[yeb])
            h, hb, hsem = hs.next()
            k.dma("sp", h[:], hv[:, :, cb * 512:(cb + 1) * 512], hsem, reads=[C.scr_b["h"]], writes=[hb])
            for i in range(16):
                sl, slb, slsem = sls.next()
                k.dma("sp", sl[:], C.selGT_d[i], slsem, reads=[C.scr_b["moe"]], writes=[slb])
                p, pb2, _ = ps.next()
                for et in range(32):
                    k.mm(p[:, :], lhsT=sl[:, et, :], rhs=ye[:, et, :], start=(et == 0), stop=(et == 31), reads=[slb, yeb],
                         writes=[pb2], signal=(et == 31))
                k.op("dve", lambda en: en.tensor_tensor(out=h[:, i, :], in0=p[:, :], in1=h[:, i, :], op=ALU.add),
                     reads=[pb2, hb], writes=[hb])
            k.dma("sp", hv[:, :, cb * 512:(cb + 1) * 512], h[:], hsem, reads=[hb], writes=[C.scr_b["h"]])


def stage_final(k, C, yb):
    with Scope(k) as sc:
        hs = sc.slots("fh", 3, [128, D_MODEL], F32)
        os_ = sc.slots("fo", 2, [128, D_MODEL], F32)
        gbc = sc.sbuf("fgbc", [128, D_MODEL], F32)
        gb = Buf("fgbc")
        k.dma("sp", gbc[:], C.g_final.partition_broadcast(128), k.dsem("misc"), writes=[gb])
        junk = sc.sbuf("fjunk", [128, D_MODEL], BF16)
        jb = Buf("fjunk")
        ss = sc.sbuf("fss", [128, 16], F32)
        rs = sc.sbuf("frs", [128, 16], F32)
        for i in range(16):
            ht, hb, hsem = hs.next()
            k.dma("sp", ht[:], C.h_d[i * 128:(i + 1) * 128, :], hsem, reads=[C.scr_b["h"]], writes=[hb])
            ssb, rsb = Buf("fss"), Buf("frs")
            k.op("act", lambda e: e.activation(out=junk[:], in_=ht[:], func=AF.Square, accum_out=ss[:, i:i + 1]),
                 reads=[hb], writes=[jb, ssb])
            _rstd(k, ss[:, i:i + 1], rs[:, i:i + 1], 1, 1.0 / D_MODEL, ssb, rsb)
            ot, ob, osem = os_.next()
            k.op("dve", lambda e: e.scalar_tensor_tensor(out=ot[:], in0=ht[:], scalar=rs[:, i:i + 1], in1=gbc[:],
                                                         op0=ALU.mult, op1=ALU.mult), reads=[hb, rsb, gb], writes=[ob])
            k.dma("sp", C.y[i * 128:(i + 1) * 128, :], ot[:], osem, reads=[ob], writes=[yb])


def alloc_scratch(k, C, dbg):
    kind = "ExternalOutput" if dbg else "Internal"
    C.scr_b = {n: Buf(n) for n in ("h", "xnT", "proj", "qkv", "oT", "mixT", "moe")}
    C.h_d = k.dram("h_d", [S, D_MODEL], F32, kind).ap()
    C.xnT_d = k.dram("xnT_d", [D_MODEL, S], BF16).ap()
    C.A_qT = k.dram("A_qT", [1536, S], BF16, kind).ap()
    C.A_kT = k.dram("A_kT", [1536, S], BF16, kind).ap()
    C.A_v = [k.dram(f"A_v{g}", [S, 512], BF16, kind).ap() for g in range(3)]
    C.B_tm = k.dram("B_tm", [S, 800], F32, kind).ap()
    C.C_tm = k.dram("C_tm", [S, 640], F32, kind).ap()
    C.C_v = k.dram("C_v", [S, 128], BF16, kind).ap()
    C.D_qT = k.dram("D_qT", [512, S], BF16, kind).ap()
    C.D_kT = k.dram("D_kT", [128, S], BF16, kind).ap()
    C.D_v = k.dram("D_v", [S, 128], BF16, kind).ap()
    C.B_qT = k.dram("B_qT", [8, 96, S], BF16, kind).ap()
    C.B_kT = k.dram("B_kT", [8, 96, S], BF16, kind).ap()
    C.B_v = k.dram("B_v", [S, 512], BF16, kind).ap()
    C.C_qT = k.dram("C_qT", [8, 64, S], BF16, kind).ap()
    C.C_kT = k.dram("C_kT", [2, 64, S], BF16, kind).ap()
    C.oT_d = k.dram("oT_d", [2048, S], BF16, kind).ap()
    C.mixT_d = k.dram("mixT_d", [D_MODEL, S], BF16, kind).ap()
    C.selGT_d = k.dram("selGT_d", [16, 128, 32, 128], BF16).ap()
    C.ye_d = k.dram("ye_d", [128, 32, D_MODEL], BF16, kind).ap()
    if dbg:
        C.dbg_probs = k.dram("dbg_probs", [S, 16], F32, kind).ap()
        C.dbg_mask = k.dram("dbg_mask", [S, 16], F32, kind).ap()
        C.dbg_rank = k.dram("dbg_rank", [S, 16], F32, kind).ap()


def build(dbg=None):
    k = K()
    C = Ctx()
    C.x = k.dram("x", [S, D_MODEL], F32, "ExternalInput").ap()
    C.w_in = k.dram("w_in", [DEPTH, D_MODEL, N_IN], F32, "ExternalInput").ap()
    C.g_attn = k.dram("g_attn_norm", [DEPTH, D_MODEL], F32, "ExternalInput").ap()
    C.c_ident = k.dram("c_ident", [128, 128], F32, "ExternalInput").ap()
    C.c_bias = k.dram("c_bias", [32, 128, 384], F32, "ExternalInput").ap()
    C.c_ones = k.dram("c_ones", [128, 128], F32, "ExternalInput").ap()
    C.c_sel64 = k.dram("c_sel64", [128, 64], F32, "ExternalInput").ap()
    C.sink_d = k.dram("sink_d", [DEPTH, 8], F32, "ExternalInput").ap()
    C.g_mla_q = k.dram("g_mla_q", [DEPTH, 512], F32, "ExternalInput").ap()
    C.b_gate = k.dram("b_gate", [DEPTH, 4, D_MODEL], F32, "ExternalInput").ap()
    C.g_ffn = k.dram("g_ffn_norm", [DEPTH, D_MODEL], F32, "ExternalInput").ap()
    C.g_final = k.dram("g_final", [D_MODEL], F32, "ExternalInput").ap()
    C.w_router = k.dram("w_router", [DEPTH, D_MODEL, NE], F32, "ExternalInput").ap()
    C.w_exp_gate = k.dram("w_exp_gate", [DEPTH, NE, D_MODEL, EFF], F32, "ExternalInput").ap()
    C.w_exp_up = k.dram("w_exp_up", [DEPTH, NE, D_MODEL, EFF], F32, "ExternalInput").ap()
    C.w_exp_down = k.dram("w_exp_down", [DEPTH, NE, EFF, D_MODEL], F32, "ExternalInput").ap()
    C.c_iota = k.dram("c_iota", [128, 256], F32, "ExternalInput").ap()
    C.c_ustr = k.dram("c_ustr", [128, 128], F32, "ExternalInput").ap()
    C.w_branch = k.dram("w_branch", [DEPTH, 4, 512, D_MODEL], F32, "ExternalInput").ap()
    C.w_out = k.dram("w_out", [DEPTH, D_MODEL, D_MODEL], F32, "ExternalInput").ap()
    C.g_mla_kv = k.dram("g_mla_kv", [DEPTH, 256], F32, "ExternalInput").ap()
    C.w_mla_uq = k.dram("w_mla_uq", [DEPTH, 512, 768], F32, "ExternalInput").ap()
    C.w_mla_ukv = k.dram("w_mla_ukv", [DEPTH, 256, 1024], F32, "ExternalInput").ap()
    C.g_c_q = k.dram("g_c_q", [DEPTH, 64], F32, "ExternalInput").ap()
    C.g_c_k = k.dram("g_c_k", [DEPTH, 64], F32, "ExternalInput").ap()
    C.c_ropeB = k.dram("c_ropeB", [S, 32], F32, "ExternalInput").ap()
    C.c_ropeC = k.dram("c_ropeC", [S, 64], F32, "ExternalInput").ap()
    C.y = k.dram("y", [S, D_MODEL], F32, "ExternalOutput").ap()
    alloc_scratch(k, C, dbg)
    yb = Buf("y")

    with Scope(k) as top:
        C.ident_bf = top.sbuf("ident_bf", [128, 128], BF16)
        C.ident_bf_b = Buf("ident_bf")
        k.dma("pool", C.ident_bf[:], C.c_ident, k.dsem("misc"), writes=[C.ident_bf_b])
        C.ones_f = top.sbuf("ones_f", [128, 128], F32)
        C.ones_f_b = Buf("ones_f")
        k.dma("sp", C.ones_f[:], C.c_ones, k.dsem("misc"), writes=[C.ones_f_b])
        for l in range(1 if dbg else DEPTH):
            with Scope(k) as lsc:
                xnT = lsc.sbuf("xnT", [128, 16, S], BF16)
                xnTb = [Buf(f"xnT{i}") for i in range(16)]
                with k.nc.named_scope('stage_norm'):
                    stage_norm(k, C, C.x if l == 0 else C.h_d, C.g_attn[l], xnT_sb=xnT, xnT_bufs=xnTb, featT_dst=C.xnT_d)
                with k.nc.named_scope('stage_proj'):
                    stage_proj(k, C, l, xnT, xnTb)
            if not dbg or 1 in dbg:
                with k.nc.named_scope('stage_prepB'):
                    stage_prepB(k, C, l)
            if not dbg or 2 in dbg:
                with k.nc.named_scope('stage_prepC'):
                    stage_prepC(k, C, l)
            units = attn_units(C)
            if dbg:
                units = [u for u in units if u["branch"] in dbg]
            with k.nc.named_scope('stage_attn'):
                stage_attn(k, C, l, units)
            if not dbg or 4 in dbg:
                with k.nc.named_scope('stage_merge'):
                    stage_merge(k, C, l)
                with k.nc.named_scope('stage_out'):
                    stage_out(k, C, l, C.x if l == 0 else C.h_d)
            if dbg and 4 not in dbg:
                k.dma("sp", C.h_d, C.x, k.dsem("misc"), writes=[C.scr_b["h"]])
            if not dbg or 5 in dbg:
                with k.nc.named_scope('stage_moe'):
                    stage_moe(k, C, l, dbg)
        if not dbg or 8 in dbg:
            with k.nc.named_scope('stage_final'):
                stage_final(k, C, yb)
        else:
            k.dma('sp', C.y[0:128, :], C.x[0:128, :], k.dsem('out'), writes=[yb])
        k.barrier()
    k.finish()
    return k


def host_consts():
    c = {"c_ident": np.eye(128, dtype=np.float32), "c_ones": np.ones((128, 128), np.float32)}
    c["c_sel64"] = np.zeros((128, 64), np.float32)
    c["c_sel64"][64, :] = 1.0
    c["c_iota"] = np.tile(np.arange(256, dtype=np.float32)[None, :], (128, 1))
    c["c_ustr"] = (np.arange(128)[:, None] < np.arange(128)[None, :]).astype(np.float32)
    bias = np.zeros((32, 128, 384), np.float32)
    kk = np.arange(128)[:, None].astype(np.float64)
    qq = np.arange(128)[None, :].astype(np.float64)
    dists = [qq - kk + 128, np.abs(qq - kk), kk + 128 - qq]
    for idx in range(32):
        if idx < 24:
            g, h = divmod(idx, 8)
            slope = 2.0 ** (-(h + 1)) * A_DIL[g]
            W = 64
        else:
            h = idx - 24
            slope = 2.0 ** (-(h + 1))
            W = 128
        for t in range(3):
            d = dists[t]
            bias[idx, :, t * 128:(t + 1) * 128] = np.where(d <= W, -slope * d * 8.0, -240000.0)
    c["c_bias"] = bias
    freqs = 10000.0 ** (-np.arange(0, 32, 2, dtype=np.float32) / 32)
    pos = np.arange(S, dtype=np.float32)
    ang = pos[:, None] * freqs[None, :]
    c["c_ropeB"] = np.concatenate([np.cos(ang), np.sin(ang)], axis=1).astype(np.float32)
    rows = np.repeat(np.arange(S // 64), 64).astype(np.float32)
    cols = np.tile(np.arange(64), S // 64).astype(np.float32)
    ar = rows[:, None] * freqs[None, :]
    ac = cols[:, None] * freqs[None, :]
    c["c_ropeC"] = np.concatenate([np.cos(ar), np.sin(ar), np.cos(ac), np.sin(ac)], axis=1).astype(np.float32)
    return c


def kernel(**inputs):
    k = build()
    consts = host_consts()
    in_maps = []
    for b in range(8):
        m = {"x": np.ascontiguousarray(inputs["x"][b])}
        for n in WEIGHT_NAMES:
            m[n] = np.ascontiguousarray(inputs[n])
        m.update(consts)
        in_maps.append(m)
    res = run_bass_kernel_spmd(k.nc, in_maps, core_ids=list(range(8)))
    return np.stack([r["y"] for r in res.results], axis=0)
```

```python
import numpy as np
from contextlib import ExitStack
import concourse.bass as bass
import concourse.mybir as mybir
from concourse.bass_utils import run_bass_kernel_spmd

F32 = mybir.dt.float32
BF16 = mybir.dt.bfloat16
AF = mybir.ActivationFunctionType
ALU = mybir.AluOpType
AX = mybir.AxisListType

D_MODEL = 2048
S = 2048
DEPTH = 2
N_IN = 15136
G0 = 6944
EPS = 1e-6
NE = 16
EFF = 1024
CAP = 256
A_DIL = (1, 4, 16)
WEIGHT_NAMES = ["w_in", "g_attn_norm", "sink_d", "g_mla_q", "g_mla_kv", "w_mla_uq", "w_mla_ukv", "g_c_q", "g_c_k", "b_gate", "w_branch", "w_out", "g_ffn_norm", "g_final", "w_router", "w_exp_gate", "w_exp_up", "w_exp_down"]


class Sem:
    def __init__(self, handle, name):
        self.h = handle
        self.name = name
        self.count = 0
        self.dma = name.startswith("d_")


class Buf:
    __slots__ = ("name", "w", "r")

    def __init__(self, name):
        self.name = name
        self.w = None
        self.r = {}


class Q:
    def __init__(self, name, eng, sem):
        self.name = name
        self.eng = eng
        self.sem = sem
        self.waited = {}


class K:
    def __init__(self):
        self.nc = bass.Bass("TRN2", target_bir_lowering=False)
        self.es = ExitStack()
        nc = self.nc
        self.sems = []
        self.q = {}
        for name, eng in (("pe", nc.tensor), ("act", nc.scalar), ("dve", nc.vector),
                          ("pool", nc.gpsimd), ("sp", nc.sync)):
            self.q[name] = Q(name, eng, self.sem("q_" + name))
        self.ninst = 0
        self.uid = 0
        self.slot_sem_idx = 0
        self.sem_pool = {}

    def sem(self, name):
        s = Sem(self.es.enter_context(self.nc.semaphore(name)), name)
        self.sems.append(s)
        return s

    def dsem(self, name):
        if name not in self.sem_pool:
            self.sem_pool[name] = self.sem("d_" + name)
        return self.sem_pool[name]

    def dram(self, name, shape, dt, kind="Internal"):
        return self.nc.dram_tensor(name, list(shape), dt, kind=kind)

    def _deps(self, q, reads, writes):
        deps = {}

        def add(s, v, raw):
            if s is q.sem:
                if not raw or q.name == "pe" or q.name == "sp":
                    return
            if deps.get(s, 0) < v:
                deps[s] = v

        for b in reads:
            if b.w is not None:
                add(b.w[0], b.w[1], True)
        for b in writes:
            if b.w is not None:
                add(b.w[0], b.w[1], False)
            for s, v in b.r.items():
                add(s, v, False)
        for s, v in deps.items():
            if s.dma:
                v = s.count
            if q.waited.get(s, 0) >= v:
                continue
            assert v <= s.count, f"wait on unsignalled token {s.name} {v}>{s.count} from {q.name}"
            q.eng.wait_ge(s.h, v)
            q.waited[s] = v

    def op(self, E, emit, reads=(), writes=(), signal=True):
        q = self.q[E]
        self._deps(q, reads, writes)
        ins = emit(q.eng)
        self.ninst += 1
        if signal:
            q.sem.count += 1
            ins.then_inc(q.sem.h, 1)
            v = q.sem.count
        else:
            v = q.sem.count + 1
        for b in reads:
            b.r[q.sem] = v
        for b in writes:
            b.w = (q.sem, v)
            b.r = {}
        return ins

    def dma(self, E, out, in_, sem, reads=(), writes=(), **kw):
        q = self.q[E]
        self._deps(q, reads, writes)
        ins = q.eng.dma_start(out=out, in_=in_, **kw)
        self.ninst += 1
        sem.count += 16
        ins.then_inc(sem.h, 16)
        for b in reads:
            b.r[sem] = sem.count
        for b in writes:
            b.w = (sem, sem.count)
            b.r = {}
        return ins

    def barrier(self):
        for q in self.q.values():
            for s in self.sems:
                if s is q.sem or s.count == 0:
                    continue
                if q.waited.get(s, 0) >= s.count:
                    continue
                q.eng.wait_ge(s.h, s.count)
                q.waited[s] = s.count

    def finish(self):
        self.barrier()
        self.nc.all_engine_barrier()
        self.nc.clear_and_free_semaphores([s.h for s in self.sems])
        self.nc.all_engine_barrier()

    def mm(self, out, lhsT, rhs, start, stop, reads, writes, signal):
        return self.op("pe", lambda e: e.matmul(out, lhsT=lhsT, rhs=rhs, start=start, stop=stop,
                                                skip_group_check=True),
                       reads=reads, writes=writes, signal=signal)

    def tr(self, out, in_, ident, reads, writes, signal):
        return self.op("pe", lambda e: e.transpose(out, in_, ident), reads=reads, writes=writes, signal=signal)


class Scope:
    def __init__(self, k):
        self.k = k
        self.es = ExitStack()

    def __enter__(self):
        self.sem_idx0 = self.k.slot_sem_idx
        return self

    def __exit__(self, *a):
        self.k.barrier()
        self.es.close()
        self.k.slot_sem_idx = self.sem_idx0
        return False

    def sbuf(self, name, shape, dt):
        self.k.uid += 1
        return self.es.enter_context(self.k.nc.sbuf_tensor(f"{name}_{self.k.uid}", list(shape), dt))

    def psum(self, name, shape, dt=F32):
        self.k.uid += 1
        return self.es.enter_context(self.k.nc.psum_tensor(f"{name}_{self.k.uid}", list(shape), dt))

    def slots(self, name, n, shape, dt):
        return Slots(self, name, n, shape, dt)


class Slots:
    def __init__(self, sc, name, n, shape, dt, psum=False):
        self.n = n
        if psum:
            self.t = [sc.psum(f"{name}{i}", shape, dt) for i in range(n)]
        else:
            self.t = [sc.sbuf(f"{name}{i}", shape, dt) for i in range(n)]
        self.b = [Buf(f"{name}{i}") for i in range(n)]
        if psum:
            self.s = [None] * n
        else:
            self.s = []
            for i in range(n):
                self.s.append(sc.k.dsem(f"S{sc.k.slot_sem_idx}"))
                sc.k.slot_sem_idx += 1
        self.i = 0

    def next(self):
        i = self.i % self.n
        self.i += 1
        return self.t[i], self.b[i], self.s[i]


def fold_view(ap2d, dil, n0, n):
    Ld = S // dil
    if dil == 1:
        return ap2d[:, n0:n0 + n]
    v = ap2d.rearrange("p (j r) -> p r j", r=dil)
    if n <= Ld:
        r, j0 = divmod(n0, Ld)
        assert j0 + n <= Ld
        return v[:, r, j0:j0 + n]
    assert n % Ld == 0 and n0 % Ld == 0
    return v[:, n0 // Ld:(n0 + n) // Ld, :]


def like_fold(ap2d, dil, n):
    Ld = S // dil
    if dil == 1 or n <= Ld:
        return ap2d
    return ap2d.rearrange("p (a j) -> p a j", j=Ld)


class Ctx:
    pass


def stage_norm(k, C, src, g_row, tok_major_dst=None, featT_dst=None, xnT_sb=None, xnT_bufs=None,
               xn_sb=None, xn_bufs=None, router=None):
    with Scope(k) as sc:
        hs = sc.slots("nh", 3, [128, D_MODEL], F32)
        gbc = sc.sbuf("gbc", [128, D_MODEL], F32)
        gb = Buf("gbc")
        k.dma("sp", gbc[:], g_row.partition_broadcast(128), k.dsem("misc"), writes=[gb])
        junk = sc.sbuf("junk", [128, D_MODEL], BF16)
        jb = Buf("junk")
        ss = sc.sbuf("ss", [128, 16], F32)
        rs = sc.sbuf("rs", [128, 16], F32)
        ssb = [Buf(f"ss{i}") for i in range(16)]
        rsb = [Buf(f"rs{i}") for i in range(16)]
        if xn_sb is None:
            xns = sc.slots("xnt", 2, [128, D_MODEL], BF16)
        pst = Slots(sc, "npt", 2, [128, 1024], BF16, psum=True)
        if router is not None:
            wr_sb, wr_b, lg_sb, lg_b = router
            psr = Slots(sc, "npr", 2, [128, 16], F32, psum=True)
            xts = sc.slots("nxT", 2, [128, 16, 128], BF16)
        ident, identb = C.ident_bf, C.ident_bf_b
        srcv = src.rearrange("(i p) d -> i p d", p=128)
        for i in range(16):
            ht, hb, hsem = hs.next()
            k.dma("sp", ht[:], srcv[i], hsem, writes=[hb])
            k.op("act", lambda e: e.activation(out=junk[:], in_=ht[:], func=AF.Square, accum_out=ss[:, i:i + 1]),
                 reads=[hb], writes=[jb, ssb[i]])
            k.op("dve", lambda e: e.tensor_scalar(out=rs[:, i:i + 1], in0=ss[:, i:i + 1], scalar1=1.0 / D_MODEL,
                                                  scalar2=EPS, op0=ALU.mult, op1=ALU.add),
                 reads=[ssb[i]], writes=[rsb[i]])
            k.op("act", lambda e: e.activation(out=rs[:, i:i + 1], in_=rs[:, i:i + 1], func=AF.Sqrt),
                 reads=[rsb[i]], writes=[rsb[i]])
            k.op("dve", lambda e: e.reciprocal(out=rs[:, i:i + 1], in_=rs[:, i:i + 1]),
                 reads=[rsb[i]], writes=[rsb[i]])
            if xn_sb is None:
                xt, xb, _ = xns.next()
                xt_ap = xt[:]
            else:
                xt_ap = xn_sb[:, i, :]
                xb = xn_bufs[i]
            k.op("dve", lambda e: e.scalar_tensor_tensor(out=xt_ap, in0=ht[:], scalar=rs[:, i:i + 1], in1=gbc[:],
                                                         op0=ALU.mult, op1=ALU.mult),
                 reads=[hb, rsb[i], gb], writes=[xb])
            if xnT_sb is None and router is None:
                continue
            if router is not None:
                xT, xTb, _ = xts.next()
            for half in range(2):
                pt, pb, _ = pst.next()
                for j in range(8):
                    c = half * 8 + j
                    k.tr(pt[:, j * 128:(j + 1) * 128], xt_ap[:, c * 128:(c + 1) * 128], ident[:],
                         reads=[xb, identb], writes=[pb], signal=(j == 7))
                src_ap = pt[:].rearrange("p (c t) -> p c t", t=128)
                if xnT_sb is not None:
                    dst = xnT_sb[:, half * 8:(half + 1) * 8, i * 128:(i + 1) * 128]
                    wb_ = [xnT_bufs[i]]
                else:
                    dst = xT[:, half * 8:(half + 1) * 8, :]
                    wb_ = [xTb]
                if half == 0:
                    k.op("act", lambda e: e.copy(out=dst, in_=src_ap), reads=[pb], writes=wb_)
                else:
                    k.op("dve", lambda e: e.tensor_copy(out=dst, in_=src_ap), reads=[pb], writes=wb_)
            if router is not None:
                pr, prb, _ = psr.next()
                for c in range(16):
                    k.mm(pr[:], lhsT=xT[:, c, :], rhs=wr_sb[:, c, :], start=(c == 0), stop=(c == 15),
                         reads=[xTb, wr_b], writes=[prb], signal=(c == 15))
                k.op("act", lambda e: e.copy(out=lg_sb[:, i, :], in_=pr[:]), reads=[prb], writes=[lg_b])
        if featT_dst is not None:
            k.dma("sp", featT_dst.rearrange("(c p) s -> p c s", p=128), xnT_sb[:], k.dsem("misc"),
                  reads=xnT_bufs, writes=[C.scr_b["xnT"]])
        if xnT_sb is not None or xn_sb is not None:
            pass


def stage_proj(k, C, l, xnT, xnTb):
    w_in = C.w_in[l]
    jobs = []
    for qk in range(2):
        for g in range(3):
            c0 = (qk * 3 + g) * 512
            dst = (C.A_qT if qk == 0 else C.A_kT)[g * 512:(g + 1) * 512, :]
            jobs.append(("fm", c0, 512, A_DIL[g], dst, BF16))
    for g in range(3):
        jobs.append(("tm", (6 + g) * 512, 512, A_DIL[g], C.A_v[g][:, :], BF16))
    for c0, n in ((0, 512), (512, 288)):
        jobs.append(("tm", 4608 + c0, n, 1, C.B_tm[:, c0:c0 + n], F32))
    for c0, n in ((0, 512), (512, 128)):
        jobs.append(("tm", 5408 + c0, n, 1, C.C_tm[:, c0:c0 + n], F32))
    jobs.append(("tm", 6048, 128, 1, C.C_v[:, :], BF16))
    jobs.append(("fm", 6176, 512, 1, C.D_qT[:, :], BF16))
    jobs.append(("fm", 6688, 128, 1, C.D_kT[:, :], BF16))
    jobs.append(("tm", 6816, 128, 1, C.D_v[:, :], BF16))

    with Scope(k) as sc:
        ws = sc.slots("pw", 3, [128, 16, 512], BF16)
        stf = sc.slots("pof", 2, [128, S], BF16)
        stt32 = sc.slots("pot32", 3, [128, 512], F32)
        stt16 = sc.slots("pot16", 3, [128, 512], BF16)
        ps = Slots(sc, "pps", 4, [128, 512], F32, psum=True)
        wv = w_in.rearrange("(c p) n -> p c n", p=128)
        loaded = {}

        def load(j):
            if j >= len(jobs) or j in loaded:
                return
            _, c0, n, _, _, _ = jobs[j]
            wt, wb, wsem = ws.next()
            k.dma("pool", wt[:, :, 0:n], wv[:, :, c0:c0 + n], wsem, writes=[wb])
            loaded[j] = (wt, wb)

        load(0)
        load(1)
        ev = 0
        for j, (mode, c0, n, dil, dst, dt) in enumerate(jobs):
            load(j + 2)
            wt, wb = loaded[j]
            if mode == "fm":
                for sub in range(n // 128):
                    ot, ob, osem = stf.next()
                    for tg in range(4):
                        p, pb, _ = ps.next()
                        for c in range(16):
                            rhs = fold_view(xnT[:, c, :], dil, tg * 512, 512)
                            k.mm(like_fold(p[:], dil, 512), lhsT=wt[:, c, sub * 128:(sub + 1) * 128], rhs=rhs,
                                 start=(c == 0), stop=(c == 15), reads=[wb] + xnTb, writes=[pb], signal=(c == 15))
                        o_ap = ot[:, tg * 512:(tg + 1) * 512]
                        if ev % 2 == 0:
                            k.op("act", lambda e: e.copy(out=o_ap, in_=p[:]), reads=[pb], writes=[ob])
                        else:
                            k.op("dve", lambda e: e.tensor_copy(out=o_ap, in_=p[:]), reads=[pb], writes=[ob])
                        ev += 1
                    k.dma("sp", dst[sub * 128:(sub + 1) * 128, :], ot[:], osem, reads=[ob], writes=[C.scr_b["proj"]])
            else:
                for i in range(16):
                    ot, ob, osem = (stt32 if dt == F32 else stt16).next()
                    p, pb, _ = ps.next()
                    for c in range(16):
                        lhsT = fold_view(xnT[:, c, :], dil, i * 128, 128)
                        k.mm(p[:, 0:n], lhsT=lhsT, rhs=wt[:, c, 0:n], start=(c == 0), stop=(c == 15),
                             reads=[wb] + xnTb, writes=[pb], signal=(c == 15))
                    o_ap = ot[:, 0:n]
                    if ev % 2 == 0:
                        k.op("act", lambda e: e.copy(out=o_ap, in_=p[:, 0:n]), reads=[pb], writes=[ob])
                    else:
                        k.op("dve", lambda e: e.tensor_copy(out=o_ap, in_=p[:, 0:n]), reads=[pb], writes=[ob])
                    ev += 1
                    k.dma("sp", dst[i * 128:(i + 1) * 128, :], ot[:, 0:n], osem, reads=[ob], writes=[C.scr_b["proj"]])


def attn_units(C):
    units = []
    for h in range(8):
        for g in range(3):
            r0 = g * 512 + h * 64
            units.append(dict(q=C.A_qT[r0:r0 + 64, :], k=C.A_kT[r0:r0 + 64, :], v=("A%d" % g, h),
                              dk=64, kind="band", dil=A_DIL[g], bidx=g * 8 + h, scale=0.125, first=(g == 0),
                              last=(g == 2), branch=0, h=h, sink=False))
    for h in range(8):
        units.append(dict(q=C.B_qT[h], k=C.B_kT[h], v=("B", h), dk=96, kind="dense",
                          scale=96 ** -0.5, first=True, last=True, branch=1, h=h, sink=False))
    for h in range(8):
        kv = h // 4
        units.append(dict(q=C.C_qT[h], k=C.C_kT[kv], v=("C", kv), dk=64, kind="dense",
                          scale=0.125, first=True, last=True, branch=2, h=h, sink=False))
    for h in range(8):
        kv = h // 4
        units.append(dict(q=C.D_qT[h * 64:(h + 1) * 64, :], k=C.D_kT[kv * 64:(kv + 1) * 64, :],
                          v=("D", kv), dk=64, kind="band", dil=1, bidx=24 + h, scale=0.125,
                          first=True, last=True, branch=3, h=h, sink=True))
    return units


def stage_attn(k, C, l, units):
    LAG = 2
    with Scope(k) as sc:
        bias_sb = sc.sbuf("bias_sb", [128, 32, 384], BF16)
        biasb = Buf("bias")
        k.dma("pool", bias_sb[:], C.c_bias.rearrange("t p n -> p t n"), k.dsem("misc"), writes=[biasb])
        sinkexp = sc.sbuf("sinkexp", [65, 8], F32)
        sinkb = Buf("sink")
        k.dma("sp", sinkexp[64:65, :], C.sink_d[l:l + 1, :], k.dsem("misc"), writes=[sinkb])
        k.op("act", lambda e: e.activation(out=sinkexp[64:65, :], in_=sinkexp[64:65, :], func=AF.Exp),
             reads=[sinkb], writes=[sinkb])
        qk_slots = {}
        for dk_ in sorted({u["dk"] for u in units}):
            qs_ = sc.slots(f"aq{dk_}", 2, [128, S], BF16)
            ks_ = sc.slots(f"ak{dk_}", 2, [128, S], BF16)
            for sl_ in (qs_, ks_):
                for i in range(2):
                    k.op("pool", lambda e: e.memset(sl_.t[i][dk_:128, :], 0.0), writes=[sl_.b[i]])
            qk_slots[dk_] = (qs_, ks_)
        vsrc = {"A0": (C.A_v[0], 8, "proj"), "A1": (C.A_v[1], 8, "proj"), "A2": (C.A_v[2], 8, "proj"),
                "B": (C.B_v, 8, "qkv"), "C": (C.C_v, 2, "proj"), "D": (C.D_v, 2, "proj")}
        vall = {}
        vstg = sc.slots("avst", 2, [128, 16, 512], BF16)
        for name in sorted({u["v"][0] for u in units}):
            src, nh, sb = vsrc[name]
            vt_ = sc.sbuf("av" + name, [128, 16, nh, 65], BF16)
            vb_ = Buf("av" + name)
            k.op("pool", lambda e: e.memset(vt_[:], 1.0), writes=[vb_])
            stg, stgb, stgsem = vstg.next()
            k.dma("sp", stg[:, :, 0:nh * 64], src.rearrange("(i p) n -> p i n", p=128), stgsem,
                  reads=[C.scr_b[sb]], writes=[stgb])
            k.op("pool", lambda e: e.tensor_copy(out=vt_[:, :, :, 0:64],
                                                 in_=stg[:, :, 0:nh * 64].rearrange("p i (h d) -> p i h d", d=64)),
                 reads=[stgb], writes=[vb_])
            vall[name] = (vt_, vb_)
        pts = sc.slots("apt", 4, [128, 512], BF16)
        accs = sc.slots("aacc", 2, [65, S], F32)
        rden = sc.sbuf("rden", [128, S], F32)
        rdenb = Buf("rden")
        k.op("pool", lambda e: e.memset(rden[:], 0.0), writes=[rdenb])
        sel64 = sc.sbuf("sel64", [128, 64], F32)
        sel64b = Buf("sel64")
        k.dma("sp", sel64[:], C.c_sel64, k.dsem("misc"), writes=[sel64b])
        ots = sc.slots("aot", 2, [64, S], BF16)
        ps_s = Slots(sc, "aps", 3, [128, 512], F32, psum=True)
        ps_a = Slots(sc, "apa", 2, [128, 512], F32, psum=True)
        ps_b = Slots(sc, "apb", 2, [64, 512], F32, psum=True)
        ident, identb = C.ident_bf, C.ident_bf_b
        loaded = {}

        def load(ui):
            if ui >= len(units) or ui in loaded:
                return
            u = units[ui]
            qs, ks = qk_slots[u["dk"]]
            qt, qb, qsem = qs.next()
            kt, kb, ksem = ks.next()
            vt_, vb = vall[u["v"][0]]
            vt = vt_[:, :, u["v"][1], :]
            dk = u["dk"]
            k.dma("sp", qt[0:dk, :], u["q"], qsem, reads=[C.scr_b["proj"], C.scr_b["qkv"]], writes=[qb])
            k.dma("sp", kt[0:dk, :], u["k"], ksem, reads=[C.scr_b["proj"], C.scr_b["qkv"]], writes=[kb])
            loaded[ui] = (qt, qb, kt, kb, vt, vb)

        load(0)
        pending = []
        for ui, u in enumerate(units):
            load(ui + 1)
            qt, qb, kt, kb, vt, vb = loaded.pop(ui)
            dk = u["dk"]
            scale = u["scale"]
            if u["first"]:
                acc_sb, acc_b, _ = accs.next()
            first_grp = u["first"]
            steps = []
            if u["kind"] == "dense":
                for qg in range(4):
                    for kc in range(16):
                        steps.append(("d", qg, kc))
            else:
                dil = u["dil"]
                Ld = S // dil
                nb = Ld // 128
                for z in range(dil):
                    for c in range(nb):
                        steps.append(("b", z, c))
            state = {}
            accst = dict(cb=0, n0=0, acc=None, accb=None)

            def emit_scores(i):
                st = steps[i]
                s, sb, _ = ps_s.next()
                pt, ptb, _ = pts.next()
                if st[0] == "d":
                    _, qg, kc = st
                    k.mm(s[:, :], lhsT=kt[:, kc * 128:(kc + 1) * 128], rhs=qt[:, qg * 512:(qg + 1) * 512],
                         start=True, stop=True, reads=[kb, qb], writes=[sb], signal=True)
                    k.op("act", lambda e: e.activation(out=pt[:, :], in_=s[:, :], func=AF.Exp, scale=scale),
                         reads=[sb], writes=[ptb])
                    state[i] = (pt, ptb, None)
                else:
                    _, z, c = st
                    chunks = [t for t in range(3) if 0 <= c - 1 + t < nb]
                    t0, t1 = chunks[0], chunks[-1] + 1
                    k.mm(s[:, t0 * 128:t1 * 128], lhsT=ident[:], rhs=bias_sb[:, u["bidx"], t0 * 128:t1 * 128],
                         start=True, stop=False, reads=[identb, biasb], writes=[sb], signal=False)
                    for t in chunks:
                        kblk = c - 1 + t
                        k.mm(s[:, t * 128:(t + 1) * 128],
                             lhsT=kt[:, z * Ld + kblk * 128: z * Ld + kblk * 128 + 128],
                             rhs=qt[:, z * Ld + c * 128: z * Ld + c * 128 + 128],
                             start=False, stop=(t == chunks[-1]), reads=[kb, qb], writes=[sb],
                             signal=(t == chunks[-1]))
                    k.op("act", lambda e: e.activation(out=pt[:, t0 * 128:t1 * 128], in_=s[:, t0 * 128:t1 * 128],
                                                       func=AF.Exp, scale=scale),
                         reads=[sb], writes=[ptb])
                    state[i] = (pt, ptb, chunks)

            def flush(n0, n, dil_):
                acc, accb = accst["acc"], accst["accb"]
                dst = fold_view(acc_sb[0:65, :], dil_, n0, n)
                src = like_fold(acc[0:65, 0:n], dil_, n)
                if first_grp:
                    k.op("act", lambda e: e.copy(out=dst, in_=src), reads=[accb], writes=[acc_b])
                else:
                    k.op("dve", lambda e: e.tensor_tensor(out=dst, in0=src, in1=dst, op=ALU.add),
                         reads=[accb, acc_b], writes=[acc_b])

            def emit_pv(i):
                st = steps[i]
                pt, ptb, chunks = state.pop(i)
                if st[0] == "d":
                    _, qg, kc = st
                    if kc == 0:
                        accst["acc"], accst["accb"], _ = ps_a.next()
                    acc, accb = accst["acc"], accst["accb"]
                    k.mm(acc[0:65, :], lhsT=vt[:, kc, :], rhs=pt[:, :], start=(kc == 0), stop=(kc == 15),
                         reads=[vb, ptb], writes=[accb], signal=(kc == 15))
                    if kc == 15:
                        flush(qg * 512, 512, 1)
                else:
                    _, z, c = st
                    if accst["cb"] == 0:
                        accst["acc"], accst["accb"], _ = ps_a.next()
                        accst["n0"] = z * Ld + c * 128
                    acc, accb = accst["acc"], accst["accb"]
                    cb = accst["cb"]
                    for t in chunks:
                        kblk = c - 1 + t
                        k.mm(acc[0:65, cb * 128:(cb + 1) * 128], lhsT=vt[:, (z * Ld + kblk * 128) // 128, :],
                             rhs=pt[:, t * 128:(t + 1) * 128], start=(t == chunks[0]), stop=(t == chunks[-1]),
                             reads=[vb, ptb], writes=[accb], signal=(t == chunks[-1]))
                    accst["cb"] += 1
                    if accst["cb"] == 4 or i == len(steps) - 1:
                        flush(accst["n0"], accst["cb"] * 128, dil)
                        accst["cb"] = 0

            DEFER = 6
            for i in range(len(steps) + LAG):
                if i < len(steps):
                    emit_scores(i)
                if i - LAG >= 0:
                    emit_pv(i - LAG)
                if i == DEFER and pending:
                    pending.pop()()

            if u["last"]:
                if pending:
                    pending.pop()()
                h = u["h"]
                if u["sink"]:
                    k.op("dve", lambda e: e.tensor_scalar(out=acc_sb[64:65, :], in0=acc_sb[64:65, :],
                                                          scalar1=sinkexp[64:65, h:h + 1], scalar2=None, op0=ALU.add),
                         reads=[acc_b, sinkb], writes=[acc_b])
                if u["kind"] == "band":
                    k.op("act", lambda e: e.activation(out=rden[64:65, :], in_=acc_sb[64:65, :], func=AF.Ln),
                         reads=[acc_b], writes=[rdenb])
                    k.op("act", lambda e: e.activation(out=rden[64:65, :], in_=rden[64:65, :], func=AF.Exp, scale=-1.0),
                         reads=[rdenb], writes=[rdenb])
                else:
                    k.op("dve", lambda e: e.reciprocal(out=rden[64:65, :], in_=acc_sb[64:65, :]),
                         reads=[acc_b], writes=[rdenb])

                def part2(acc_sb=acc_sb, acc_b=acc_b, h=h, branch=u["branch"]):
                    ot, ob, osem = ots.next()
                    for qg in range(4):
                        bc, bcb, _ = ps_b.next()
                        k.mm(bc[0:64, :], lhsT=sel64[:, :], rhs=rden[:, qg * 512:(qg + 1) * 512],
                             start=True, stop=True, reads=[rdenb, sel64b], writes=[bcb], signal=True)
                        k.op("dve", lambda e: e.tensor_tensor(out=ot[0:64, qg * 512:(qg + 1) * 512],
                                                              in0=acc_sb[0:64, qg * 512:(qg + 1) * 512], in1=bc[0:64, :],
                                                              op=ALU.mult),
                             reads=[acc_b, bcb], writes=[ob])
                    r0 = branch * 512 + h * 64
                    k.dma("sp", C.oT_d[r0:r0 + 64, :], ot[0:64, :], osem, reads=[ob], writes=[C.scr_b["oT"]])

                pending.append(part2)
        while pending:
            pending.pop()()


def _rstd(k, ss_ap, rs_ap, n, inv, ssb, rsb):
    k.op("dve", lambda e: e.tensor_scalar(out=rs_ap, in0=ss_ap, scalar1=inv, scalar2=EPS, op0=ALU.mult, op1=ALU.add),
         reads=[ssb], writes=[rsb])
    k.op("act", lambda e: e.activation(out=rs_ap, in_=rs_ap, func=AF.Sqrt), reads=[rsb], writes=[rsb])
    k.op("dve", lambda e: e.reciprocal(out=rs_ap, in_=rs_ap), reads=[rsb], writes=[rsb])


def _rope(k, x1, x2, cos, sin, o1, o2, shape, tmp, rb, wb, tb):
    t1, t2 = tmp
    k.op("dve", lambda e: e.tensor_tensor(out=t1, in0=x1, in1=cos, op=ALU.mult), reads=rb, writes=[tb])
    k.op("dve", lambda e: e.tensor_tensor(out=t2, in0=x2, in1=sin, op=ALU.mult), reads=rb, writes=[tb])
    k.op("dve", lambda e: e.tensor_tensor(out=o1, in0=t1, in1=t2, op=ALU.subtract), reads=[tb], writes=wb)
    k.op("dve", lambda e: e.tensor_tensor(out=t1, in0=x1, in1=sin, op=ALU.mult), reads=rb + [tb], writes=[tb])
    k.op("dve", lambda e: e.tensor_tensor(out=t2, in0=x2, in1=cos, op=ALU.mult), reads=rb, writes=[tb])
    k.op("dve", lambda e: e.tensor_tensor(out=o2, in0=t1, in1=t2, op=ALU.add), reads=[tb], writes=wb)


def stage_prepB(k, C, l):
    with Scope(k) as sc:
        ms = k.dsem("misc")
        rope = sc.sbuf("ropeB", [128, 16, 32], F32)
        ropeb = Buf("ropeB")
        k.dma("sp", rope[:], C.c_ropeB.rearrange("(i p) n -> p i n", p=128), ms, writes=[ropeb])
        gq = sc.sbuf("gq", [128, 512], F32)
        gkv = sc.sbuf("gkv", [128, 256], F32)
        gb = Buf("gB")
        k.dma("sp", gq[:], C.g_mla_q[l].partition_broadcast(128), ms, writes=[gb])
        k.dma("sp", gkv[:], C.g_mla_kv[l].partition_broadcast(128), ms, writes=[gb])
        wuq = sc.sbuf("wuq", [128, 4, 768], BF16)
        wukv = sc.sbuf("wukv", [128, 2, 1024], BF16)
        wb = Buf("wB")
        k.dma("pool", wuq[:], C.w_mla_uq[l].rearrange("(c p) n -> p c n", p=128), ms, writes=[wb])
        k.dma("pool", wukv[:], C.w_mla_ukv[l].rearrange("(c p) n -> p c n", p=128), ms, writes=[wb])
        bts = sc.slots("bt", 2, [128, 800], F32)
        junk = sc.sbuf("bjunk", [128, 512], BF16)
        jb = Buf("bjunk")
        ss = sc.sbuf("bss", [128, 32], F32)
        rs = sc.sbuf("brs", [128, 32], F32)
        cn = sc.slots("bcn", 2, [128, 768], BF16)
        cT = sc.slots("bcT", 2, [128, 6, 128], BF16)
        qtm = sc.slots("bqtm", 2, [128, 8, 96], BF16)
        ktm = sc.slots("bktm", 2, [128, 8, 96], BF16)
        tmp1 = sc.sbuf("btmp1", [128, 8, 16], F32)
        tmp2 = sc.sbuf("btmp2", [128, 8, 16], F32)
        tb = Buf("btmp")
        kr = sc.sbuf("bkr", [128, 32], F32)
        krb = Buf("bkr")
        vtm = sc.sbuf("bvtm", [128, 16, 512], BF16)
        vtmb = Buf("bvtm")
        qTs = sc.sbuf("bqTs", [96, 8, S], BF16)
        kTs = sc.sbuf("bkTs", [96, 8, S], BF16)
        qTb = Buf("bqTs")
        kTb = Buf("bkTs")
        pt_in = Slots(sc, "bpi", 1, [128, 1024], BF16, psum=True)
        psq = Slots(sc, "bpq", 1, [128, 512], F32, psum=True)
        psqr = Slots(sc, "bpqr", 1, [128, 256], F32, psum=True)
        pskv = Slots(sc, "bpkv", 1, [128, 512], F32, psum=True)
        psv = Slots(sc, "bpv", 1, [128, 512], F32, psum=True)
        pt_q = Slots(sc, "bptq", 1, [96, 1024], BF16, psum=True)
        pt_k = Slots(sc, "bptk", 1, [96, 1024], BF16, psum=True)
        ident, identb = C.ident_bf, C.ident_bf_b
        for i in range(16):
            bt, bb, bsem = bts.next()
            k.dma("sp", bt[:], C.B_tm[i * 128:(i + 1) * 128, :], bsem, reads=[C.scr_b["proj"]], writes=[bb])
            ssb, rsb = Buf("bss"), Buf("brs")
            k.op("act", lambda e: e.activation(out=junk[:, 0:512], in_=bt[:, 0:512], func=AF.Square,
                                               accum_out=ss[:, 2 * i:2 * i + 1]), reads=[bb], writes=[jb, ssb])
            k.op("act", lambda e: e.activation(out=junk[:, 0:256], in_=bt[:, 512:768], func=AF.Square,
                                               accum_out=ss[:, 2 * i + 1:2 * i + 2]), reads=[bb], writes=[jb, ssb])
            k.op("dve", lambda e: e.tensor_scalar(out=ss[:, 2 * i:2 * i + 1], in0=ss[:, 2 * i:2 * i + 1], scalar1=0.5,
                                                  scalar2=None, op0=ALU.mult), reads=[ssb], writes=[ssb])
            _rstd(k, ss[:, 2 * i:2 * i + 2], rs[:, 2 * i:2 * i + 2], 2, 1.0 / 256, ssb, rsb)
            cnt, cnb, _ = cn.next()
            k.op("dve", lambda e: e.scalar_tensor_tensor(out=cnt[:, 0:512], in0=bt[:, 0:512], scalar=rs[:, 2 * i:2 * i + 1],
                                                         in1=gq[:], op0=ALU.mult, op1=ALU.mult),
                 reads=[bb, rsb, gb], writes=[cnb])
            k.op("dve", lambda e: e.scalar_tensor_tensor(out=cnt[:, 512:768], in0=bt[:, 512:768],
                                                         scalar=rs[:, 2 * i + 1:2 * i + 2], in1=gkv[:], op0=ALU.mult,
                                                         op1=ALU.mult),
                 reads=[bb, rsb, gb], writes=[cnb])
            pin, pinb, _ = pt_in.next()
            for c in range(6):
                k.tr(pin[:, c * 128:(c + 1) * 128], cnt[:, c * 128:(c + 1) * 128], ident[:], reads=[cnb, identb],
                     writes=[pinb], signal=(c == 5))
            ct, ctb, _ = cT.next()
            k.op("act", lambda e: e.copy(out=ct[:], in_=pin[:, 0:768].rearrange("p (c t) -> p c t", t=128)),
                 reads=[pinb], writes=[ctb])
            pq, pqb, _ = psq.next()
            pqr, pqrb, _ = psqr.next()
            wuqv = wuq[:].rearrange("p c (h d) -> p c h d", d=96)
            for kc in range(4):
                k.mm(pq[:, :].rearrange("p (h d) -> p h d", d=64), lhsT=ct[:, kc, :], rhs=wuqv[:, kc, :, 0:64],
                     start=(kc == 0), stop=(kc == 3), reads=[ctb, wb], writes=[pqb], signal=(kc == 3))
            for kc in range(4):
                k.mm(pqr[:, :].rearrange("p (h d) -> p h d", d=32), lhsT=ct[:, kc, :], rhs=wuqv[:, kc, :, 64:96],
                     start=(kc == 0), stop=(kc == 3), reads=[ctb, wb], writes=[pqrb], signal=(kc == 3))
            pkv, pkvb, _ = pskv.next()
            pv, pvb, _ = psv.next()
            wukvv = wukv[:].rearrange("p c (h d) -> p c h d", d=128)
            for kc in range(2):
                k.mm(pkv[:, :].rearrange("p (h d) -> p h d", d=64), lhsT=ct[:, 4 + kc, :], rhs=wukvv[:, kc, :, 0:64],
                     start=(kc == 0), stop=(kc == 1), reads=[ctb, wb], writes=[pkvb], signal=(kc == 1))
            for kc in range(2):
                k.mm(pv[:, :].rearrange("p (h d) -> p h d", d=64), lhsT=ct[:, 4 + kc, :], rhs=wukvv[:, kc, :, 64:128],
                     start=(kc == 0), stop=(kc == 1), reads=[ctb, wb], writes=[pvb], signal=(kc == 1))
            qt, qtb, _ = qtm.next()
            kt, ktb, _ = ktm.next()
            pqv = pq[:, :].rearrange("p (h d) -> p h d", d=64)
            pqrv = pqr[:, :].rearrange("p (h d) -> p h d", d=32)
            pkvv = pkv[:, :].rearrange("p (h d) -> p h d", d=64)
            pvv = pv[:, :].rearrange("p (h d) -> p h d", d=64)
            k.op("act", lambda e: e.copy(out=qt[:, :, 0:64], in_=pqv[:, :, 0:64]), reads=[pqb], writes=[qtb])
            cosb = rope[:, i, 0:16].unsqueeze(1).to_broadcast([128, 8, 16])
            sinb = rope[:, i, 16:32].unsqueeze(1).to_broadcast([128, 8, 16])
            _rope(k, pqrv[:, :, 0:16], pqrv[:, :, 16:32], cosb, sinb, qt[:, :, 64:80], qt[:, :, 80:96], None,
                  (tmp1[:], tmp2[:]), [pqrb, ropeb], [qtb], tb)
            k.op("act", lambda e: e.copy(out=kt[:, :, 0:64], in_=pkvv[:, :, 0:64]), reads=[pkvb], writes=[ktb])
            k.op("act", lambda e: e.copy(out=vtm[:, i, :], in_=pv[:, :]), reads=[pvb], writes=[vtmb])
            _rope(k, bt[:, 768:784], bt[:, 784:800], rope[:, i, 0:16], rope[:, i, 16:32], kr[:, 0:16], kr[:, 16:32], None,
                  (tmp1[:, 0, :], tmp2[:, 0, :]), [bb, ropeb], [krb], tb)
            k.op("dve", lambda e: e.tensor_copy(out=kt[:, :, 64:96], in_=kr[:].unsqueeze(1).to_broadcast([128, 8, 32])),
                 reads=[krb], writes=[ktb])
            for (src, srcb, pts_, dst, dstb, eng) in ((qt, qtb, pt_q, qTs, qTb, "act"), (kt, ktb, pt_k, kTs, kTb, "dve")):
                po, pob, _ = pts_.next()
                for h in range(8):
                    k.tr(po[0:96, h * 128:(h + 1) * 128], src[:, h, :], ident[:], reads=[srcb, identb], writes=[pob],
                         signal=(h == 7))
                o_ap = dst[0:96, :, i * 128:(i + 1) * 128]
                i_ap = po[0:96, :].rearrange("p (h t) -> p h t", t=128)
                if eng == "act":
                    k.op("act", lambda e: e.copy(out=o_ap, in_=i_ap), reads=[pob], writes=[dstb])
                else:
                    k.op("dve", lambda e: e.tensor_copy(out=o_ap, in_=i_ap), reads=[pob], writes=[dstb])
        st = k.dsem("bstore")
        k.dma("sp", C.B_qT.rearrange("h p s -> p h s"), qTs[:], st, reads=[qTb], writes=[C.scr_b["qkv"]])
        k.dma("sp", C.B_kT.rearrange("h p s -> p h s"), kTs[:], st, reads=[kTb], writes=[C.scr_b["qkv"]])
        k.dma("sp", C.B_v.rearrange("(i p) n -> p i n", p=128), vtm[:], st, reads=[vtmb], writes=[C.scr_b["qkv"]])


def stage_prepC(k, C, l):
    with Scope(k) as sc:
        ms = k.dsem("misc")
        rope = sc.sbuf("ropeC", [128, 16, 64], F32)
        ropeb = Buf("ropeC")
        k.dma("sp", rope[:], C.c_ropeC.rearrange("(i p) n -> p i n", p=128), ms, writes=[ropeb])
        gc = sc.sbuf("gc", [128, 10, 64], F32)
        gb = Buf("gC")
        k.dma("sp", gc[:, 0:8, :], C.g_c_q[l:l + 1, :].partition_broadcast(128).to_broadcast([128, 8, 64]), ms, writes=[gb])
        k.dma("sp", gc[:, 8:10, :], C.g_c_k[l:l + 1, :].partition_broadcast(128).to_broadcast([128, 2, 64]), ms, writes=[gb])
        cts = sc.slots("ct", 2, [128, 640], F32)
        sq = sc.sbuf("csq", [128, 640], F32)
        sqb = Buf("csq")
        xn = sc.sbuf("cxn", [128, 640], F32)
        xnb = Buf("cxn")
        ss = sc.sbuf("css", [128, 160], F32)
        rs = sc.sbuf("crs", [128, 160], F32)
        tmp1 = sc.sbuf("ctmp1", [128, 10, 2, 16], F32)
        tmp2 = sc.sbuf("ctmp2", [128, 10, 2, 16], F32)
        tb = Buf("ctmp")
        qtm = sc.slots("cqtm", 2, [128, 10, 64], BF16)
        qTs = sc.sbuf("cqTs", [64, 10, S], BF16)
        qTb = Buf("cqTs")
        pt_q = Slots(sc, "cptq", 2, [64, 1024], BF16, psum=True)
        pt_k = Slots(sc, "cptk", 2, [64, 256], BF16, psum=True)
        ident, identb = C.ident_bf, C.ident_bf_b
        for i in range(16):
            ct, cb, csem = cts.next()
            k.dma("sp", ct[:], C.C_tm[i * 128:(i + 1) * 128, :], csem, reads=[C.scr_b["proj"]], writes=[cb])
            ssb, rsb = Buf("css"), Buf("crs")
            k.op("dve", lambda e: e.tensor_tensor(out=sq[:], in0=ct[:], in1=ct[:], op=ALU.mult), reads=[cb], writes=[sqb])
            k.op("dve", lambda e: e.tensor_reduce(out=ss[:, 10 * i:10 * i + 10], in_=sq[:].rearrange("p (h d) -> p h d", d=64),
                                                  axis=AX.X, op=ALU.add), reads=[sqb], writes=[ssb])
            _rstd(k, ss[:, 10 * i:10 * i + 10], rs[:, 10 * i:10 * i + 10], 10, 1.0 / 64, ssb, rsb)
            ctv = ct[:].rearrange("p (h d) -> p h d", d=64)
            xnv = xn[:].rearrange("p (h d) -> p h d", d=64)
            k.op("dve", lambda e: e.tensor_tensor(out=xnv, in0=ctv,
                                                  in1=rs[:, 10 * i:10 * i + 10].unsqueeze(2).to_broadcast([128, 10, 64]),
                                                  op=ALU.mult), reads=[cb, rsb], writes=[xnb])
            k.op("dve", lambda e: e.tensor_tensor(out=xnv, in0=xnv, in1=gc[:], op=ALU.mult), reads=[xnb, gb], writes=[xnb])
            qt, qtb, _ = qtm.next()
            x5 = xn[:].rearrange("p (h a t f) -> p h a t f", a=2, t=2, f=16)
            o5 = qt[:].rearrange("p h (a t f) -> p h a t f", a=2, t=2, f=16)
            r4 = rope[:, i, :].rearrange("p (a t f) -> p a t f", a=2, t=2, f=16)
            cosb = r4[:, :, 0, :].unsqueeze(1).to_broadcast([128, 10, 2, 16])
            sinb = r4[:, :, 1, :].unsqueeze(1).to_broadcast([128, 10, 2, 16])
            _rope(k, x5[:, :, :, 0, :], x5[:, :, :, 1, :], cosb, sinb, o5[:, :, :, 0, :], o5[:, :, :, 1, :], None,
                  (tmp1[:], tmp2[:]), [xnb, ropeb], [qtb], tb)
            po, pob, _ = pt_q.next()
            for h in range(8):
                k.tr(po[0:64, h * 128:(h + 1) * 128], qt[:, h, :], ident[:], reads=[qtb, identb], writes=[pob],
                     signal=(h == 7))
            k.op("act", lambda e: e.copy(out=qTs[0:64, 0:8, i * 128:(i + 1) * 128],
                                         in_=po[0:64, :].rearrange("p (h t) -> p h t", t=128)), reads=[pob], writes=[qTb])
            pk, pkb, _ = pt_k.next()
            for h in range(2):
                k.tr(pk[0:64, h * 128:(h + 1) * 128], qt[:, 8 + h, :], ident[:], reads=[qtb, identb], writes=[pkb],
                     signal=(h == 1))
            k.op("act", lambda e: e.copy(out=qTs[0:64, 8:10, i * 128:(i + 1) * 128],
                                         in_=pk[0:64, :].rearrange("p (h t) -> p h t", t=128)), reads=[pkb], writes=[qTb])
        st = k.dsem("bstore")
        k.dma("sp", C.C_qT.rearrange("h p s -> p h s"), qTs[:, 0:8, :], st, reads=[qTb], writes=[C.scr_b["qkv"]])
        k.dma("sp", C.C_kT.rearrange("h p s -> p h s"), qTs[:, 8:10, :], st, reads=[qTb], writes=[C.scr_b["qkv"]])


def stage_merge(k, C, l):
    with Scope(k) as sc:
        ms = k.dsem("misc")
        xnT = sc.sbuf("mxnT", [128, 16, S], BF16)
        xb = Buf("mxnT")
        k.dma("sp", xnT[:], C.xnT_d.rearrange("(c p) s -> p c s", p=128), k.dsem("mld0"), reads=[C.scr_b["xnT"]], writes=[xb])
        oT = sc.sbuf("moT", [128, 16, S], BF16)
        ob = Buf("moT")
        k.dma("sp", oT[:], C.oT_d.rearrange("(c p) s -> p c s", p=128), k.dsem("mld1"), reads=[C.scr_b["oT"]], writes=[ob])
        bg = sc.sbuf("mbg", [128, 4, 16], F32)
        bgb = Buf("mbg")
        k.dma("sp", bg[:], C.b_gate[l].rearrange("i (c p) -> p i c", p=128), ms, writes=[bgb], allow_slow_non_contiguous=True)
        wgs = sc.slots("mwg", 2, [128, 16, 4, 128], BF16)
        wbs = sc.slots("mwb", 2, [128, 4, 4, 128], BF16)
        sgs = sc.slots("msg", 4, [128, 512], F32)
        tmp = sc.slots("mtmp", 2, [128, 512], F32)
        macc = sc.slots("macc", 2, [128, 512], F32)
        sts = sc.slots("mst", 2, [128, S], BF16)
        pg = Slots(sc, "mpg", 3, [128, 512], F32, psum=True)
        pb_ = Slots(sc, "mpb", 3, [128, 512], F32, psum=True)
        wgv = C.w_in[l][:, G0:].rearrange("(c p) (i n) -> p c i n", p=128, i=4)
        wbv = C.w_branch[l].rearrange("i (kc p) n -> p i kc n", p=128)
        loaded = {}

        def load(cc):
            if cc >= 16 or cc in loaded:
                return
            wg, wgb, wgsem = wgs.next()
            wb, wbb, wbsem = wbs.next()
            for i in range(4):
                k.dma("pool", wg[:, :, i, :], wgv[:, :, i, cc * 128:(cc + 1) * 128], wgsem, writes=[wgb])
            for i in range(4):
                k.dma("pool", wb[:, i, :, :], wbv[:, i, :, cc * 128:(cc + 1) * 128], wbsem, writes=[wbb])
            loaded[cc] = (wg, wgb, wb, wbb)

        load(0)
        for cc in range(16):
            load(cc + 1)
            wg, wgb, wb, wbb = loaded.pop(cc)
            st, stb, stsem = sts.next()
            for tg in range(4):
                tsl = slice(tg * 512, (tg + 1) * 512)
                sg_l = []
                for i in range(4):
                    p, pb, _ = pg.next()
                    for c in range(16):
                        k.mm(p[:, :], lhsT=wg[:, c, i, :], rhs=xnT[:, c, tsl], start=(c == 0), stop=(c == 15),
                             reads=[wgb, xb], writes=[pb], signal=(c == 15))
                    sg, sgb, _ = sgs.next()
                    k.op("act", lambda e: e.activation(out=sg[:, :], in_=p[:, :], func=AF.Sigmoid, bias=bg[:, i, cc:cc + 1]),
                         reads=[pb, bgb], writes=[sgb])
                    sg_l.append((sg, sgb))
                m, mb, _ = macc.next()
                for i in range(4):
                    p, pb, _ = pb_.next()
                    for kc in range(4):
                        k.mm(p[:, :], lhsT=wb[:, i, kc, :], rhs=oT[:, i * 4 + kc, tsl], start=(kc == 0), stop=(kc == 3),
                             reads=[wbb, ob], writes=[pb], signal=(kc == 3))
                    sg, sgb = sg_l[i]
                    if i == 0:
                        k.op("dve", lambda e: e.tensor_tensor(out=m[:, :], in0=p[:, :], in1=sg[:, :], op=ALU.mult),
                             reads=[pb, sgb], writes=[mb])
                    else:
                        t, tb, _ = tmp.next()
                        k.op("dve", lambda e: e.tensor_tensor(out=t[:, :], in0=p[:, :], in1=sg[:, :], op=ALU.mult),
                             reads=[pb, sgb], writes=[tb])
                        o_ap = m[:, :] if i < 3 else st[:, tsl]
                        k.op("pool", lambda e: e.tensor_tensor(out=o_ap, in0=m[:, :], in1=t[:, :], op=ALU.add),
                             reads=[mb, tb], writes=[mb] if i < 3 else [stb])
            k.dma("sp", C.mixT_d[cc * 128:(cc + 1) * 128, :], st[:, :], stsem, reads=[stb], writes=[C.scr_b["mixT"]])


def stage_out(k, C, l, h_src):
    with Scope(k) as sc:
        mixT = sc.sbuf("omixT", [128, 16, S], BF16)
        mb = Buf("omixT")
        k.dma("sp", mixT[:], C.mixT_d.rearrange("(c p) s -> p c s", p=128), k.dsem("mld0"), reads=[C.scr_b["mixT"]], writes=[mb])
        ws = sc.slots("ow", 2, [128, 16, 512], BF16)
        hs = sc.slots("oh", 2, [128, 16, 512], F32)
        ps = Slots(sc, "ops", 4, [128, 512], F32, psum=True)
        wv = C.w_out[l].rearrange("(c p) n -> p c n", p=128)
        hv = h_src.rearrange("(i p) n -> p i n", p=128)
        ho = C.h_d.rearrange("(i p) n -> p i n", p=128)
        loaded = {}

        def load(cb):
            if cb >= 4 or cb in loaded:
                return
            w, wb, wsem = ws.next()
            k.dma("pool", w[:], wv[:, :, cb * 512:(cb + 1) * 512], wsem, writes=[wb])
            h, hb, hsem = hs.next()
            k.dma("sp", h[:], hv[:, :, cb * 512:(cb + 1) * 512], hsem, reads=[C.scr_b["h"]], writes=[hb])
            loaded[cb] = (w, wb, h, hb, hsem)

        load(0)
        for cb in range(4):
            load(cb + 1)
            w, wb, h, hb, hsem = loaded.pop(cb)
            for i in range(16):
                p, pb, _ = ps.next()
                for c in range(16):
                    k.mm(p[:, :], lhsT=mixT[:, c, i * 128:(i + 1) * 128], rhs=w[:, c, :], start=(c == 0), stop=(c == 15),
                         reads=[mb, wb], writes=[pb], signal=(c == 15))
                k.op("dve", lambda e: e.tensor_tensor(out=h[:, i, :], in0=p[:, :], in1=h[:, i, :], op=ALU.add),
                     reads=[pb, hb], writes=[hb])
            k.dma("sp", ho[:, :, cb * 512:(cb + 1) * 512], h[:], hsem, reads=[hb], writes=[C.scr_b["h"]])


def moe_routing(k, C, lg, lgb, probs, pb_, mask, mkb, rank, rkb, gm, gmb, upto=99):
    ms = k.dsem("misc")
    with Scope(k) as sc:
        mx = sc.sbuf("emx", [128, 16], F32)
        mxb = Buf("emx")
        identf = sc.sbuf("eidf", [128, 128], F32)
        idfb = Buf("eidf")
        k.dma("sp", identf[:], C.c_ident, ms, writes=[idfb])
        ustr = sc.sbuf("eustr", [128, 128], BF16)
        onesb = sc.sbuf("eones", [128, 128], BF16)
        ub = Buf("eustr")
        k.dma("pool", ustr[:], C.c_ustr, ms, writes=[ub])
        k.dma("pool", onesb[:], C.c_ones, ms, writes=[ub])
        import os
        PRE = os.environ.get("PRE", "")
        k.op("dve", lambda e: e.tensor_reduce(out=mx[:], in_=lg[:], axis=AX.X, op=ALU.max), reads=[lgb], writes=[mxb])
        k.op("dve", lambda e: e.tensor_tensor(out=lg[:], in0=lg[:], in1=mx[:].unsqueeze(2).to_broadcast([128, 16, 16]),
                                              op=ALU.subtract), reads=[lgb, mxb], writes=[lgb])
        if "noexp" not in PRE:
            k.op("act", lambda e: e.activation(out=lg[:], in_=lg[:], func=AF.Exp), reads=[lgb], writes=[lgb])
        k.op("dve", lambda e: e.tensor_reduce(out=mx[:], in_=lg[:], axis=AX.X, op=ALU.add), reads=[lgb], writes=[mxb])
        k.op("dve", lambda e: e.reciprocal(out=mx[:], in_=mx[:]), reads=[mxb], writes=[mxb])
        k.op("dve", lambda e: e.tensor_tensor(out=probs[:], in0=lg[:], in1=mx[:].unsqueeze(2).to_broadcast([128, 16, 16]),
                                              op=ALU.mult), reads=[lgb, mxb], writes=[pb_])
        if upto < 1:
            return
        pT = sc.sbuf("epT", [128, S], F32)[0:16, :]
        work = sc.sbuf("ework", [128, S], F32)[0:16, :]
        mT = sc.sbuf("emT", [128, S], F32)[0:16, :]
        pTb, wkb, mTb = Buf("epT"), Buf("ework"), Buf("emT")
        m8 = sc.sbuf("em8", [128, 8], F32)[0:16, :]
        m8b = Buf("em8")
        ppT = Slots(sc, "eppT", 4, [128, 512], F32, psum=True)
        import os
        NB = int(os.environ.get("NB", "4"))
        for bnk in range(NB):
            p, pb2, _ = ppT.next()
            for j in range(4):
                i = bnk * 4 + j
                if "mm" not in os.environ.get("SKIP", ""):
                    k.mm(p[0:16, j * 128:(j + 1) * 128], lhsT=probs[:, i, :], rhs=identf[:], start=True, stop=True,
                         reads=[pb_, idfb], writes=[pb2], signal=(j == 3))
            if "act" not in os.environ.get("SKIP", ""):
                k.op("act", lambda e: e.copy(out=pT[:, bnk * 512:(bnk + 1) * 512], in_=p[0:16, :]), reads=[pb2], writes=[pTb])
            if "dve" not in os.environ.get("SKIP", ""):
                k.op("dve", lambda e: e.tensor_copy(out=work[:, bnk * 512:(bnk + 1) * 512],
                                                    in_=pT[:, bnk * 512:(bnk + 1) * 512]), reads=[pTb], writes=[wkb])
        if upto < 2:
            return
        for r in range(CAP // 8):
            k.op("dve", lambda e: e.max(out=m8[:], in_=work[:]), reads=[wkb], writes=[m8b])
            if r < CAP // 8 - 1:
                k.op("dve", lambda e: e.match_replace(out=work[:], in_to_replace=m8[:], in_values=work[:], imm_value=-1.0),
                     reads=[wkb, m8b], writes=[wkb])
        k.op("dve", lambda e: e.tensor_scalar(out=mT[:], in0=pT[:], scalar1=m8[:, 7:8], scalar2=None, op0=ALU.is_ge),
             reads=[pTb, m8b], writes=[mTb])
        if upto < 3:
            return
        pm = Slots(sc, "epm", 1, [128, 256], F32, psum=True)
        p, pb2, _ = pm.next()
        for i in range(16):
            k.mm(p[:, i * 16:(i + 1) * 16], lhsT=mT[:, i * 128:(i + 1) * 128], rhs=identf[0:16, 0:16], start=True,
                 stop=True, reads=[mTb, idfb], writes=[pb2], signal=(i == 15))
        k.op("act", lambda e: e.copy(out=mask[:].rearrange("p i e -> p (i e)"), in_=p[:, :]), reads=[pb2], writes=[mkb])
        maskbf = sc.sbuf("emaskbf", [128, 16, 16], BF16)
        mbfb = Buf("emaskbf")
        k.op("dve", lambda e: e.tensor_copy(out=maskbf[:], in_=mask[:]), reads=[mkb], writes=[mbfb])
        k.op("dve", lambda e: e.tensor_tensor(out=gm[:], in0=probs[:], in1=mask[:], op=ALU.mult), reads=[pb_, mkb],
             writes=[gmb])
        if upto < 4:
            return
        pr = Slots(sc, "epr", 1, [128, 256], F32, psum=True)
        p, pb2, _ = pr.next()
        for i in range(16):
            for j in range(i + 1):
                k.mm(p[:, i * 16:(i + 1) * 16], lhsT=(ustr[:] if j == i else onesb[:]), rhs=maskbf[:, j, :],
                     start=(j == 0), stop=(j == i), reads=[ub, mbfb], writes=[pb2], signal=(i == 15 and j == i))
        k.op("act", lambda e: e.copy(out=rank[:].rearrange("p i e -> p (i e)"), in_=p[:, :]), reads=[pb2], writes=[rkb])


def stage_moe(k, C, l, dbg=None):
    with Scope(k) as osc:
        ms = k.dsem("misc")
        xn = osc.sbuf("exn", [128, 16, D_MODEL], BF16)
        xnb = [Buf(f"exn{i}") for i in range(16)]
        lg = osc.sbuf("elg", [128, 16, 16], F32)
        lgb = Buf("elg")
        wr = osc.sbuf("ewr", [128, 16, 16], BF16)
        wrb = Buf("ewr")
        k.dma("pool", wr[:], C.w_router[l].rearrange("(c p) e -> p c e", p=128), ms, writes=[wrb])
        stage_norm(k, C, C.h_d, C.g_ffn[l], xn_sb=xn, xn_bufs=xnb, router=(wr, wrb, lg, lgb))
        probs = osc.sbuf("eprobs", [128, 16, 16], F32)
        mask = osc.sbuf("emask", [128, 16, 16], F32)
        rank = osc.sbuf("erank", [128, 16, 16], F32)
        gm = osc.sbuf("egm", [128, 16, 16], F32)
        pb_, mkb, rkb, gmb = Buf("eprobs"), Buf("emask"), Buf("erank"), Buf("egm")
        iota = osc.sbuf("eiota", [128, 256], F32)
        iob = Buf("eiota")
        k.dma("sp", iota[:], C.c_iota, ms, writes=[iob])
        moe_routing(k, C, lg, lgb, probs, pb_, mask, mkb, rank, rkb, gm, gmb)
        if dbg:
            st = k.dsem("bstore")
            k.dma("sp", C.dbg_probs.rearrange("(i p) e -> p i e", p=128), probs[:], st, reads=[pb_], writes=[C.scr_b["moe"]])
            k.dma("sp", C.dbg_mask.rearrange("(i p) e -> p i e", p=128), mask[:], st, reads=[mkb], writes=[C.scr_b["moe"]])
            k.dma("sp", C.dbg_rank.rearrange("(i p) e -> p i e", p=128), rank[:], st, reads=[rkb], writes=[C.scr_b["moe"]])
        if dbg and 6 in dbg:
            return
        with Scope(k) as sc:
            sels = sc.slots("esel", 2, [128, 16, 256], BF16)
            selg = sc.slots("eselg", 1, [128, 16, 256], BF16)
            sgT = sc.slots("esgT", 1, [128, 2, S], BF16)
            xeTs = sc.slots("exeT", 1, [128, 16, 256], BF16)
            hTs = sc.slots("ehT", 1, [128, 8, 256], BF16)
            sgt = sc.slots("esg", 2, [128, 256], F32)
            yes = sc.slots("eye", 1, [128, 2, D_MODEL], BF16)
            NW = 9
            wring = sc.slots("ewr", NW, [128, 4096], BF16)
            pgt = Slots(sc, "epg", 3, [128, 512], F32, psum=True)
            ptr = Slots(sc, "eptr", 1, [128, 1024], BF16, psum=True)
            pgu = Slots(sc, "epgu", 2, [128, 512], F32, psum=True)
            py = Slots(sc, "epy", 2, [128, 512], F32, psum=True)
            ident, identb = C.ident_bf, C.ident_bf_b
            wjobs = []
            for e in range(NE):
                for fb in range(4):
                    wjobs.append((e, 0, fb))
                    wjobs.append((e, 1, fb))
                for cb in range(4):
                    wjobs.append((e, 2, cb))
            wl = {}
            wnext = [0]

            def ensure(upto):
                while wnext[0] <= upto and wnext[0] < len(wjobs):
                    j = wnext[0]
                    e, kind, idx = wjobs[j]
                    w, wb, wsem = wring.next()
                    if kind < 2:
                        src = (C.w_exp_gate if kind == 0 else C.w_exp_up)[l, e].rearrange("(c p) f -> p c f", p=128)
                        wv_ = w[:].rearrange("p (c f) -> p c f", f=256)
                        k.dma("pool", wv_, src[:, :, idx * 256:(idx + 1) * 256], wsem, writes=[wb])
                    else:
                        src = C.w_exp_down[l, e].rearrange("(f p) n -> p f n", p=128)
                        wv_ = w[:].rearrange("p (f n) -> p f n", n=512)
                        k.dma("pool", wv_, src[:, :, idx * 512:(idx + 1) * 512], wsem, writes=[wb])
                    wl[j] = (wv_, wb)
                    wnext[0] += 1

            ensure(NW - 1)
            def build_sel(e):
                sel, selb, _ = sels.next()
                for i in range(16):
                    k.op("dve", lambda en: en.tensor_scalar(out=sel[:, i, :], in0=iota[:], scalar1=rank[:, i, e:e + 1],
                                                            scalar2=mask[:, i, e:e + 1], op0=ALU.is_equal, op1=ALU.mult),
                         reads=[iob, rkb, mkb], writes=[selb])
                return sel, selb

            def build_selg(e):
                sg_, sgb_, _ = selg.next()
                for i in range(16):
                    k.op("dve", lambda en: en.tensor_scalar(out=sg_[:, i, :], in0=iota[:], scalar1=rank[:, i, e:e + 1],
                                                            scalar2=gm[:, i, e:e + 1], op0=ALU.is_equal, op1=ALU.mult),
                         reads=[iob, rkb, gmb], writes=[sgb_])
                return sg_, sgb_

            nxt_sel = build_sel(0)
            nxt_selg = build_selg(0)
            for e in range(NE):
                sel, selb = nxt_sel
                sg_, sgb_ = nxt_selg
                xeT, xeTb, _ = xeTs.next()
                for d2 in range(8):
                    p, pb2, _ = pgt.next()
                    for dd in range(2):
                        dc = d2 * 2 + dd
                        for i in range(16):
                            k.mm(p[:, dd * 256:(dd + 1) * 256], lhsT=xn[:, i, dc * 128:(dc + 1) * 128], rhs=sel[:, i, :],
                                 start=(i == 0), stop=(i == 15), reads=[xnb[i], selb], writes=[pb2],
                                 signal=(i == 15 and dd == 1))
                    k.op("act", lambda en: en.copy(out=xeT[:, d2 * 2:d2 * 2 + 2, :],
                                                   in_=p[:, :].rearrange("p (a c) -> p a c", a=2)),
                         reads=[pb2], writes=[xeTb])
                st_, stb_, stsem = sgT.next()
                for ct in range(2):
                    for half in range(2):
                        p, pb2, _ = ptr.next()
                        for j in range(8):
                            i = half * 8 + j
                            k.tr(p[:, j * 128:(j + 1) * 128], sg_[:, i, ct * 128:(ct + 1) * 128], ident[:],
                                 reads=[sgb_, identb], writes=[pb2], signal=(j == 7))
                        k.op("dve", lambda en: en.tensor_copy(out=st_[:, ct, half * 1024:(half + 1) * 1024], in_=p[:, :]),
                             reads=[pb2], writes=[stb_])
                    k.dma("sp", C.selGT_d[:, :, 2 * e + ct, :].rearrange("i c t -> c i t"),
                          st_[:, ct, :].rearrange("c (i t) -> c i t", t=128), stsem, reads=[stb_], writes=[C.scr_b["moe"]])
                if e + 1 < NE:
                    nxt_sel = build_sel(e + 1)
                    nxt_selg = build_selg(e + 1)
                hT, hTb, _ = hTs.next()
                for fb in range(4):
                    j0 = e * 12 + fb * 2
                    ensure(j0 + NW - 1)
                    wg, wgb = wl.pop(j0)
                    wu, wub = wl.pop(j0 + 1)
                    for fc in range(2):
                        p, pb2, _ = pgu.next()
                        for (w_, wb_, off) in ((wg, wgb, 0), (wu, wub, 256)):
                            for dc in range(16):
                                k.mm(p[:, off:off + 256], lhsT=w_[:, dc, fc * 128:(fc + 1) * 128], rhs=xeT[:, dc, :],
                                     start=(dc == 0), stop=(dc == 15), reads=[wb_, xeTb], writes=[pb2],
                                     signal=(dc == 15 and off == 256))
                        s_, sb_, _ = sgt.next()
                        k.op("act", lambda en: en.activation(out=s_[:, :], in_=p[:, 0:256], func=AF.Silu), reads=[pb2],
                             writes=[sb_])
                        k.op("dve", lambda en: en.tensor_tensor(out=hT[:, fb * 2 + fc, :], in0=p[:, 256:512], in1=s_[:, :],
                                                                op=ALU.mult), reads=[pb2, sb_], writes=[hTb])
                ye, yeb, yesem = yes.next()
                for cb in range(4):
                    j = e * 12 + 8 + cb
                    ensure(j + NW - 1)
                    wd, wdb = wl.pop(j)
                    for ct in range(2):
                        p, pb2, _ = py.next()
                        for f in range(8):
                            k.mm(p[:, :], lhsT=hT[:, f, ct * 128:(ct + 1) * 128], rhs=wd[:, f, :], start=(f == 0),
                                 stop=(f == 7), reads=[hTb, wdb], writes=[pb2], signal=(f == 7))
                        if ct == 0:
                            k.op("act", lambda en: en.copy(out=ye[:, ct, cb * 512:(cb + 1) * 512], in_=p[:, :]), reads=[pb2],
                                 writes=[yeb])
                        else:
                            k.op("dve", lambda en: en.tensor_copy(out=ye[:, ct, cb * 512:(cb + 1) * 512], in_=p[:, :]),
                                 reads=[pb2], writes=[yeb])
                k.dma("sp", C.ye_d[:, 2 * e:2 * e + 2, :], ye[:], yesem, reads=[yeb], writes=[C.scr_b["moe"]])
    if dbg and 7 in dbg:
        return
    with Scope(k) as sc:
        yes = sc.slots("sye", 2, [128, 32, 512], BF16)
        sls = sc.slots("ssl", 2, [128, 32, 128], BF16)
        hs = sc.slots("sh", 2, [128, 16, 512], F32)
        ps = Slots(sc, "sps", 3, [128, 512], F32, psum=True)
        hv = C.h_d.rearrange("(i p) n -> p i n", p=128)
        for cb in range(4):
            ye, yeb, yesem = yes.next()
            k.dma("sp", ye[:], C.ye_d[:, :, cb * 512:(cb + 1) * 512], yesem, reads=[C.scr_b["moe"]], writes=[yeb])
            h, hb, hsem = hs.next()
            k.dma("sp", h[:], hv[:, :, cb * 512:(cb + 1) * 512], hsem, reads=[C.scr_b["h"]], writes=[hb])
            for i in range(16):
                sl, slb, slsem = sls.next()
                k.dma("sp", sl[:], C.selGT_d[i], slsem, reads=[C.scr_b["moe"]], writes=[slb])
                p, pb2, _ = ps.next()
                for et in range(32):
                    k.mm(p[:, :], lhsT=sl[:, et, :], rhs=ye[:, et, :], start=(et == 0), stop=(et == 31), reads=[slb, yeb],
                         writes=[pb2], signal=(et == 31))
                k.op("dve", lambda en: en.tensor_tensor(out=h[:, i, :], in0=p[:, :], in1=h[:, i, :], op=ALU.add),
                     reads=[pb2, hb], writes=[hb])
            k.dma("sp", hv[:, :, cb * 512:(cb + 1) * 512], h[:], hsem, reads=[hb], writes=[C.scr_b["h"]])


def stage_final(k, C, yb):
    with Scope(k) as sc:
        hs = sc.slots("fh", 3, [128, D_MODEL], F32)
        os_ = sc.slots("fo", 2, [128, D_MODEL], F32)
        gbc = sc.sbuf("fgbc", [128, D_MODEL], F32)
        gb = Buf("fgbc")
        k.dma("sp", gbc[:], C.g_final.partition_broadcast(128), k.dsem("misc"), writes=[gb])
        junk = sc.sbuf("fjunk", [128, D_MODEL], BF16)
        jb = Buf("fjunk")
        ss = sc.sbuf("fss", [128, 16], F32)
        rs = sc.sbuf("frs", [128, 16], F32)
        for i in range(16):
            ht, hb, hsem = hs.next()
            k.dma("sp", ht[:], C.h_d[i * 128:(i + 1) * 128, :], hsem, reads=[C.scr_b["h"]], writes=[hb])
            ssb, rsb = Buf("fss"), Buf("frs")
            k.op("act", lambda e: e.activation(out=junk[:], in_=ht[:], func=AF.Square, accum_out=ss[:, i:i + 1]),
                 reads=[hb], writes=[jb, ssb])
            _rstd(k, ss[:, i:i + 1], rs[:, i:i + 1], 1, 1.0 / D_MODEL, ssb, rsb)
            ot, ob, osem = os_.next()
            k.op("dve", lambda e: e.scalar_tensor_tensor(out=ot[:], in0=ht[:], scalar=rs[:, i:i + 1], in1=gbc[:],
                                                         op0=ALU.mult, op1=ALU.mult), reads=[hb, rsb, gb], writes=[ob])
            k.dma("sp", C.y[i * 128:(i + 1) * 128, :], ot[:], osem, reads=[ob], writes=[yb])


def alloc_scratch(k, C, dbg):
    kind = "ExternalOutput" if dbg else "Internal"
    C.scr_b = {n: Buf(n) for n in ("h", "xnT", "proj", "qkv", "oT", "mixT", "moe")}
    C.h_d = k.dram("h_d", [S, D_MODEL], F32, kind).ap()
    C.xnT_d = k.dram("xnT_d", [D_MODEL, S], BF16).ap()
    C.A_qT = k.dram("A_qT", [1536, S], BF16, kind).ap()
    C.A_kT = k.dram("A_kT", [1536, S], BF16, kind).ap()
    C.A_v = [k.dram(f"A_v{g}", [S, 512], BF16, kind).ap() for g in range(3)]
    C.B_tm = k.dram("B_tm", [S, 800], F32, kind).ap()
    C.C_tm = k.dram("C_tm", [S, 640], F32, kind).ap()
    C.C_v = k.dram("C_v", [S, 128], BF16, kind).ap()
    C.D_qT = k.dram("D_qT", [512, S], BF16, kind).ap()
    C.D_kT = k.dram("D_kT", [128, S], BF16, kind).ap()
    C.D_v = k.dram("D_v", [S, 128], BF16, kind).ap()
    C.B_qT = k.dram("B_qT", [8, 96, S], BF16, kind).ap()
    C.B_kT = k.dram("B_kT", [8, 96, S], BF16, kind).ap()
    C.B_v = k.dram("B_v", [S, 512], BF16, kind).ap()
    C.C_qT = k.dram("C_qT", [8, 64, S], BF16, kind).ap()
    C.C_kT = k.dram("C_kT", [2, 64, S], BF16, kind).ap()
    C.oT_d = k.dram("oT_d", [2048, S], BF16, kind).ap()
    C.mixT_d = k.dram("mixT_d", [D_MODEL, S], BF16, kind).ap()
    C.selGT_d = k.dram("selGT_d", [16, 128, 32, 128], BF16).ap()
    C.ye_d = k.dram("ye_d", [128, 32, D_MODEL], BF16, kind).ap()
    if dbg:
        C.dbg_probs = k.dram("dbg_probs", [S, 16], F32, kind).ap()
        C.dbg_mask = k.dram("dbg_mask", [S, 16], F32, kind).ap()
        C.dbg_rank = k.dram("dbg_rank", [S, 16], F32, kind).ap()


def build(dbg=None):
    k = K()
    C = Ctx()
    C.x = k.dram("x", [S, D_MODEL], F32, "ExternalInput").ap()
    C.w_in = k.dram("w_in", [DEPTH, D_MODEL, N_IN], F32, "ExternalInput").ap()
    C.g_attn = k.dram("g_attn_norm", [DEPTH, D_MODEL], F32, "ExternalInput").ap()
    C.c_ident = k.dram("c_ident", [128, 128], F32, "ExternalInput").ap()
    C.c_bias = k.dram("c_bias", [32, 128, 384], F32, "ExternalInput").ap()
    C.c_ones = k.dram("c_ones", [128, 128], F32, "ExternalInput").ap()
    C.c_sel64 = k.dram("c_sel64", [128, 64], F32, "ExternalInput").ap()
    C.sink_d = k.dram("sink_d", [DEPTH, 8], F32, "ExternalInput").ap()
    C.g_mla_q = k.dram("g_mla_q", [DEPTH, 512], F32, "ExternalInput").ap()
    C.b_gate = k.dram("b_gate", [DEPTH, 4, D_MODEL], F32, "ExternalInput").ap()
    C.g_ffn = k.dram("g_ffn_norm", [DEPTH, D_MODEL], F32, "ExternalInput").ap()
    C.g_final = k.dram("g_final", [D_MODEL], F32, "ExternalInput").ap()
    C.w_router = k.dram("w_router", [DEPTH, D_MODEL, NE], F32, "ExternalInput").ap()
    C.w_exp_gate = k.dram("w_exp_gate", [DEPTH, NE, D_MODEL, EFF], F32, "ExternalInput").ap()
    C.w_exp_up = k.dram("w_exp_up", [DEPTH, NE, D_MODEL, EFF], F32, "ExternalInput").ap()
    C.w_exp_down = k.dram("w_exp_down", [DEPTH, NE, EFF, D_MODEL], F32, "ExternalInput").ap()
    C.c_iota = k.dram("c_iota", [128, 256], F32, "ExternalInput").ap()
    C.c_ustr = k.dram("c_ustr", [128, 128], F32, "ExternalInput").ap()
    C.w_branch = k.dram("w_branch", [DEPTH, 4, 512, D_MODEL], F32, "ExternalInput").ap()
    C.w_out = k.dram("w_out", [DEPTH, D_MODEL, D_MODEL], F32, "ExternalInput").ap()
    C.g_mla_kv = k.dram("g_mla_kv", [DEPTH, 256], F32, "ExternalInput").ap()
    C.w_mla_uq = k.dram("w_mla_uq", [DEPTH, 512, 768], F32, "ExternalInput").ap()
    C.w_mla_ukv = k.dram("w_mla_ukv", [DEPTH, 256, 1024], F32, "ExternalInput").ap()
    C.g_c_q = k.dram("g_c_q", [DEPTH, 64], F32, "ExternalInput").ap()
    C.g_c_k = k.dram("g_c_k", [DEPTH, 64], F32, "ExternalInput").ap()
    C.c_ropeB = k.dram("c_ropeB", [S, 32], F32, "ExternalInput").ap()
    C.c_ropeC = k.dram("c_ropeC", [S, 64], F32, "ExternalInput").ap()
    C.y = k.dram("y", [S, D_MODEL], F32, "ExternalOutput").ap()
    alloc_scratch(k, C, dbg)
    yb = Buf("y")

    with Scope(k) as top:
        C.ident_bf = top.sbuf("ident_bf", [128, 128], BF16)
        C.ident_bf_b = Buf("ident_bf")
        k.dma("pool", C.ident_bf[:], C.c_ident, k.dsem("misc"), writes=[C.ident_bf_b])
        C.ones_f = top.sbuf("ones_f", [128, 128], F32)
        C.ones_f_b = Buf("ones_f")
        k.dma("sp", C.ones_f[:], C.c_ones, k.dsem("misc"), writes=[C.ones_f_b])
        for l in range(1 if dbg else DEPTH):
            with Scope(k) as lsc:
                xnT = lsc.sbuf("xnT", [128, 16, S], BF16)
                xnTb = [Buf(f"xnT{i}") for i in range(16)]
                with k.nc.named_scope('stage_norm'):
                    stage_norm(k, C, C.x if l == 0 else C.h_d, C.g_attn[l], xnT_sb=xnT, xnT_bufs=xnTb, featT_dst=C.xnT_d)
                with k.nc.named_scope('stage_proj'):
                    stage_proj(k, C, l, xnT, xnTb)
            if not dbg or 1 in dbg:
                with k.nc.named_scope('stage_prepB'):
                    stage_prepB(k, C, l)
            if not dbg or 2 in dbg:
                with k.nc.named_scope('stage_prepC'):
                    stage_prepC(k, C, l)
            units = attn_units(C)
            if dbg:
                units = [u for u in units if u["branch"] in dbg]
            with k.nc.named_scope('stage_attn'):
                stage_attn(k, C, l, units)
            if not dbg or 4 in dbg:
                with k.nc.named_scope('stage_merge'):
                    stage_merge(k, C, l)
                with k.nc.named_scope('stage_out'):
                    stage_out(k, C, l, C.x if l == 0 else C.h_d)
            if dbg and 4 not in dbg:
                k.dma("sp", C.h_d, C.x, k.dsem("misc"), writes=[C.scr_b["h"]])
            if not dbg or 5 in dbg:
                with k.nc.named_scope('stage_moe'):
                    stage_moe(k, C, l, dbg)
        if not dbg or 8 in dbg:
            with k.nc.named_scope('stage_final'):
                stage_final(k, C, yb)
        else:
            k.dma('sp', C.y[0:128, :], C.x[0:128, :], k.dsem('out'), writes=[yb])
        k.barrier()
    k.finish()
    return k


def host_consts():
    c = {"c_ident": np.eye(128, dtype=np.float32), "c_ones": np.ones((128, 128), np.float32)}
    c["c_sel64"] = np.zeros((128, 64), np.float32)
    c["c_sel64"][64, :] = 1.0
    c["c_iota"] = np.tile(np.arange(256, dtype=np.float32)[None, :], (128, 1))
    c["c_ustr"] = (np.arange(128)[:, None] < np.arange(128)[None, :]).astype(np.float32)
    bias = np.zeros((32, 128, 384), np.float32)
    kk = np.arange(128)[:, None].astype(np.float64)
    qq = np.arange(128)[None, :].astype(np.float64)
    dists = [qq - kk + 128, np.abs(qq - kk), kk + 128 - qq]
    for idx in range(32):
        if idx < 24:
            g, h = divmod(idx, 8)
            slope = 2.0 ** (-(h + 1)) * A_DIL[g]
            W = 64
        else:
            h = idx - 24
            slope = 2.0 ** (-(h + 1))
            W = 128
        for t in range(3):
            d = dists[t]
            bias[idx, :, t * 128:(t + 1) * 128] = np.where(d <= W, -slope * d * 8.0, -240000.0)
    c["c_bias"] = bias
    freqs = 10000.0 ** (-np.arange(0, 32, 2, dtype=np.float32) / 32)
    pos = np.arange(S, dtype=np.float32)
    ang = pos[:, None] * freqs[None, :]
    c["c_ropeB"] = np.concatenate([np.cos(ang), np.sin(ang)], axis=1).astype(np.float32)
    rows = np.repeat(np.arange(S // 64), 64).astype(np.float32)
    cols = np.tile(np.arange(64), S // 64).astype(np.float32)
    ar = rows[:, None] * freqs[None, :]
    ac = cols[:, None] * freqs[None, :]
    c["c_ropeC"] = np.concatenate([np.cos(ar), np.sin(ar), np.cos(ac), np.sin(ac)], axis=1).astype(np.float32)
    return c


def kernel(**inputs):
    k = build()
    consts = host_consts()
    in_maps = []
    for b in range(8):
        m = {"x": np.ascontiguousarray(inputs["x"][b])}
        for n in WEIGHT_NAMES:
            m[n] = np.ascontiguousarray(inputs[n])
        m.update(consts)
        in_maps.append(m)
    res = run_bass_kernel_spmd(k.nc, in_maps, core_ids=list(range(8)))
    return np.stack([r["y"] for r in res.results], axis=0)
```

```python
import numpy as np
from contextlib import ExitStack
import concourse.bass as bass
import concourse.mybir as mybir
from concourse.bass_utils import run_bass_kernel_spmd

F32 = mybir.dt.float32
BF16 = mybir.dt.bfloat16
AF = mybir.ActivationFunctionType
ALU = mybir.AluOpType
AX = mybir.AxisListType

D_MODEL = 2048
S = 2048
DEPTH = 2
N_IN = 15136
G0 = 6944
EPS = 1e-6
NE = 16
EFF = 1024
CAP = 256
A_DIL = (1, 4, 16)
WEIGHT_NAMES = ["w_in", "g_attn_norm", "sink_d", "g_mla_q", "g_mla_kv", "w_mla_uq", "w_mla_ukv", "g_c_q", "g_c_k", "b_gate", "w_branch", "w_out", "g_ffn_norm", "g_final", "w_router", "w_exp_gate", "w_exp_up", "w_exp_down"]


class Sem:
    def __init__(self, handle, name):
        self.h = handle
        self.name = name
        self.count = 0
        self.dma = name.startswith("d_")


class Buf:
    __slots__ = ("name", "w", "r")

    def __init__(self, name):
        self.name = name
        self.w = None
        self.r = {}


class Q:
    def __init__(self, name, eng, sem):
        self.name = name
        self.eng = eng
        self.sem = sem
        self.waited = {}


class K:
    def __init__(self):
        self.nc = bass.Bass("TRN2", target_bir_lowering=False)
        self.es = ExitStack()
        nc = self.nc
        self.sems = []
        self.q = {}
        for name, eng in (("pe", nc.tensor), ("act", nc.scalar), ("dve", nc.vector),
                          ("pool", nc.gpsimd), ("sp", nc.sync)):
            self.q[name] = Q(name, eng, self.sem("q_" + name))
        self.ninst = 0
        self.uid = 0
        self.slot_sem_idx = 0
        self.sem_pool = {}

    def sem(self, name):
        s = Sem(self.es.enter_context(self.nc.semaphore(name)), name)
        self.sems.append(s)
        return s

    def dsem(self, name):
        if name not in self.sem_pool:
            self.sem_pool[name] = self.sem("d_" + name)
        return self.sem_pool[name]

    def dram(self, name, shape, dt, kind="Internal"):
        return self.nc.dram_tensor(name, list(shape), dt, kind=kind)

    def _deps(self, q, reads, writes):
        deps = {}

        def add(s, v, raw):
            if s is q.sem:
                if not raw or q.name == "pe" or q.name == "sp":
                    return
            if deps.get(s, 0) < v:
                deps[s] = v

        for b in reads:
            if b.w is not None:
                add(b.w[0], b.w[1], True)
        for b in writes:
            if b.w is not None:
                add(b.w[0], b.w[1], False)
            for s, v in b.r.items():
                add(s, v, False)
        for s, v in deps.items():
            if s.dma:
                v = s.count
            if q.waited.get(s, 0) >= v:
                continue
            assert v <= s.count, f"wait on unsignalled token {s.name} {v}>{s.count} from {q.name}"
            q.eng.wait_ge(s.h, v)
            q.waited[s] = v

    def op(self, E, emit, reads=(), writes=(), signal=True):
        q = self.q[E]
        self._deps(q, reads, writes)
        ins = emit(q.eng)
        self.ninst += 1
        if signal:
            q.sem.count += 1
            ins.then_inc(q.sem.h, 1)
            v = q.sem.count
        else:
            v = q.sem.count + 1
        for b in reads:
            b.r[q.sem] = v
        for b in writes:
            b.w = (q.sem, v)
            b.r = {}
        return ins

    def dma(self, E, out, in_, sem, reads=(), writes=(), **kw):
        q = self.q[E]
        self._deps(q, reads, writes)
        ins = q.eng.dma_start(out=out, in_=in_, **kw)
        self.ninst += 1
        sem.count += 16
        ins.then_inc(sem.h, 16)
        for b in reads:
            b.r[sem] = sem.count
        for b in writes:
            b.w = (sem, sem.count)
            b.r = {}
        return ins

    def barrier(self):
        for q in self.q.values():
            for s in self.sems:
                if s is q.sem or s.count == 0:
                    continue
                if q.waited.get(s, 0) >= s.count:
                    continue
                q.eng.wait_ge(s.h, s.count)
                q.waited[s] = s.count

    def finish(self):
        self.barrier()
        self.nc.all_engine_barrier()
        self.nc.clear_and_free_semaphores([s.h for s in self.sems])
        self.nc.all_engine_barrier()

    def mm(self, out, lhsT, rhs, start, stop, reads, writes, signal):
        return self.op("pe", lambda e: e.matmul(out, lhsT=lhsT, rhs=rhs, start=start, stop=stop,
                                                skip_group_check=True),
                       reads=reads, writes=writes, signal=signal)

    def tr(self, out, in_, ident, reads, writes, signal):
        return self.op("pe", lambda e: e.transpose(out, in_, ident), reads=reads, writes=writes, signal=signal)


class Scope:
    def __init__(self, k):
        self.k = k
        self.es = ExitStack()

    def __enter__(self):
        self.sem_idx0 = self.k.slot_sem_idx
        return self

    def __exit__(self, *a):
        self.k.barrier()
        self.es.close()
        self.k.slot_sem_idx = self.sem_idx0
        return False

    def sbuf(self, name, shape, dt):
        self.k.uid += 1
        return self.es.enter_context(self.k.nc.sbuf_tensor(f"{name}_{self.k.uid}", list(shape), dt))

    def psum(self, name, shape, dt=F32):
        self.k.uid += 1
        return self.es.enter_context(self.k.nc.psum_tensor(f"{name}_{self.k.uid}", list(shape), dt))

    def slots(self, name, n, shape, dt):
        return Slots(self, name, n, shape, dt)


class Slots:
    def __init__(self, sc, name, n, shape, dt, psum=False):
        self.n = n
        if psum:
            self.t = [sc.psum(f"{name}{i}", shape, dt) for i in range(n)]
        else:
            self.t = [sc.sbuf(f"{name}{i}", shape, dt) for i in range(n)]
        self.b = [Buf(f"{name}{i}") for i in range(n)]
        if psum:
            self.s = [None] * n
        else:
            self.s = []
            for i in range(n):
                self.s.append(sc.k.dsem(f"S{sc.k.slot_sem_idx}"))
                sc.k.slot_sem_idx += 1
        self.i = 0

    def next(self):
        i = self.i % self.n
        self.i += 1
        return self.t[i], self.b[i], self.s[i]


def fold_view(ap2d, dil, n0, n):
    Ld = S // dil
    if dil == 1:
        return ap2d[:, n0:n0 + n]
    v = ap2d.rearrange("p (j r) -> p r j", r=dil)
    if n <= Ld:
        r, j0 = divmod(n0, Ld)
        assert j0 + n <= Ld
        return v[:, r, j0:j0 + n]
    assert n % Ld == 0 and n0 % Ld == 0
    return v[:, n0 // Ld:(n0 + n) // Ld, :]


def like_fold(ap2d, dil, n):
    Ld = S // dil
    if dil == 1 or n <= Ld:
        return ap2d
    return ap2d.rearrange("p (a j) -> p a j", j=Ld)


class Ctx:
    pass


def stage_norm(k, C, src, g_row, tok_major_dst=None, featT_dst=None, xnT_sb=None, xnT_bufs=None,
               xn_sb=None, xn_bufs=None, router=None):
    with Scope(k) as sc:
        hs = sc.slots("nh", 3, [128, D_MODEL], F32)
        gbc = sc.sbuf("gbc", [128, D_MODEL], F32)
        gb = Buf("gbc")
        k.dma("sp", gbc[:], g_row.partition_broadcast(128), k.dsem("misc"), writes=[gb])
        junk = sc.sbuf("junk", [128, D_MODEL], BF16)
        jb = Buf("junk")
        ss = sc.sbuf("ss", [128, 16], F32)
        rs = sc.sbuf("rs", [128, 16], F32)
        ssb = [Buf(f"ss{i}") for i in range(16)]
        rsb = [Buf(f"rs{i}") for i in range(16)]
        if xn_sb is None:
            xns = sc.slots("xnt", 2, [128, D_MODEL], BF16)
        pst = Slots(sc, "npt", 2, [128, 1024], BF16, psum=True)
        if router is not None:
            wr_sb, wr_b, lg_sb, lg_b = router
            psr = Slots(sc, "npr", 2, [128, 16], F32, psum=True)
            xts = sc.slots("nxT", 2, [128, 16, 128], BF16)
        ident, identb = C.ident_bf, C.ident_bf_b
        srcv = src.rearrange("(i p) d -> i p d", p=128)
        for i in range(16):
            ht, hb, hsem = hs.next()
            k.dma("sp", ht[:], srcv[i], hsem, writes=[hb])
            k.op("act", lambda e: e.activation(out=junk[:], in_=ht[:], func=AF.Square, accum_out=ss[:, i:i + 1]),
                 reads=[hb], writes=[jb, ssb[i]])
            k.op("dve", lambda e: e.tensor_scalar(out=rs[:, i:i + 1], in0=ss[:, i:i + 1], scalar1=1.0 / D_MODEL,
                                                  scalar2=EPS, op0=ALU.mult, op1=ALU.add),
                 reads=[ssb[i]], writes=[rsb[i]])
            k.op("act", lambda e: e.activation(out=rs[:, i:i + 1], in_=rs[:, i:i + 1], func=AF.Sqrt),
                 reads=[rsb[i]], writes=[rsb[i]])
            k.op("dve", lambda e: e.reciprocal(out=rs[:, i:i + 1], in_=rs[:, i:i + 1]),
                 reads=[rsb[i]], writes=[rsb[i]])
            if xn_sb is None:
                xt, xb, _ = xns.next()
                xt_ap = xt[:]
            else:
                xt_ap = xn_sb[:, i, :]
                xb = xn_bufs[i]
            k.op("dve", lambda e: e.scalar_tensor_tensor(out=xt_ap, in0=ht[:], scalar=rs[:, i:i + 1], in1=gbc[:],
                                                         op0=ALU.mult, op1=ALU.mult),
                 reads=[hb, rsb[i], gb], writes=[xb])
            if xnT_sb is None and router is None:
                continue
            if router is not None:
                xT, xTb, _ = xts.next()
            for half in range(2):
                pt, pb, _ = pst.next()
                for j in range(8):
                    c = half * 8 + j
                    k.tr(pt[:, j * 128:(j + 1) * 128], xt_ap[:, c * 128:(c + 1) * 128], ident[:],
                         reads=[xb, identb], writes=[pb], signal=(j == 7))
                src_ap = pt[:].rearrange("p (c t) -> p c t", t=128)
                if xnT_sb is not None:
                    dst = xnT_sb[:, half * 8:(half + 1) * 8, i * 128:(i + 1) * 128]
                    wb_ = [xnT_bufs[i]]
                else:
                    dst = xT[:, half * 8:(half + 1) * 8, :]
                    wb_ = [xTb]
                if half == 0:
                    k.op("act", lambda e: e.copy(out=dst, in_=src_ap), reads=[pb], writes=wb_)
                else:
                    k.op("dve", lambda e: e.tensor_copy(out=dst, in_=src_ap), reads=[pb], writes=wb_)
            if router is not None:
                pr, prb, _ = psr.next()
                for c in range(16):
                    k.mm(pr[:], lhsT=xT[:, c, :], rhs=wr_sb[:, c, :], start=(c == 0), stop=(c == 15),
                         reads=[xTb, wr_b], writes=[prb], signal=(c == 15))
                k.op("act", lambda e: e.copy(out=lg_sb[:, i, :], in_=pr[:]), reads=[prb], writes=[lg_b])
        if featT_dst is not None:
            k.dma("sp", featT_dst.rearrange("(c p) s -> p c s", p=128), xnT_sb[:], k.dsem("misc"),
                  reads=xnT_bufs, writes=[C.scr_b["xnT"]])
        if xnT_sb is not None or xn_sb is not None:
            pass


def stage_proj(k, C, l, xnT, xnTb):
    w_in = C.w_in[l]
    jobs = []
    for qk in range(2):
        for g in range(3):
            c0 = (qk * 3 + g) * 512
            dst = (C.A_qT if qk == 0 else C.A_kT)[g * 512:(g + 1) * 512, :]
            jobs.append(("fm", c0, 512, A_DIL[g], dst, BF16))
    for g in range(3):
        jobs.append(("tm", (6 + g) * 512, 512, A_DIL[g], C.A_v[g][:, :], BF16))
    for c0, n in ((0, 512), (512, 288)):
        jobs.append(("tm", 4608 + c0, n, 1, C.B_tm[:, c0:c0 + n], F32))
    for c0, n in ((0, 512), (512, 128)):
        jobs.append(("tm", 5408 + c0, n, 1, C.C_tm[:, c0:c0 + n], F32))
    jobs.append(("tm", 6048, 128, 1, C.C_v[:, :], BF16))
    jobs.append(("fm", 6176, 512, 1, C.D_qT[:, :], BF16))
    jobs.append(("fm", 6688, 128, 1, C.D_kT[:, :], BF16))
    jobs.append(("tm", 6816, 128, 1, C.D_v[:, :], BF16))

    with Scope(k) as sc:
        ws = sc.slots("pw", 3, [128, 16, 512], BF16)
        stf = sc.slots("pof", 2, [128, S], BF16)
        stt32 = sc.slots("pot32", 3, [128, 512], F32)
        stt16 = sc.slots("pot16", 3, [128, 512], BF16)
        ps = Slots(sc, "pps", 4, [128, 512], F32, psum=True)
        wv = w_in.rearrange("(c p) n -> p c n", p=128)
        loaded = {}

        def load(j):
            if j >= len(jobs) or j in loaded:
                return
            _, c0, n, _, _, _ = jobs[j]
            wt, wb, wsem = ws.next()
            k.dma("pool", wt[:, :, 0:n], wv[:, :, c0:c0 + n], wsem, writes=[wb])
            loaded[j] = (wt, wb)

        load(0)
        load(1)
        ev = 0
        for j, (mode, c0, n, dil, dst, dt) in enumerate(jobs):
            load(j + 2)
            wt, wb = loaded[j]
            if mode == "fm":
                for sub in range(n // 128):
                    ot, ob, osem = stf.next()
                    for tg in range(4):
                        p, pb, _ = ps.next()
                        for c in range(16):
                            k.mm(p[:], lhsT=wt[:, c, sub * 128:(sub + 1) * 128], rhs=xnT[:, c, tg * 512:(tg + 1) * 512],
                                 start=(c == 0), stop=(c == 15), reads=[wb] + xnTb, writes=[pb], signal=(c == 15))
                        if dil == 1:
                            o_ap = ot[:, tg * 512:(tg + 1) * 512]
                            i_ap = p[:]
                        else:
                            nj = 512 // dil
                            o_ap = ot[:].rearrange("p (r j) -> p j r", r=dil)[:, tg * nj:(tg + 1) * nj, :]
                            i_ap = p[:].rearrange("p (j r) -> p j r", r=dil)
                        if ev % 2 == 0:
                            k.op("act", lambda e: e.copy(out=o_ap, in_=i_ap), reads=[pb], writes=[ob])
                        else:
                            k.op("dve", lambda e: e.tensor_copy(out=o_ap, in_=i_ap), reads=[pb], writes=[ob])
                        ev += 1
                    k.dma("sp", dst[sub * 128:(sub + 1) * 128, :], ot[:], osem, reads=[ob], writes=[C.scr_b["proj"]])
            else:
                for i in range(16):
                    ot, ob, osem = (stt32 if dt == F32 else stt16).next()
                    p, pb, _ = ps.next()
                    for c in range(16):
                        k.mm(p[:, 0:n], lhsT=xnT[:, c, i * 128:(i + 1) * 128], rhs=wt[:, c, 0:n], start=(c == 0), stop=(c == 15),
                             reads=[wb] + xnTb, writes=[pb], signal=(c == 15))
                    o_ap = ot[:, 0:n]
                    if ev % 2 == 0:
                        k.op("act", lambda e: e.copy(out=o_ap, in_=p[:, 0:n]), reads=[pb], writes=[ob])
                    else:
                        k.op("dve", lambda e: e.tensor_copy(out=o_ap, in_=p[:, 0:n]), reads=[pb], writes=[ob])
                    ev += 1
                    k.dma("sp", dst[i * 128:(i + 1) * 128, :], ot[:, 0:n], osem, reads=[ob], writes=[C.scr_b["proj"]])


def attn_units(C):
    units = []
    for h in range(8):
        for g in range(3):
            r0 = g * 512 + h * 64
            units.append(dict(q=C.A_qT[r0:r0 + 64, :], k=C.A_kT[r0:r0 + 64, :], v=("A%d" % g, h),
                              dk=64, kind="band", dil=A_DIL[g], bidx=g * 8 + h, scale=0.125, first=(g == 0),
                              last=(g == 2), branch=0, h=h, sink=False))
    for h in range(8):
        units.append(dict(q=C.B_qT[h], k=C.B_kT[h], v=("B", h), dk=96, kind="dense",
                          scale=96 ** -0.5, first=True, last=True, branch=1, h=h, sink=False))
    for h in range(8):
        kv = h // 4
        units.append(dict(q=C.C_qT[h], k=C.C_kT[kv], v=("C", kv), dk=64, kind="dense",
                          scale=0.125, first=True, last=True, branch=2, h=h, sink=False))
    for h in range(8):
        kv = h // 4
        units.append(dict(q=C.D_qT[h * 64:(h + 1) * 64, :], k=C.D_kT[kv * 64:(kv + 1) * 64, :],
                          v=("D", kv), dk=64, kind="band", dil=1, bidx=24 + h, scale=0.125,
                          first=True, last=True, branch=3, h=h, sink=True))
    return units


def stage_attn(k, C, l, units):
    LAG = 2
    with Scope(k) as sc:
        bias_sb = sc.sbuf("bias_sb", [128, 32, 384], BF16)
        biasb = Buf("bias")
        k.dma("pool", bias_sb[:], C.c_bias.rearrange("t p n -> p t n"), k.dsem("misc"), writes=[biasb])
        sinkexp = sc.sbuf("sinkexp", [65, 8], F32)
        sinkb = Buf("sink")
        k.dma("sp", sinkexp[64:65, :], C.sink_d[l:l + 1, :], k.dsem("misc"), writes=[sinkb])
        k.op("act", lambda e: e.activation(out=sinkexp[64:65, :], in_=sinkexp[64:65, :], func=AF.Exp),
             reads=[sinkb], writes=[sinkb])
        qk_slots = {}
        for dk_ in sorted({u["dk"] for u in units}):
            qs_ = sc.slots(f"aq{dk_}", 2, [128, S], BF16)
            ks_ = sc.slots(f"ak{dk_}", 2, [128, S], BF16)
            for sl_ in (qs_, ks_):
                for i in range(2):
                    k.op("pool", lambda e: e.memset(sl_.t[i][dk_:128, :], 0.0), writes=[sl_.b[i]])
            qk_slots[dk_] = (qs_, ks_)
        vsrc = {"A0": (C.A_v[0], 8, "proj"), "A1": (C.A_v[1], 8, "proj"), "A2": (C.A_v[2], 8, "proj"),
                "B": (C.B_v, 8, "qkv"), "C": (C.C_v, 2, "proj"), "D": (C.D_v, 2, "proj")}
        vall = {}
        vstg = sc.slots("avst", 2, [128, 16, 512], BF16)
        for name in sorted({u["v"][0] for u in units}):
            src, nh, sb = vsrc[name]
            vt_ = sc.sbuf("av" + name, [128, 16, nh, 65], BF16)
            vb_ = Buf("av" + name)
            k.op("pool", lambda e: e.memset(vt_[:], 1.0), writes=[vb_])
            stg, stgb, stgsem = vstg.next()
            dil_ = {"A1": 4, "A2": 16}.get(name, 1)
            if dil_ == 1:
                k.dma("sp", stg[:, :, 0:nh * 64], src.rearrange("(i p) n -> p i n", p=128), stgsem,
                      reads=[C.scr_b[sb]], writes=[stgb])
            else:
                nti = (S // dil_) // 128
                srcv = src.rearrange("(ii p r) n -> r p ii n", p=128, r=dil_)
                for r in range(dil_):
                    k.dma("sp", stg[:, r * nti:(r + 1) * nti, 0:nh * 64], srcv[r], stgsem,
                          reads=[C.scr_b[sb]], writes=[stgb])
            k.op("pool", lambda e: e.tensor_copy(out=vt_[:, :, :, 0:64],
                                                 in_=stg[:, :, 0:nh * 64].rearrange("p i (h d) -> p i h d", d=64)),
                 reads=[stgb], writes=[vb_])
            vall[name] = (vt_, vb_)
        pts = sc.slots("apt", 4, [128, 512], BF16)
        accs = sc.slots("aacc", 2, [65, S], F32)
        rden = sc.sbuf("rden", [128, S], F32)
        rdenb = Buf("rden")
        k.op("pool", lambda e: e.memset(rden[:], 0.0), writes=[rdenb])
        sel64 = sc.sbuf("sel64", [128, 64], F32)
        sel64b = Buf("sel64")
        k.dma("sp", sel64[:], C.c_sel64, k.dsem("misc"), writes=[sel64b])
        ots = sc.slots("aot", 2, [64, S], BF16)
        ps_s = Slots(sc, "aps", 3, [128, 512], F32, psum=True)
        ps_a = Slots(sc, "apa", 2, [128, 512], F32, psum=True)
        ps_b = Slots(sc, "apb", 2, [64, 512], F32, psum=True)
        ident, identb = C.ident_bf, C.ident_bf_b
        loaded = {}

        def load(ui):
            if ui >= len(units) or ui in loaded:
                return
            u = units[ui]
            qs, ks = qk_slots[u["dk"]]
            qt, qb, qsem = qs.next()
            kt, kb, ksem = ks.next()
            vt_, vb = vall[u["v"][0]]
            vt = vt_[:, :, u["v"][1], :]
            dk = u["dk"]
            k.dma("sp", qt[0:dk, :], u["q"], qsem, reads=[C.scr_b["proj"], C.scr_b["qkv"]], writes=[qb])
            k.dma("sp", kt[0:dk, :], u["k"], ksem, reads=[C.scr_b["proj"], C.scr_b["qkv"]], writes=[kb])
            loaded[ui] = (qt, qb, kt, kb, vt, vb)

        load(0)
        pending = []
        for ui, u in enumerate(units):
            load(ui + 1)
            qt, qb, kt, kb, vt, vb = loaded.pop(ui)
            dk = u["dk"]
            scale = u["scale"]
            if u["first"]:
                acc_sb, acc_b, _ = accs.next()
            first_grp = u["first"]
            steps = []
            if u["kind"] == "dense":
                for qg in range(4):
                    for kc in range(16):
                        steps.append(("d", qg, kc))
            else:
                dil = u["dil"]
                Ld = S // dil
                nb = Ld // 128
                for z in range(dil):
                    for c in range(nb):
                        steps.append(("b", z, c))
            state = {}
            accst = dict(cb=0, n0=0, acc=None, accb=None)

            def emit_scores(i):
                st = steps[i]
                s, sb, _ = ps_s.next()
                pt, ptb, _ = pts.next()
                if st[0] == "d":
                    _, qg, kc = st
                    k.mm(s[:, :], lhsT=kt[:, kc * 128:(kc + 1) * 128], rhs=qt[:, qg * 512:(qg + 1) * 512],
                         start=True, stop=True, reads=[kb, qb], writes=[sb], signal=True)
                    k.op("act", lambda e: e.activation(out=pt[:, :], in_=s[:, :], func=AF.Exp, scale=scale),
                         reads=[sb], writes=[ptb])
                    state[i] = (pt, ptb, None)
                else:
                    _, z, c = st
                    chunks = [t for t in range(3) if 0 <= c - 1 + t < nb]
                    t0, t1 = chunks[0], chunks[-1] + 1
                    k.mm(s[:, t0 * 128:t1 * 128], lhsT=ident[:], rhs=bias_sb[:, u["bidx"], t0 * 128:t1 * 128],
                         start=True, stop=False, reads=[identb, biasb], writes=[sb], signal=False)
                    for t in chunks:
                        kblk = c - 1 + t
                        k.mm(s[:, t * 128:(t + 1) * 128],
                             lhsT=kt[:, z * Ld + kblk * 128: z * Ld + kblk * 128 + 128],
                             rhs=qt[:, z * Ld + c * 128: z * Ld + c * 128 + 128],
                             start=False, stop=(t == chunks[-1]), reads=[kb, qb], writes=[sb],
                             signal=(t == chunks[-1]))
                    k.op("act", lambda e: e.activation(out=pt[:, t0 * 128:t1 * 128], in_=s[:, t0 * 128:t1 * 128],
                                                       func=AF.Exp, scale=scale),
                         reads=[sb], writes=[ptb])
                    state[i] = (pt, ptb, chunks)

            def flush(n0, n, dil_):
                acc, accb = accst["acc"], accst["accb"]
                dst = fold_view(acc_sb[0:65, :], dil_, n0, n)
                src = like_fold(acc[0:65, 0:n], dil_, n)
                if first_grp and u["kind"] == "dense":
                    k.op("dve", lambda e: e.tensor_copy(out=dst, in_=src), reads=[accb], writes=[acc_b])
                elif first_grp:
                    k.op("act", lambda e: e.copy(out=dst, in_=src), reads=[accb], writes=[acc_b])
                else:
                    k.op("dve", lambda e: e.tensor_tensor(out=dst, in0=src, in1=dst, op=ALU.add),
                         reads=[accb, acc_b], writes=[acc_b])

            def emit_pv(i):
                st = steps[i]
                pt, ptb, chunks = state.pop(i)
                if st[0] == "d":
                    _, qg, kc = st
                    if kc == 0:
                        accst["acc"], accst["accb"], _ = ps_a.next()
                    acc, accb = accst["acc"], accst["accb"]
                    k.mm(acc[0:65, :], lhsT=vt[:, kc, :], rhs=pt[:, :], start=(kc == 0), stop=(kc == 15),
                         reads=[vb, ptb], writes=[accb], signal=(kc == 15))
                    if kc == 15:
                        flush(qg * 512, 512, 1)
                else:
                    _, z, c = st
                    if accst["cb"] == 0:
                        accst["acc"], accst["accb"], _ = ps_a.next()
                        accst["n0"] = z * Ld + c * 128
                    acc, accb = accst["acc"], accst["accb"]
                    cb = accst["cb"]
                    for t in chunks:
                        kblk = c - 1 + t
                        k.mm(acc[0:65, cb * 128:(cb + 1) * 128], lhsT=vt[:, (z * Ld + kblk * 128) // 128, :],
                             rhs=pt[:, t * 128:(t + 1) * 128], start=(t == chunks[0]), stop=(t == chunks[-1]),
                             reads=[vb, ptb], writes=[accb], signal=(t == chunks[-1]))
                    accst["cb"] += 1
                    if accst["cb"] == 4 or i == len(steps) - 1:
                        flush(accst["n0"], accst["cb"] * 128, dil)
                        accst["cb"] = 0

            DEFER = 6
            for i in range(len(steps) + LAG):
                if i < len(steps):
                    emit_scores(i)
                if i - LAG >= 0:
                    emit_pv(i - LAG)
                if i == DEFER and pending:
                    pending.pop()()

            if u["last"]:
                if pending:
                    pending.pop()()
                h = u["h"]
                if u["sink"]:
                    k.op("dve", lambda e: e.tensor_scalar(out=acc_sb[64:65, :], in0=acc_sb[64:65, :],
                                                          scalar1=sinkexp[64:65, h:h + 1], scalar2=None, op0=ALU.add),
                         reads=[acc_b, sinkb], writes=[acc_b])
                if u["kind"] == "band":
                    k.op("act", lambda e: e.activation(out=rden[64:65, :], in_=acc_sb[64:65, :], func=AF.Ln),
                         reads=[acc_b], writes=[rdenb])
                    k.op("act", lambda e: e.activation(out=rden[64:65, :], in_=rden[64:65, :], func=AF.Exp, scale=-1.0),
                         reads=[rdenb], writes=[rdenb])
                else:
                    k.op("dve", lambda e: e.reciprocal(out=rden[64:65, :], in_=acc_sb[64:65, :]),
                         reads=[acc_b], writes=[rdenb])

                def part2(acc_sb=acc_sb, acc_b=acc_b, h=h, branch=u["branch"]):
                    ot, ob, osem = ots.next()
                    for qg in range(4):
                        bc, bcb, _ = ps_b.next()
                        k.mm(bc[0:64, :], lhsT=sel64[:, :], rhs=rden[:, qg * 512:(qg + 1) * 512],
                             start=True, stop=True, reads=[rdenb, sel64b], writes=[bcb], signal=True)
                        k.op("dve", lambda e: e.tensor_tensor(out=ot[0:64, qg * 512:(qg + 1) * 512],
                                                              in0=acc_sb[0:64, qg * 512:(qg + 1) * 512], in1=bc[0:64, :],
                                                              op=ALU.mult),
                             reads=[acc_b, bcb], writes=[ob])
                    r0 = branch * 512 + h * 64
                    k.dma("sp", C.oT_d[r0:r0 + 64, :], ot[0:64, :], osem, reads=[ob], writes=[C.scr_b["oT"]])

                pending.append(part2)
        while pending:
            pending.pop()()


def _rstd(k, ss_ap, rs_ap, n, inv, ssb, rsb):
    k.op("dve", lambda e: e.tensor_scalar(out=rs_ap, in0=ss_ap, scalar1=inv, scalar2=EPS, op0=ALU.mult, op1=ALU.add),
         reads=[ssb], writes=[rsb])
    k.op("act", lambda e: e.activation(out=rs_ap, in_=rs_ap, func=AF.Sqrt), reads=[rsb], writes=[rsb])
    k.op("dve", lambda e: e.reciprocal(out=rs_ap, in_=rs_ap), reads=[rsb], writes=[rsb])


def _rope(k, x1, x2, cos, sin, o1, o2, shape, tmp, rb, wb, tb):
    t1, t2 = tmp
    k.op("dve", lambda e: e.tensor_tensor(out=t1, in0=x1, in1=cos, op=ALU.mult), reads=rb, writes=[tb])
    k.op("dve", lambda e: e.tensor_tensor(out=t2, in0=x2, in1=sin, op=ALU.mult), reads=rb, writes=[tb])
    k.op("dve", lambda e: e.tensor_tensor(out=o1, in0=t1, in1=t2, op=ALU.subtract), reads=[tb], writes=wb)
    k.op("dve", lambda e: e.tensor_tensor(out=t1, in0=x1, in1=sin, op=ALU.mult), reads=rb + [tb], writes=[tb])
    k.op("dve", lambda e: e.tensor_tensor(out=t2, in0=x2, in1=cos, op=ALU.mult), reads=rb, writes=[tb])
    k.op("dve", lambda e: e.tensor_tensor(out=o2, in0=t1, in1=t2, op=ALU.add), reads=[tb], writes=wb)


def stage_prepB(k, C, l):
    with Scope(k) as sc:
        ms = k.dsem("misc")
        rope = sc.sbuf("ropeB", [128, 16, 32], F32)
        ropeb = Buf("ropeB")
        k.dma("sp", rope[:], C.c_ropeB.rearrange("(i p) n -> p i n", p=128), ms, writes=[ropeb])
        gq = sc.sbuf("gq", [128, 512], F32)
        gkv = sc.sbuf("gkv", [128, 256], F32)
        gb = Buf("gB")
        k.dma("sp", gq[:], C.g_mla_q[l].partition_broadcast(128), ms, writes=[gb])
        k.dma("sp", gkv[:], C.g_mla_kv[l].partition_broadcast(128), ms, writes=[gb])
        wuq = sc.sbuf("wuq", [128, 4, 768], BF16)
        wukv = sc.sbuf("wukv", [128, 2, 1024], BF16)
        wb = Buf("wB")
        k.dma("pool", wuq[:], C.w_mla_uq[l].rearrange("(c p) n -> p c n", p=128), ms, writes=[wb])
        k.dma("pool", wukv[:], C.w_mla_ukv[l].rearrange("(c p) n -> p c n", p=128), ms, writes=[wb])
        bts = sc.slots("bt", 2, [128, 800], F32)
        junk = sc.sbuf("bjunk", [128, 512], BF16)
        jb = Buf("bjunk")
        ss = sc.sbuf("bss", [128, 32], F32)
        rs = sc.sbuf("brs", [128, 32], F32)
        cn = sc.slots("bcn", 2, [128, 768], BF16)
        cT = sc.slots("bcT", 2, [128, 6, 128], BF16)
        qtm = sc.slots("bqtm", 2, [128, 8, 96], BF16)
        ktm = sc.slots("bktm", 2, [128, 8, 96], BF16)
        tmp1 = sc.sbuf("btmp1", [128, 8, 16], F32)
        tmp2 = sc.sbuf("btmp2", [128, 8, 16], F32)
        tb = Buf("btmp")
        kr = sc.sbuf("bkr", [128, 32], F32)
        krb = Buf("bkr")
        vtm = sc.sbuf("bvtm", [128, 16, 512], BF16)
        vtmb = Buf("bvtm")
        qTs = sc.sbuf("bqTs", [96, 8, S], BF16)
        kTs = sc.sbuf("bkTs", [96, 8, S], BF16)
        qTb = Buf("bqTs")
        kTb = Buf("bkTs")
        pt_in = Slots(sc, "bpi", 1, [128, 1024], BF16, psum=True)
        psq = Slots(sc, "bpq", 1, [128, 512], F32, psum=True)
        psqr = Slots(sc, "bpqr", 1, [128, 256], F32, psum=True)
        pskv = Slots(sc, "bpkv", 1, [128, 512], F32, psum=True)
        psv = Slots(sc, "bpv", 1, [128, 512], F32, psum=True)
        pt_q = Slots(sc, "bptq", 1, [96, 1024], BF16, psum=True)
        pt_k = Slots(sc, "bptk", 1, [96, 1024], BF16, psum=True)
        ident, identb = C.ident_bf, C.ident_bf_b
        for i in range(16):
            bt, bb, bsem = bts.next()
            k.dma("sp", bt[:], C.B_tm[i * 128:(i + 1) * 128, :], bsem, reads=[C.scr_b["proj"]], writes=[bb])
            ssb, rsb = Buf("bss"), Buf("brs")
            k.op("act", lambda e: e.activation(out=junk[:, 0:512], in_=bt[:, 0:512], func=AF.Square,
                                               accum_out=ss[:, 2 * i:2 * i + 1]), reads=[bb], writes=[jb, ssb])
            k.op("act", lambda e: e.activation(out=junk[:, 0:256], in_=bt[:, 512:768], func=AF.Square,
                                               accum_out=ss[:, 2 * i + 1:2 * i + 2]), reads=[bb], writes=[jb, ssb])
            k.op("dve", lambda e: e.tensor_scalar(out=ss[:, 2 * i:2 * i + 1], in0=ss[:, 2 * i:2 * i + 1], scalar1=0.5,
                                                  scalar2=None, op0=ALU.mult), reads=[ssb], writes=[ssb])
            _rstd(k, ss[:, 2 * i:2 * i + 2], rs[:, 2 * i:2 * i + 2], 2, 1.0 / 256, ssb, rsb)
            cnt, cnb, _ = cn.next()
            k.op("dve", lambda e: e.scalar_tensor_tensor(out=cnt[:, 0:512], in0=bt[:, 0:512], scalar=rs[:, 2 * i:2 * i + 1],
                                                         in1=gq[:], op0=ALU.mult, op1=ALU.mult),
                 reads=[bb, rsb, gb], writes=[cnb])
            k.op("dve", lambda e: e.scalar_tensor_tensor(out=cnt[:, 512:768], in0=bt[:, 512:768],
                                                         scalar=rs[:, 2 * i + 1:2 * i + 2], in1=gkv[:], op0=ALU.mult,
                                                         op1=ALU.mult),
                 reads=[bb, rsb, gb], writes=[cnb])
            pin, pinb, _ = pt_in.next()
            for c in range(6):
                k.tr(pin[:, c * 128:(c + 1) * 128], cnt[:, c * 128:(c + 1) * 128], ident[:], reads=[cnb, identb],
                     writes=[pinb], signal=(c == 5))
            ct, ctb, _ = cT.next()
            k.op("act", lambda e: e.copy(out=ct[:], in_=pin[:, 0:768].rearrange("p (c t) -> p c t", t=128)),
                 reads=[pinb], writes=[ctb])
            pq, pqb, _ = psq.next()
            pqr, pqrb, _ = psqr.next()
            wuqv = wuq[:].rearrange("p c (h d) -> p c h d", d=96)
            for kc in range(4):
                k.mm(pq[:, :].rearrange("p (h d) -> p h d", d=64), lhsT=ct[:, kc, :], rhs=wuqv[:, kc, :, 0:64],
                     start=(kc == 0), stop=(kc == 3), reads=[ctb, wb], writes=[pqb], signal=(kc == 3))
            for kc in range(4):
                k.mm(pqr[:, :].rearrange("p (h d) -> p h d", d=32), lhsT=ct[:, kc, :], rhs=wuqv[:, kc, :, 64:96],
                     start=(kc == 0), stop=(kc == 3), reads=[ctb, wb], writes=[pqrb], signal=(kc == 3))
            pkv, pkvb, _ = pskv.next()
            pv, pvb, _ = psv.next()
            wukvv = wukv[:].rearrange("p c (h d) -> p c h d", d=128)
            for kc in range(2):
                k.mm(pkv[:, :].rearrange("p (h d) -> p h d", d=64), lhsT=ct[:, 4 + kc, :], rhs=wukvv[:, kc, :, 0:64],
                     start=(kc == 0), stop=(kc == 1), reads=[ctb, wb], writes=[pkvb], signal=(kc == 1))
            for kc in range(2):
                k.mm(pv[:, :].rearrange("p (h d) -> p h d", d=64), lhsT=ct[:, 4 + kc, :], rhs=wukvv[:, kc, :, 64:128],
                     start=(kc == 0), stop=(kc == 1), reads=[ctb, wb], writes=[pvb], signal=(kc == 1))
            qt, qtb, _ = qtm.next()
            kt, ktb, _ = ktm.next()
            pqv = pq[:, :].rearrange("p (h d) -> p h d", d=64)
            pqrv = pqr[:, :].rearrange("p (h d) -> p h d", d=32)
            pkvv = pkv[:, :].rearrange("p (h d) -> p h d", d=64)
            pvv = pv[:, :].rearrange("p (h d) -> p h d", d=64)
            k.op("act", lambda e: e.copy(out=qt[:, :, 0:64], in_=pqv[:, :, 0:64]), reads=[pqb], writes=[qtb])
            cosb = rope[:, i, 0:16].unsqueeze(1).to_broadcast([128, 8, 16])
            sinb = rope[:, i, 16:32].unsqueeze(1).to_broadcast([128, 8, 16])
            _rope(k, pqrv[:, :, 0:16], pqrv[:, :, 16:32], cosb, sinb, qt[:, :, 64:80], qt[:, :, 80:96], None,
                  (tmp1[:], tmp2[:]), [pqrb, ropeb], [qtb], tb)
            k.op("act", lambda e: e.copy(out=kt[:, :, 0:64], in_=pkvv[:, :, 0:64]), reads=[pkvb], writes=[ktb])
            k.op("act", lambda e: e.copy(out=vtm[:, i, :], in_=pv[:, :]), reads=[pvb], writes=[vtmb])
            _rope(k, bt[:, 768:784], bt[:, 784:800], rope[:, i, 0:16], rope[:, i, 16:32], kr[:, 0:16], kr[:, 16:32], None,
                  (tmp1[:, 0, :], tmp2[:, 0, :]), [bb, ropeb], [krb], tb)
            k.op("dve", lambda e: e.tensor_copy(out=kt[:, :, 64:96], in_=kr[:].unsqueeze(1).to_broadcast([128, 8, 32])),
                 reads=[krb], writes=[ktb])
            for (src, srcb, pts_, dst, dstb, eng) in ((qt, qtb, pt_q, qTs, qTb, "act"), (kt, ktb, pt_k, kTs, kTb, "dve")):
                po, pob, _ = pts_.next()
                for h in range(8):
                    k.tr(po[0:96, h * 128:(h + 1) * 128], src[:, h, :], ident[:], reads=[srcb, identb], writes=[pob],
                         signal=(h == 7))
                o_ap = dst[0:96, :, i * 128:(i + 1) * 128]
                i_ap = po[0:96, :].rearrange("p (h t) -> p h t", t=128)
                if eng == "act":
                    k.op("act", lambda e: e.copy(out=o_ap, in_=i_ap), reads=[pob], writes=[dstb])
                else:
                    k.op("dve", lambda e: e.tensor_copy(out=o_ap, in_=i_ap), reads=[pob], writes=[dstb])
        st = k.dsem("bstore")
        k.dma("sp", C.B_qT.rearrange("h p s -> p h s"), qTs[:], st, reads=[qTb], writes=[C.scr_b["qkv"]])
        k.dma("sp", C.B_kT.rearrange("h p s -> p h s"), kTs[:], st, reads=[kTb], writes=[C.scr_b["qkv"]])
        k.dma("sp", C.B_v.rearrange("(i p) n -> p i n", p=128), vtm[:], st, reads=[vtmb], writes=[C.scr_b["qkv"]])


def stage_prepC(k, C, l):
    with Scope(k) as sc:
        ms = k.dsem("misc")
        rope = sc.sbuf("ropeC", [128, 16, 64], F32)
        ropeb = Buf("ropeC")
        k.dma("sp", rope[:], C.c_ropeC.rearrange("(i p) n -> p i n", p=128), ms, writes=[ropeb])
        gc = sc.sbuf("gc", [128, 10, 64], F32)
        gb = Buf("gC")
        k.dma("sp", gc[:, 0:8, :], C.g_c_q[l:l + 1, :].partition_broadcast(128).to_broadcast([128, 8, 64]), ms, writes=[gb])
        k.dma("sp", gc[:, 8:10, :], C.g_c_k[l:l + 1, :].partition_broadcast(128).to_broadcast([128, 2, 64]), ms, writes=[gb])
        cts = sc.slots("ct", 2, [128, 640], F32)
        sq = sc.sbuf("csq", [128, 640], F32)
        sqb = Buf("csq")
        xn = sc.sbuf("cxn", [128, 640], F32)
        xnb = Buf("cxn")
        ss = sc.sbuf("css", [128, 160], F32)
        rs = sc.sbuf("crs", [128, 160], F32)
        tmp1 = sc.sbuf("ctmp1", [128, 10, 2, 16], F32)
        tmp2 = sc.sbuf("ctmp2", [128, 10, 2, 16], F32)
        tb = Buf("ctmp")
        qtm = sc.slots("cqtm", 2, [128, 10, 64], BF16)
        qTs = sc.sbuf("cqTs", [64, 10, S], BF16)
        qTb = Buf("cqTs")
        pt_q = Slots(sc, "cptq", 2, [64, 1024], BF16, psum=True)
        pt_k = Slots(sc, "cptk", 2, [64, 256], BF16, psum=True)
        ident, identb = C.ident_bf, C.ident_bf_b
        for i in range(16):
            ct, cb, csem = cts.next()
            k.dma("sp", ct[:], C.C_tm[i * 128:(i + 1) * 128, :], csem, reads=[C.scr_b["proj"]], writes=[cb])
            ssb, rsb = Buf("css"), Buf("crs")
            k.op("dve", lambda e: e.tensor_tensor(out=sq[:], in0=ct[:], in1=ct[:], op=ALU.mult), reads=[cb], writes=[sqb])
            k.op("dve", lambda e: e.tensor_reduce(out=ss[:, 10 * i:10 * i + 10], in_=sq[:].rearrange("p (h d) -> p h d", d=64),
                                                  axis=AX.X, op=ALU.add), reads=[sqb], writes=[ssb])
            _rstd(k, ss[:, 10 * i:10 * i + 10], rs[:, 10 * i:10 * i + 10], 10, 1.0 / 64, ssb, rsb)
            ctv = ct[:].rearrange("p (h d) -> p h d", d=64)
            xnv = xn[:].rearrange("p (h d) -> p h d", d=64)
            k.op("dve", lambda e: e.tensor_tensor(out=xnv, in0=ctv,
                                                  in1=rs[:, 10 * i:10 * i + 10].unsqueeze(2).to_broadcast([128, 10, 64]),
                                                  op=ALU.mult), reads=[cb, rsb], writes=[xnb])
            k.op("dve", lambda e: e.tensor_tensor(out=xnv, in0=xnv, in1=gc[:], op=ALU.mult), reads=[xnb, gb], writes=[xnb])
            qt, qtb, _ = qtm.next()
            x5 = xn[:].rearrange("p (h a t f) -> p h a t f", a=2, t=2, f=16)
            o5 = qt[:].rearrange("p h (a t f) -> p h a t f", a=2, t=2, f=16)
            r4 = rope[:, i, :].rearrange("p (a t f) -> p a t f", a=2, t=2, f=16)
            cosb = r4[:, :, 0, :].unsqueeze(1).to_broadcast([128, 10, 2, 16])
            sinb = r4[:, :, 1, :].unsqueeze(1).to_broadcast([128, 10, 2, 16])
            _rope(k, x5[:, :, :, 0, :], x5[:, :, :, 1, :], cosb, sinb, o5[:, :, :, 0, :], o5[:, :, :, 1, :], None,
                  (tmp1[:], tmp2[:]), [xnb, ropeb], [qtb], tb)
            po, pob, _ = pt_q.next()
            for h in range(8):
                k.tr(po[0:64, h * 128:(h + 1) * 128], qt[:, h, :], ident[:], reads=[qtb, identb], writes=[pob],
                     signal=(h == 7))
            k.op("act", lambda e: e.copy(out=qTs[0:64, 0:8, i * 128:(i + 1) * 128],
                                         in_=po[0:64, :].rearrange("p (h t) -> p h t", t=128)), reads=[pob], writes=[qTb])
            pk, pkb, _ = pt_k.next()
            for h in range(2):
                k.tr(pk[0:64, h * 128:(h + 1) * 128], qt[:, 8 + h, :], ident[:], reads=[qtb, identb], writes=[pkb],
                     signal=(h == 1))
            k.op("act", lambda e: e.copy(out=qTs[0:64, 8:10, i * 128:(i + 1) * 128],
                                         in_=pk[0:64, :].rearrange("p (h t) -> p h t", t=128)), reads=[pkb], writes=[qTb])
        st = k.dsem("bstore")
        k.dma("sp", C.C_qT.rearrange("h p s -> p h s"), qTs[:, 0:8, :], st, reads=[qTb], writes=[C.scr_b["qkv"]])
        k.dma("sp", C.C_kT.rearrange("h p s -> p h s"), qTs[:, 8:10, :], st, reads=[qTb], writes=[C.scr_b["qkv"]])


def stage_merge(k, C, l):
    with Scope(k) as sc:
        ms = k.dsem("misc")
        xnT = sc.sbuf("mxnT", [128, 16, S], BF16)
        xb = Buf("mxnT")
        k.dma("sp", xnT[:], C.xnT_d.rearrange("(c p) s -> p c s", p=128), k.dsem("mld0"), reads=[C.scr_b["xnT"]], writes=[xb])
        oT = sc.sbuf("moT", [128, 16, S], BF16)
        ob = Buf("moT")
        k.dma("sp", oT[:], C.oT_d.rearrange("(c p) s -> p c s", p=128), k.dsem("mld1"), reads=[C.scr_b["oT"]], writes=[ob])
        bg = sc.sbuf("mbg", [128, 4, 16], F32)
        bgb = Buf("mbg")
        k.dma("sp", bg[:], C.b_gate[l].rearrange("i (c p) -> p i c", p=128), ms, writes=[bgb], allow_slow_non_contiguous=True)
        wgs = sc.slots("mwg", 2, [128, 16, 4, 128], BF16)
        wbs = sc.slots("mwb", 2, [128, 4, 4, 128], BF16)
        sgs = sc.slots("msg", 4, [128, 512], F32)
        tmp = sc.slots("mtmp", 2, [128, 512], F32)
        macc = sc.slots("macc", 2, [128, 512], F32)
        sts = sc.slots("mst", 2, [128, S], BF16)
        pg = Slots(sc, "mpg", 3, [128, 512], F32, psum=True)
        pb_ = Slots(sc, "mpb", 3, [128, 512], F32, psum=True)
        wgv = C.w_in[l][:, G0:].rearrange("(c p) (i n) -> p c i n", p=128, i=4)
        wbv = C.w_branch[l].rearrange("i (kc p) n -> p i kc n", p=128)
        loaded = {}

        def load(cc):
            if cc >= 16 or cc in loaded:
                return
            wg, wgb, wgsem = wgs.next()
            wb, wbb, wbsem = wbs.next()
            for i in range(4):
                k.dma("pool", wg[:, :, i, :], wgv[:, :, i, cc * 128:(cc + 1) * 128], wgsem, writes=[wgb])
            for i in range(4):
                k.dma("pool", wb[:, i, :, :], wbv[:, i, :, cc * 128:(cc + 1) * 128], wbsem, writes=[wbb])
            loaded[cc] = (wg, wgb, wb, wbb)

        load(0)
        for cc in range(16):
            load(cc + 1)
            wg, wgb, wb, wbb = loaded.pop(cc)
            st, stb, stsem = sts.next()
            for tg in range(4):
                tsl = slice(tg * 512, (tg + 1) * 512)
                sg_l = []
                for i in range(4):
                    p, pb, _ = pg.next()
                    for c in range(16):
                        k.mm(p[:, :], lhsT=wg[:, c, i, :], rhs=xnT[:, c, tsl], start=(c == 0), stop=(c == 15),
                             reads=[wgb, xb], writes=[pb], signal=(c == 15))
                    sg, sgb, _ = sgs.next()
                    k.op("act", lambda e: e.activation(out=sg[:, :], in_=p[:, :], func=AF.Sigmoid, bias=bg[:, i, cc:cc + 1]),
                         reads=[pb, bgb], writes=[sgb])
                    sg_l.append((sg, sgb))
                m, mb, _ = macc.next()
                for i in range(4):
                    p, pb, _ = pb_.next()
                    for kc in range(4):
                        k.mm(p[:, :], lhsT=wb[:, i, kc, :], rhs=oT[:, i * 4 + kc, tsl], start=(kc == 0), stop=(kc == 3),
                             reads=[wbb, ob], writes=[pb], signal=(kc == 3))
                    sg, sgb = sg_l[i]
                    if i == 0:
                        k.op("dve", lambda e: e.tensor_tensor(out=m[:, :], in0=p[:, :], in1=sg[:, :], op=ALU.mult),
                             reads=[pb, sgb], writes=[mb])
                    else:
                        t, tb, _ = tmp.next()
                        k.op("dve", lambda e: e.tensor_tensor(out=t[:, :], in0=p[:, :], in1=sg[:, :], op=ALU.mult),
                             reads=[pb, sgb], writes=[tb])
                        o_ap = m[:, :] if i < 3 else st[:, tsl]
                        k.op("pool", lambda e: e.tensor_tensor(out=o_ap, in0=m[:, :], in1=t[:, :], op=ALU.add),
                             reads=[mb, tb], writes=[mb] if i < 3 else [stb])
            k.dma("sp", C.mixT_d[cc * 128:(cc + 1) * 128, :], st[:, :], stsem, reads=[stb], writes=[C.scr_b["mixT"]])


def stage_out(k, C, l, h_src):
    with Scope(k) as sc:
        mixT = sc.sbuf("omixT", [128, 16, S], BF16)
        mb = Buf("omixT")
        k.dma("sp", mixT[:], C.mixT_d.rearrange("(c p) s -> p c s", p=128), k.dsem("mld0"), reads=[C.scr_b["mixT"]], writes=[mb])
        ws = sc.slots("ow", 2, [128, 16, 512], BF16)
        hs = sc.slots("oh", 2, [128, 16, 512], F32)
        ps = Slots(sc, "ops", 4, [128, 512], F32, psum=True)
        wv = C.w_out[l].rearrange("(c p) n -> p c n", p=128)
        hv = h_src.rearrange("(i p) n -> p i n", p=128)
        ho = C.h_d.rearrange("(i p) n -> p i n", p=128)
        loaded = {}

        def load(cb):
            if cb >= 4 or cb in loaded:
                return
            w, wb, wsem = ws.next()
            k.dma("pool", w[:], wv[:, :, cb * 512:(cb + 1) * 512], wsem, writes=[wb])
            h, hb, hsem = hs.next()
            k.dma("sp", h[:], hv[:, :, cb * 512:(cb + 1) * 512], hsem, reads=[C.scr_b["h"]], writes=[hb])
            loaded[cb] = (w, wb, h, hb, hsem)

        load(0)
        for cb in range(4):
            load(cb + 1)
            w, wb, h, hb, hsem = loaded.pop(cb)
            for i in range(16):
                p, pb, _ = ps.next()
                for c in range(16):
                    k.mm(p[:, :], lhsT=mixT[:, c, i * 128:(i + 1) * 128], rhs=w[:, c, :], start=(c == 0), stop=(c == 15),
                         reads=[mb, wb], writes=[pb], signal=(c == 15))
                k.op("dve", lambda e: e.tensor_tensor(out=h[:, i, :], in0=p[:, :], in1=h[:, i, :], op=ALU.add),
                     reads=[pb, hb], writes=[hb])
            k.dma("sp", ho[:, :, cb * 512:(cb + 1) * 512], h[:], hsem, reads=[hb], writes=[C.scr_b["h"]])


def moe_routing(k, C, lg, lgb, probs, pb_, mask, mkb, rank, rkb, gm, gmb, upto=99):
    ms = k.dsem("misc")
    with Scope(k) as sc:
        mx = sc.sbuf("emx", [128, 16], F32)
        mxb = Buf("emx")
        identf = sc.sbuf("eidf", [128, 128], F32)
        idfb = Buf("eidf")
        k.dma("sp", identf[:], C.c_ident, ms, writes=[idfb])
        ustr = sc.sbuf("eustr", [128, 128], BF16)
        onesb = sc.sbuf("eones", [128, 128], BF16)
        ub = Buf("eustr")
        k.dma("pool", ustr[:], C.c_ustr, ms, writes=[ub])
        k.dma("pool", onesb[:], C.c_ones, ms, writes=[ub])
        import os
        PRE = os.environ.get("PRE", "")
        k.op("dve", lambda e: e.tensor_reduce(out=mx[:], in_=lg[:], axis=AX.X, op=ALU.max), reads=[lgb], writes=[mxb])
        k.op("dve", lambda e: e.tensor_tensor(out=lg[:], in0=lg[:], in1=mx[:].unsqueeze(2).to_broadcast([128, 16, 16]),
                                              op=ALU.subtract), reads=[lgb, mxb], writes=[lgb])
        if "noexp" not in PRE:
            k.op("act", lambda e: e.activation(out=lg[:], in_=lg[:], func=AF.Exp), reads=[lgb], writes=[lgb])
        k.op("dve", lambda e: e.tensor_reduce(out=mx[:], in_=lg[:], axis=AX.X, op=ALU.add), reads=[lgb], writes=[mxb])
        k.op("dve", lambda e: e.reciprocal(out=mx[:], in_=mx[:]), reads=[mxb], writes=[mxb])
        k.op("dve", lambda e: e.tensor_tensor(out=probs[:], in0=lg[:], in1=mx[:].unsqueeze(2).to_broadcast([128, 16, 16]),
                                              op=ALU.mult), reads=[lgb, mxb], writes=[pb_])
        if upto < 1:
            return
        pT = sc.sbuf("epT", [128, S], F32)[0:16, :]
        work = sc.sbuf("ework", [128, S], F32)[0:16, :]
        mT = sc.sbuf("emT", [128, S], F32)[0:16, :]
        pTb, wkb, mTb = Buf("epT"), Buf("ework"), Buf("emT")
        m8 = sc.sbuf("em8", [128, 8], F32)[0:16, :]
        m8b = Buf("em8")
        ppT = Slots(sc, "eppT", 4, [128, 512], F32, psum=True)
        import os
        NB = int(os.environ.get("NB", "4"))
        for bnk in range(NB):
            p, pb2, _ = ppT.next()
            for j in range(4):
                i = bnk * 4 + j
                if "mm" not in os.environ.get("SKIP", ""):
                    k.mm(p[0:16, j * 128:(j + 1) * 128], lhsT=probs[:, i, :], rhs=identf[:], start=True, stop=True,
                         reads=[pb_, idfb], writes=[pb2], signal=(j == 3))
            if "act" not in os.environ.get("SKIP", ""):
                k.op("act", lambda e: e.copy(out=pT[:, bnk * 512:(bnk + 1) * 512], in_=p[0:16, :]), reads=[pb2], writes=[pTb])
            if "dve" not in os.environ.get("SKIP", ""):
                k.op("dve", lambda e: e.tensor_copy(out=work[:, bnk * 512:(bnk + 1) * 512],
                                                    in_=pT[:, bnk * 512:(bnk + 1) * 512]), reads=[pTb], writes=[wkb])
        if upto < 2:
            return
        for r in range(CAP // 8):
            k.op("dve", lambda e: e.max(out=m8[:], in_=work[:]), reads=[wkb], writes=[m8b])
            if r < CAP // 8 - 1:
                k.op("dve", lambda e: e.match_replace(out=work[:], in_to_replace=m8[:], in_values=work[:], imm_value=-1.0),
                     reads=[wkb, m8b], writes=[wkb])
        k.op("dve", lambda e: e.tensor_scalar(out=mT[:], in0=pT[:], scalar1=m8[:, 7:8], scalar2=None, op0=ALU.is_ge),
             reads=[pTb, m8b], writes=[mTb])
        if upto < 3:
            return
        pm = Slots(sc, "epm", 1, [128, 256], F32, psum=True)
        p, pb2, _ = pm.next()
        for i in range(16):
            k.mm(p[:, i * 16:(i + 1) * 16], lhsT=mT[:, i * 128:(i + 1) * 128], rhs=identf[0:16, 0:16], start=True,
                 stop=True, reads=[mTb, idfb], writes=[pb2], signal=(i == 15))
        k.op("act", lambda e: e.copy(out=mask[:].rearrange("p i e -> p (i e)"), in_=p[:, :]), reads=[pb2], writes=[mkb])
        maskbf = sc.sbuf("emaskbf", [128, 16, 16], BF16)
        mbfb = Buf("emaskbf")
        k.op("dve", lambda e: e.tensor_copy(out=maskbf[:], in_=mask[:]), reads=[mkb], writes=[mbfb])
        k.op("dve", lambda e: e.tensor_tensor(out=gm[:], in0=probs[:], in1=mask[:], op=ALU.mult), reads=[pb_, mkb],
             writes=[gmb])
        if upto < 4:
            return
        pr = Slots(sc, "epr", 1, [128, 256], F32, psum=True)
        p, pb2, _ = pr.next()
        for i in range(16):
            for j in range(i + 1):
                k.mm(p[:, i * 16:(i + 1) * 16], lhsT=(ustr[:] if j == i else onesb[:]), rhs=maskbf[:, j, :],
                     start=(j == 0), stop=(j == i), reads=[ub, mbfb], writes=[pb2], signal=(i == 15 and j == i))
        k.op("act", lambda e: e.copy(out=rank[:].rearrange("p i e -> p (i e)"), in_=p[:, :]), reads=[pb2], writes=[rkb])


def stage_moe(k, C, l, dbg=None):
    with Scope(k) as osc:
        ms = k.dsem("misc")
        xn = osc.sbuf("exn", [128, 16, D_MODEL], BF16)
        xnb = [Buf(f"exn{i}") for i in range(16)]
        lg = osc.sbuf("elg", [128, 16, 16], F32)
        lgb = Buf("elg")
        wr = osc.sbuf("ewr", [128, 16, 16], BF16)
        wrb = Buf("ewr")
        k.dma("pool", wr[:], C.w_router[l].rearrange("(c p) e -> p c e", p=128), ms, writes=[wrb])
        stage_norm(k, C, C.h_d, C.g_ffn[l], xn_sb=xn, xn_bufs=xnb, router=(wr, wrb, lg, lgb))
        probs = osc.sbuf("eprobs", [128, 16, 16], F32)
        mask = osc.sbuf("emask", [128, 16, 16], F32)
        rank = osc.sbuf("erank", [128, 16, 16], F32)
        gm = osc.sbuf("egm", [128, 16, 16], F32)
        pb_, mkb, rkb, gmb = Buf("eprobs"), Buf("emask"), Buf("erank"), Buf("egm")
        iota = osc.sbuf("eiota", [128, 256], F32)
        iob = Buf("eiota")
        k.dma("sp", iota[:], C.c_iota, ms, writes=[iob])
        moe_routing(k, C, lg, lgb, probs, pb_, mask, mkb, rank, rkb, gm, gmb)
        if dbg:
            st = k.dsem("bstore")
            k.dma("sp", C.dbg_probs.rearrange("(i p) e -> p i e", p=128), probs[:], st, reads=[pb_], writes=[C.scr_b["moe"]])
            k.dma("sp", C.dbg_mask.rearrange("(i p) e -> p i e", p=128), mask[:], st, reads=[mkb], writes=[C.scr_b["moe"]])
            k.dma("sp", C.dbg_rank.rearrange("(i p) e -> p i e", p=128), rank[:], st, reads=[rkb], writes=[C.scr_b["moe"]])
        if dbg and 6 in dbg:
            return
        with Scope(k) as sc:
            sels = sc.slots("esel", 2, [128, 16, 256], BF16)
            selg = sc.slots("eselg", 1, [128, 16, 256], BF16)
            sgT = sc.slots("esgT", 1, [128, 2, S], BF16)
            xeTs = sc.slots("exeT", 1, [128, 16, 256], BF16)
            hTs = sc.slots("ehT", 1, [128, 8, 256], BF16)
            sgt = sc.slots("esg", 2, [128, 256], F32)
            yes = sc.slots("eye", 1, [128, 2, D_MODEL], BF16)
            NW = 9
            wring = sc.slots("ewr", NW, [128, 4096], BF16)
            pgt = Slots(sc, "epg", 2, [128, 512], F32, psum=True)
            ptr = Slots(sc, "eptr", 1, [128, 1024], BF16, psum=True)
            pgu = Slots(sc, "epgu", 2, [128, 512], F32, psum=True)
            py = Slots(sc, "epy", 2, [128, 512], F32, psum=True)
            ident, identb = C.ident_bf, C.ident_bf_b
            wjobs = []
            for e in range(NE):
                for fb in range(4):
                    wjobs.append((e, 0, fb))
                    wjobs.append((e, 1, fb))
                for cb in range(4):
                    wjobs.append((e, 2, cb))
            wl = {}
            wnext = [0]

            def ensure(upto):
                while wnext[0] <= upto and wnext[0] < len(wjobs):
                    j = wnext[0]
                    e, kind, idx = wjobs[j]
                    w, wb, wsem = wring.next()
                    if kind < 2:
                        src = (C.w_exp_gate if kind == 0 else C.w_exp_up)[l, e].rearrange("(c p) f -> p c f", p=128)
                        wv_ = w[:].rearrange("p (c f) -> p c f", f=256)
                        k.dma("pool", wv_, src[:, :, idx * 256:(idx + 1) * 256], wsem, writes=[wb])
                    else:
                        src = C.w_exp_down[l, e].rearrange("(f p) n -> p f n", p=128)
                        wv_ = w[:].rearrange("p (f n) -> p f n", n=512)
                        k.dma("pool", wv_, src[:, :, idx * 512:(idx + 1) * 512], wsem, writes=[wb])
                    wl[j] = (wv_, wb)
                    wnext[0] += 1

            ensure(NW - 1)
            def build_sel(e):
                sel, selb, _ = sels.next()
                for i in range(16):
                    k.op("dve", lambda en: en.tensor_scalar(out=sel[:, i, :], in0=iota[:], scalar1=rank[:, i, e:e + 1],
                                                            scalar2=mask[:, i, e:e + 1], op0=ALU.is_equal, op1=ALU.mult),
                         reads=[iob, rkb, mkb], writes=[selb])
                return sel, selb

            nxt_sel = build_sel(0)
            for e in range(NE):
                sel, selb = nxt_sel
                sg_, sgb_, _ = selg.next()
                for i in range(16):
                    k.op("dve", lambda en: en.tensor_scalar(out=sg_[:, i, :], in0=iota[:], scalar1=rank[:, i, e:e + 1],
                                                            scalar2=gm[:, i, e:e + 1], op0=ALU.is_equal, op1=ALU.mult),
                         reads=[iob, rkb, gmb], writes=[sgb_])
                xeT, xeTb, _ = xeTs.next()
                for d2 in range(8):
                    p, pb2, _ = pgt.next()
                    for dd in range(2):
                        dc = d2 * 2 + dd
                        for i in range(16):
                            k.mm(p[:, dd * 256:(dd + 1) * 256], lhsT=xn[:, i, dc * 128:(dc + 1) * 128], rhs=sel[:, i, :],
                                 start=(i == 0), stop=(i == 15), reads=[xnb[i], selb], writes=[pb2],
                                 signal=(i == 15 and dd == 1))
                    k.op("act", lambda en: en.copy(out=xeT[:, d2 * 2:d2 * 2 + 2, :],
                                                   in_=p[:, :].rearrange("p (a c) -> p a c", a=2)),
                         reads=[pb2], writes=[xeTb])
                st_, stb_, stsem = sgT.next()
                for ct in range(2):
                    for half in range(2):
                        p, pb2, _ = ptr.next()
                        for j in range(8):
                            i = half * 8 + j
                            k.tr(p[:, j * 128:(j + 1) * 128], sg_[:, i, ct * 128:(ct + 1) * 128], ident[:],
                                 reads=[sgb_, identb], writes=[pb2], signal=(j == 7))
                        k.op("dve", lambda en: en.tensor_copy(out=st_[:, ct, half * 1024:(half + 1) * 1024], in_=p[:, :]),
                             reads=[pb2], writes=[stb_])
                    k.dma("sp", C.selGT_d[:, :, 2 * e + ct, :].rearrange("i c t -> c i t"),
                          st_[:, ct, :].rearrange("c (i t) -> c i t", t=128), stsem, reads=[stb_], writes=[C.scr_b["moe"]])
                hT, hTb, _ = hTs.next()
                for fb in range(4):
                    j0 = e * 12 + fb * 2
                    ensure(j0 + NW - 1)
                    wg, wgb = wl.pop(j0)
                    wu, wub = wl.pop(j0 + 1)
                    for fc in range(2):
                        p, pb2, _ = pgu.next()
                        for (w_, wb_, off) in ((wg, wgb, 0), (wu, wub, 256)):
                            for dc in range(16):
                                k.mm(p[:, off:off + 256], lhsT=w_[:, dc, fc * 128:(fc + 1) * 128], rhs=xeT[:, dc, :],
                                     start=(dc == 0), stop=(dc == 15), reads=[wb_, xeTb], writes=[pb2],
                                     signal=(dc == 15 and off == 256))
                        s_, sb_, _ = sgt.next()
                        k.op("act", lambda en: en.activation(out=s_[:, :], in_=p[:, 0:256], func=AF.Silu), reads=[pb2],
                             writes=[sb_])
                        k.op("dve", lambda en: en.tensor_tensor(out=hT[:, fb * 2 + fc, :], in0=p[:, 256:512], in1=s_[:, :],
                                                                op=ALU.mult), reads=[pb2, sb_], writes=[hTb])
                if e + 1 < NE:
                    nxt_sel = build_sel(e + 1)
                ye, yeb, yesem = yes.next()
                for cb in range(4):
                    j = e * 12 + 8 + cb
                    ensure(j + NW - 1)
                    wd, wdb = wl.pop(j)
                    for ct in range(2):
                        p, pb2, _ = py.next()
                        for f in range(8):
                            k.mm(p[:, :], lhsT=hT[:, f, ct * 128:(ct + 1) * 128], rhs=wd[:, f, :], start=(f == 0),
                                 stop=(f == 7), reads=[hTb, wdb], writes=[pb2], signal=(f == 7))
                        k.op("act", lambda en: en.copy(out=ye[:, ct, cb * 512:(cb + 1) * 512], in_=p[:, :]), reads=[pb2],
                             writes=[yeb])
                k.dma("sp", C.ye_d[:, 2 * e:2 * e + 2, :], ye[:], yesem, reads=[yeb], writes=[C.scr_b["moe"]])
    if dbg and 7 in dbg:
        return
    with Scope(k) as sc:
        yes = sc.slots("sye", 2, [128, 32, 512], BF16)
        sls = sc.slots("ssl", 2, [128, 32, 128], BF16)
        hs = sc.slots("sh", 2, [128, 16, 512], F32)
        ps = Slots(sc, "sps", 3, [128, 512], F32, psum=True)
        hv = C.h_d.rearrange("(i p) n -> p i n", p=128)
        for cb in range(4):
            ye, yeb, yesem = yes.next()
            k.dma("sp", ye[:], C.ye_d[:, :, cb * 512:(cb + 1) * 512], yesem, reads=[C.scr_b["moe"]], writes=[yeb])
            h, hb, hsem = hs.next()
            k.dma("sp", h[:], hv[:, :, cb * 512:(cb + 1) * 512], hsem, reads=[C.scr_b["h"]], writes=[hb])
            for i in range(16):
                sl, slb, slsem = sls.next()
                k.dma("sp", sl[:], C.selGT_d[i], slsem, reads=[C.scr_b["moe"]], writes=[slb])
                p, pb2, _ = ps.next()
                for et in range(32):
                    k.mm(p[:, :], lhsT=sl[:, et, :], rhs=ye[:, et, :], start=(et == 0), stop=(et == 31), reads=[slb, yeb],
                         writes=[pb2], signal=(et == 31))
                k.op("dve", lambda en: en.tensor_tensor(out=h[:, i, :], in0=p[:, :], in1=h[:, i, :], op=ALU.add),
                     reads=[pb2, hb], writes=[hb])
            k.dma("sp", hv[:, :, cb * 512:(cb + 1) * 512], h[:], hsem, reads=[hb], writes=[C.scr_b["h"]])


def stage_final(k, C, yb):
    with Scope(k) as sc:
        hs = sc.slots("fh", 3, [128, D_MODEL], F32)
        os_ = sc.slots("fo", 2, [128, D_MODEL], F32)
        gbc = sc.sbuf("fgbc", [128, D_MODEL], F32)
        gb = Buf("fgbc")
        k.dma("sp", gbc[:], C.g_final.partition_broadcast(128), k.dsem("misc"), writes=[gb])
        junk = sc.sbuf("fjunk", [128, D_MODEL], BF16)
        jb = Buf("fjunk")
        ss = sc.sbuf("fss", [128, 16], F32)
        rs = sc.sbuf("frs", [128, 16], F32)
        for i in range(16):
            ht, hb, hsem = hs.next()
            k.dma("sp", ht[:], C.h_d[i * 128:(i + 1) * 128, :], hsem, reads=[C.scr_b["h"]], writes=[hb])
            ssb, rsb = Buf("fss"), Buf("frs")
            k.op("act", lambda e: e.activation(out=junk[:], in_=ht[:], func=AF.Square, accum_out=ss[:, i:i + 1]),
                 reads=[hb], writes=[jb, ssb])
            _rstd(k, ss[:, i:i + 1], rs[:, i:i + 1], 1, 1.0 / D_MODEL, ssb, rsb)
            ot, ob, osem = os_.next()
            k.op("dve", lambda e: e.scalar_tensor_tensor(out=ot[:], in0=ht[:], scalar=rs[:, i:i + 1], in1=gbc[:],
                                                         op0=ALU.mult, op1=ALU.mult), reads=[hb, rsb, gb], writes=[ob])
            k.dma("sp", C.y[i * 128:(i + 1) * 128, :], ot[:], osem, reads=[ob], writes=[yb])


def alloc_scratch(k, C, dbg):
    kind = "ExternalOutput" if dbg else "Internal"
    C.scr_b = {n: Buf(n) for n in ("h", "xnT", "proj", "qkv", "oT", "mixT", "moe")}
    C.h_d = k.dram("h_d", [S, D_MODEL], F32, kind).ap()
    C.xnT_d = k.dram("xnT_d", [D_MODEL, S], BF16).ap()
    C.A_qT = k.dram("A_qT", [1536, S], BF16, kind).ap()
    C.A_kT = k.dram("A_kT", [1536, S], BF16, kind).ap()
    C.A_v = [k.dram(f"A_v{g}", [S, 512], BF16, kind).ap() for g in range(3)]
    C.B_tm = k.dram("B_tm", [S, 800], F32, kind).ap()
    C.C_tm = k.dram("C_tm", [S, 640], F32, kind).ap()
    C.C_v = k.dram("C_v", [S, 128], BF16, kind).ap()
    C.D_qT = k.dram("D_qT", [512, S], BF16, kind).ap()
    C.D_kT = k.dram("D_kT", [128, S], BF16, kind).ap()
    C.D_v = k.dram("D_v", [S, 128], BF16, kind).ap()
    C.B_qT = k.dram("B_qT", [8, 96, S], BF16, kind).ap()
    C.B_kT = k.dram("B_kT", [8, 96, S], BF16, kind).ap()
    C.B_v = k.dram("B_v", [S, 512], BF16, kind).ap()
    C.C_qT = k.dram("C_qT", [8, 64, S], BF16, kind).ap()
    C.C_kT = k.dram("C_kT", [2, 64, S], BF16, kind).ap()
    C.oT_d = k.dram("oT_d", [2048, S], BF16, kind).ap()
    C.mixT_d = k.dram("mixT_d", [D_MODEL, S], BF16, kind).ap()
    C.selGT_d = k.dram("selGT_d", [16, 128, 32, 128], BF16).ap()
    C.ye_d = k.dram("ye_d", [128, 32, D_MODEL], BF16, kind).ap()
    if dbg:
        C.dbg_probs = k.dram("dbg_probs", [S, 16], F32, kind).ap()
        C.dbg_mask = k.dram("dbg_mask", [S, 16], F32, kind).ap()
        C.dbg_rank = k.dram("dbg_rank", [S, 16], F32, kind).ap()


def build(dbg=None):
    k = K()
    C = Ctx()
    C.x = k.dram("x", [S, D_MODEL], F32, "ExternalInput").ap()
    C.w_in = k.dram("w_in", [DEPTH, D_MODEL, N_IN], F32, "ExternalInput").ap()
    C.g_attn = k.dram("g_attn_norm", [DEPTH, D_MODEL], F32, "ExternalInput").ap()
    C.c_ident = k.dram("c_ident", [128, 128], F32, "ExternalInput").ap()
    C.c_bias = k.dram("c_bias", [32, 128, 384], F32, "ExternalInput").ap()
    C.c_ones = k.dram("c_ones", [128, 128], F32, "ExternalInput").ap()
    C.c_sel64 = k.dram("c_sel64", [128, 64], F32, "ExternalInput").ap()
    C.sink_d = k.dram("sink_d", [DEPTH, 8], F32, "ExternalInput").ap()
    C.g_mla_q = k.dram("g_mla_q", [DEPTH, 512], F32, "ExternalInput").ap()
    C.b_gate = k.dram("b_gate", [DEPTH, 4, D_MODEL], F32, "ExternalInput").ap()
    C.g_ffn = k.dram("g_ffn_norm", [DEPTH, D_MODEL], F32, "ExternalInput").ap()
    C.g_final = k.dram("g_final", [D_MODEL], F32, "ExternalInput").ap()
    C.w_router = k.dram("w_router", [DEPTH, D_MODEL, NE], F32, "ExternalInput").ap()
    C.w_exp_gate = k.dram("w_exp_gate", [DEPTH, NE, D_MODEL, EFF], F32, "ExternalInput").ap()
    C.w_exp_up = k.dram("w_exp_up", [DEPTH, NE, D_MODEL, EFF], F32, "ExternalInput").ap()
    C.w_exp_down = k.dram("w_exp_down", [DEPTH, NE, EFF, D_MODEL], F32, "ExternalInput").ap()
    C.c_iota = k.dram("c_iota", [128, 256], F32, "ExternalInput").ap()
    C.c_ustr = k.dram("c_ustr", [128, 128], F32, "ExternalInput").ap()
    C.w_branch = k.dram("w_branch", [DEPTH, 4, 512, D_MODEL], F32, "ExternalInput").ap()
    C.w_out = k.dram("w_out", [DEPTH, D_MODEL, D_MODEL], F32, "ExternalInput").ap()
    C.g_mla_kv = k.dram("g_mla_kv", [DEPTH, 256], F32, "ExternalInput").ap()
    C.w_mla_uq = k.dram("w_mla_uq", [DEPTH, 512, 768], F32, "ExternalInput").ap()
    C.w_mla_ukv = k.dram("w_mla_ukv", [DEPTH, 256, 1024], F32, "ExternalInput").ap()
    C.g_c_q = k.dram("g_c_q", [DEPTH, 64], F32, "ExternalInput").ap()
    C.g_c_k = k.dram("g_c_k", [DEPTH, 64], F32, "ExternalInput").ap()
    C.c_ropeB = k.dram("c_ropeB", [S, 32], F32, "ExternalInput").ap()
    C.c_ropeC = k.dram("c_ropeC", [S, 64], F32, "ExternalInput").ap()
    C.y = k.dram("y", [S, D_MODEL], F32, "ExternalOutput").ap()
    alloc_scratch(k, C, dbg)
    yb = Buf("y")

    with Scope(k) as top:
        C.ident_bf = top.sbuf("ident_bf", [128, 128], BF16)
        C.ident_bf_b = Buf("ident_bf")
        k.dma("pool", C.ident_bf[:], C.c_ident, k.dsem("misc"), writes=[C.ident_bf_b])
        C.ones_f = top.sbuf("ones_f", [128, 128], F32)
        C.ones_f_b = Buf("ones_f")
        k.dma("sp", C.ones_f[:], C.c_ones, k.dsem("misc"), writes=[C.ones_f_b])
        for l in range(1 if dbg else DEPTH):
            with Scope(k) as lsc:
                xnT = lsc.sbuf("xnT", [128, 16, S], BF16)
                xnTb = [Buf(f"xnT{i}") for i in range(16)]
                with k.nc.named_scope('stage_norm'):
                    stage_norm(k, C, C.x if l == 0 else C.h_d, C.g_attn[l], xnT_sb=xnT, xnT_bufs=xnTb, featT_dst=C.xnT_d)
                with k.nc.named_scope('stage_proj'):
                    stage_proj(k, C, l, xnT, xnTb)
            if not dbg or 1 in dbg:
                with k.nc.named_scope('stage_prepB'):
                    stage_prepB(k, C, l)
            if not dbg or 2 in dbg:
                with k.nc.named_scope('stage_prepC'):
                    stage_prepC(k, C, l)
            units = attn_units(C)
            if dbg:
                units = [u for u in units if u["branch"] in dbg]
            with k.nc.named_scope('stage_attn'):
                stage_attn(k, C, l, units)
            if not dbg or 4 in dbg:
                with k.nc.named_scope('stage_merge'):
                    stage_merge(k, C, l)
                with k.nc.named_scope('stage_out'):
                    stage_out(k, C, l, C.x if l == 0 else C.h_d)
            if dbg and 4 not in dbg:
                k.dma("sp", C.h_d, C.x, k.dsem("misc"), writes=[C.scr_b["h"]])
            if not dbg or 5 in dbg:
                with k.nc.named_scope('stage_moe'):
                    stage_moe(k, C, l, dbg)
        if not dbg or 8 in dbg:
            with k.nc.named_scope('stage_final'):
                stage_final(k, C, yb)
        else:
            k.dma('sp', C.y[0:128, :], C.x[0:128, :], k.dsem('out'), writes=[yb])
        k.barrier()
    k.finish()
    return k


def host_consts():
    c = {"c_ident": np.eye(128, dtype=np.float32), "c_ones": np.ones((128, 128), np.float32)}
    c["c_sel64"] = np.zeros((128, 64), np.float32)
    c["c_sel64"][64, :] = 1.0
    c["c_iota"] = np.tile(np.arange(256, dtype=np.float32)[None, :], (128, 1))
    c["c_ustr"] = (np.arange(128)[:, None] < np.arange(128)[None, :]).astype(np.float32)
    bias = np.zeros((32, 128, 384), np.float32)
    kk = np.arange(128)[:, None].astype(np.float64)
    qq = np.arange(128)[None, :].astype(np.float64)
    dists = [qq - kk + 128, np.abs(qq - kk), kk + 128 - qq]
    for idx in range(32):
        if idx < 24:
            g, h = divmod(idx, 8)
            slope = 2.0 ** (-(h + 1)) * A_DIL[g]
            W = 64
        else:
            h = idx - 24
            slope = 2.0 ** (-(h + 1))
            W = 128
        for t in range(3):
            d = dists[t]
            bias[idx, :, t * 128:(t + 1) * 128] = np.where(d <= W, -slope * d * 8.0, -240000.0)
    c["c_bias"] = bias
    freqs = 10000.0 ** (-np.arange(0, 32, 2, dtype=np.float32) / 32)
    pos = np.arange(S, dtype=np.float32)
    ang = pos[:, None] * freqs[None, :]
    c["c_ropeB"] = np.concatenate([np.cos(ang), np.sin(ang)], axis=1).astype(np.float32)
    rows = np.repeat(np.arange(S // 64), 64).astype(np.float32)
    cols = np.tile(np.arange(64), S // 64).astype(np.float32)
    ar = rows[:, None] * freqs[None, :]
    ac = cols[:, None] * freqs[None, :]
    c["c_ropeC"] = np.concatenate([np.cos(ar), np.sin(ar), np.cos(ac), np.sin(ac)], axis=1).astype(np.float32)
    return c


def kernel(**inputs):
    k = build()
    consts = host_consts()
    in_maps = []
    for b in range(8):
        m = {"x": np.ascontiguousarray(inputs["x"][b])}
        for n in WEIGHT_NAMES:
            m[n] = np.ascontiguousarray(inputs[n])
        m.update(consts)
        in_maps.append(m)
    res = run_bass_kernel_spmd(k.nc, in_maps, core_ids=list(range(8)))
    return np.stack([r["y"] for r in res.results], axis=0)
```
